# Optimizing a Trainium2 kernel written in Bass

```python
import jax, jax.numpy as jnp
from jax import lax
import numpy as np

D_MODEL = 1024
BATCH = 8
SEQ = 2048
DEPTH = 2

CHUNK = 64
HEAD_DIM = 64
SWA_HEADS = 8
SWA_KV_HEADS = 2
SWA_GROUP = SWA_HEADS // SWA_KV_HEADS
SWA_WINDOW = 128
SWA_WIN_CHUNKS = (SWA_WINDOW + CHUNK - 1) // CHUNK
RWKV_HEADS = 4
RWKV_W_RANK = 64
RWKV_A_RANK = 64
RWKV_G_RANK = 128
RWKV_GN_EPS = 64e-5
FOX_HEADS = 4
FOX_BLOCK = 128
DSA_HEADS = 4
IDX_HEADS = 4
IDX_DIM = 64
DSA_TOPK_MAX = 256
DSA_BLOCK = 128
D_FF = 2816
N_EXPERTS = 8
TOP_K = 2
D_FF_EXPERT = 1408
N_DENSE = (DEPTH + 1) // 2
N_MOE = DEPTH // 2
N_BRANCHES = 4
DN_ALPHA = (2 * DEPTH) ** 0.25
DN_BETA = (8 * DEPTH) ** -0.25
LN_EPS = 1e-5
ATTN_SCALE = HEAD_DIM ** -0.5
IDX_SCALE = IDX_DIM ** -0.5
IDX_W_SCALE = IDX_HEADS ** -0.5

SWA_Q = SWA_HEADS * HEAD_DIM
SWA_KV = SWA_KV_HEADS * HEAD_DIM
RWKV_W = RWKV_HEADS * HEAD_DIM
FOX_W = FOX_HEADS * HEAD_DIM
DSA_W = DSA_HEADS * HEAD_DIM
MIX_WIDTHS = (SWA_Q, RWKV_W, FOX_W, DSA_W)
MIX_WIDTH = SWA_Q + RWKV_W + FOX_W + DSA_W
SWA_SPLITS = (SWA_Q, SWA_KV, SWA_KV)
RWKV_SPLITS = (RWKV_W, RWKV_W, RWKV_W, RWKV_W_RANK, RWKV_A_RANK, RWKV_G_RANK)
FOX_SPLITS = (FOX_W, FOX_W, FOX_W, FOX_HEADS)
DSA_SPLITS = (DSA_W, HEAD_DIM, HEAD_DIM, IDX_HEADS * IDX_DIM, IDX_DIM, IDX_HEADS)
RWKV_IN = sum(RWKV_SPLITS)
GATE_COLS = N_BRANCHES * D_MODEL
IN_GROUPS = (sum(SWA_SPLITS), RWKV_IN, sum(FOX_SPLITS), sum(DSA_SPLITS), GATE_COLS)
D_IN = sum(IN_GROUPS)

kernel_name = 'hybrid_gated_4mixer_deepnorm_moe'


def split_cols(t, sizes, axis=-1):
    idx = [int(i) for i in np.cumsum(sizes)[:-1]]
    return jnp.split(t, idx, axis=axis)


def layer_norm(x, g, b):
    xf = x.astype(jnp.float32)
    mu = jnp.mean(xf, axis=-1, keepdims=True)
    var = jnp.mean(jnp.square(xf - mu), axis=-1, keepdims=True)
    return ((xf - mu) * lax.rsqrt(var + LN_EPS) * g + b).astype(x.dtype)


def swa_branch(q, k, v, sinks):
    B, S, _ = q.shape
    nc = S // CHUNK
    W = SWA_WIN_CHUNKS
    q = q.reshape(B, nc, CHUNK, SWA_KV_HEADS, SWA_GROUP, HEAD_DIM)
    pad = ((0, 0), (W, 0), (0, 0), (0, 0), (0, 0))
    kp = jnp.pad(k.reshape(B, nc, CHUNK, SWA_KV_HEADS, HEAD_DIM), pad)
    vp = jnp.pad(v.reshape(B, nc, CHUNK, SWA_KV_HEADS, HEAD_DIM), pad)
    kb = jnp.concatenate([kp[:, j:j + nc] for j in range(W + 1)], axis=2)
    vb = jnp.concatenate([vp[:, j:j + nc] for j in range(W + 1)], axis=2)
    key_chunk = jnp.arange(nc)[:, None] - W + jnp.arange(W + 1)[None, :]
    valid = jnp.repeat(key_chunk >= 0, CHUNK, axis=1)
    logits = jnp.einsum('bcqhgd,bckhd->bchgqk', q, kb).astype(jnp.float32) * ATTN_SCALE
    logits = jnp.where(valid[None, :, None, None, None, :], logits, -jnp.inf)
    sink = sinks.astype(jnp.float32).reshape(1, 1, SWA_KV_HEADS, SWA_GROUP, 1, 1)
    m = jnp.maximum(jnp.max(logits, axis=-1, keepdims=True), sink)
    e = jnp.exp(logits - m)
    p = e / (jnp.sum(e, axis=-1, keepdims=True) + jnp.exp(sink - m))
    o = jnp.einsum('bchgqk,bckhd->bcqhgd', p.astype(v.dtype), vb)
    return o.reshape(B, S, SWA_Q)


def rwkv7_branch(p, mu, w0, w_up, a0, a_up, g_up, k_k, k_a, r_k, gn_g, gn_b):
    B, S, _ = p.shape
    p_prev = jnp.pad(p[:, :-1], ((0, 0), (1, 0), (0, 0)))
    p = p + mu * (p_prev - p)
    r, k, v, w_lo, a_lo, g_lo = split_cols(p, RWKV_SPLITS)
    log_w = -jnp.exp(-jax.nn.softplus(-(w0 + jnp.tanh(w_lo) @ w_up).astype(jnp.float32)) - 0.5)
    a = jax.nn.sigmoid(a0 + a_lo @ a_up)
    g = jax.nn.sigmoid(g_lo) @ g_up
    hd = lambda t: t.reshape(B, S, RWKV_HEADS, HEAD_DIM).astype(jnp.float32)
    kk = hd(k * k_k)
    kk = kk * lax.rsqrt(jnp.sum(kk * kk, axis=-1, keepdims=True) + 1e-12)
    k = k * (1.0 + (a - 1.0) * k_a)
    r_h, k_h, v_h, a_h, w_h = hd(r), hd(k), hd(v), hd(a), hd(jnp.exp(log_w))
    seq = tuple(jnp.moveaxis(t, 1, 0) for t in (r_h, w_h, k_h, v_h, kk, a_h))

    def step(state, inp):
        r_t, w_t, k_t, v_t, kk_t, a_t = inp
        sa = jnp.einsum('bhvk,bhk->bhv', state, -kk_t)
        state = (state * w_t[:, :, None, :]
                 + sa[..., None] * (kk_t * a_t)[:, :, None, :]
                 + v_t[..., None] * k_t[:, :, None, :])
        return state, jnp.einsum('bhvk,bhk->bhv', state, r_t)

    s0 = jnp.zeros((B, RWKV_HEADS, HEAD_DIM, HEAD_DIM), jnp.float32)
    _, y = lax.scan(step, s0, seq)
    y = jnp.moveaxis(y, 0, 1)
    ym = jnp.mean(y, axis=-1, keepdims=True)
    yv = jnp.mean(jnp.square(y - ym), axis=-1, keepdims=True)
    y = ((y - ym) * lax.rsqrt(yv + RWKV_GN_EPS)).reshape(B, S, RWKV_W) * gn_g + gn_b
    bonus = (jnp.sum(r_h * k_h * r_k, axis=-1, keepdims=True) * v_h).reshape(B, S, RWKV_W)
    return ((y + bonus) * g).astype(p.dtype)


def fox_branch(q, k, v, f_logit, f_bias):
    B, S, _ = q.shape
    q = q.reshape(B, S, FOX_HEADS, HEAD_DIM)
    k = k.reshape(B, S, FOX_HEADS, HEAD_DIM)
    v = v.reshape(B, S, FOX_HEADS, HEAD_DIM)
    log_f = jax.nn.log_sigmoid(f_logit.astype(jnp.float32) + f_bias)
    c = jnp.cumsum(log_f, axis=1).transpose(0, 2, 1)
    outs = []
    for blk in range(S // FOX_BLOCK):
        lo, hi = blk * FOX_BLOCK, (blk + 1) * FOX_BLOCK
        logits = jnp.einsum('bqhd,bkhd->bhqk', q[:, lo:hi], k[:, :hi]).astype(jnp.float32) * ATTN_SCALE
        logits = logits + c[:, :, lo:hi, None] - c[:, :, None, :hi]
        causal = jnp.arange(lo, hi)[:, None] >= jnp.arange(hi)[None, :]
        logits = jnp.where(causal, logits, -jnp.inf)
        p = jax.nn.softmax(logits, axis=-1)
        outs.append(jnp.einsum('bhqk,bkhd->bqhd', p.astype(v.dtype), v[:, :hi]))
    return jnp.concatenate(outs, axis=1).reshape(B, S, FOX_W)


def dsa_branch(q, k, v, q_idx, k_idx, w_idx):
    B, S, _ = q.shape
    topk = min(DSA_TOPK_MAX, S // 4)
    nb = S // DSA_BLOCK
    qT = jnp.moveaxis(q.reshape(B, nb, DSA_BLOCK, DSA_HEADS, HEAD_DIM), 1, 0)
    qiT = jnp.moveaxis(q_idx.reshape(B, nb, DSA_BLOCK, IDX_HEADS, IDX_DIM), 1, 0)
    wiT = jnp.moveaxis(w_idx.reshape(B, nb, DSA_BLOCK, IDX_HEADS), 1, 0)
    key_chunk = jnp.arange(S) // CHUNK

    def block(args):
        blk, qb, qib, wib = args
        t = blk * DSA_BLOCK + jnp.arange(DSA_BLOCK)
        admissible = key_chunk[None, :] <= (t // CHUNK)[:, None]
        rel = jax.nn.relu(jnp.einsum('bqhd,bsd->bqhs', qib, k_idx) * IDX_SCALE)
        score = jnp.einsum('bqhs,bqh->bqs', rel, wib * IDX_W_SCALE).astype(jnp.float32)
        score = jnp.where(admissible[None], score, -jnp.inf)
        top_val, top_idx = lax.top_k(score, topk)
        valid = jnp.isfinite(top_val)
        k_sel = jax.vmap(lambda kk, ii: kk[ii])(k, top_idx)
        v_sel = jax.vmap(lambda vv, ii: vv[ii])(v, top_idx)
        logits = jnp.einsum('bqhd,bqkd->bqhk', qb, k_sel).astype(jnp.float32) * ATTN_SCALE
        logits = jnp.where(valid[:, :, None, :], logits, -jnp.inf)
        p = jax.nn.softmax(logits, axis=-1)
        return jnp.einsum('bqhk,bqkd->bqhd', p.astype(v.dtype), v_sel)

    outs = lax.map(block, (jnp.arange(nb), qT, qiT, wiT))
    return jnp.moveaxis(outs, 0, 1).reshape(B, S, DSA_W)


def mixer_block(x, w_in, sinks, mu, w0, w_up, a0, a_up, g_up, k_k, k_a, r_k, gn_g, gn_b,
                f_bias, gate_bias, w_branch, w_out):
    B, S, _ = x.shape
    p_swa, p_rwkv, p_fox, p_dsa, p_gate = split_cols(x @ w_in, IN_GROUPS)
    o_swa = swa_branch(*split_cols(p_swa, SWA_SPLITS), sinks)
    o_rwkv = rwkv7_branch(p_rwkv, mu, w0, w_up, a0, a_up, g_up, k_k, k_a, r_k, gn_g, gn_b)
    o_fox = fox_branch(*split_cols(p_fox, FOX_SPLITS), f_bias)
    o_dsa = dsa_branch(*split_cols(p_dsa, DSA_SPLITS))
    gates = jax.nn.sigmoid(p_gate.reshape(B, S, N_BRANCHES, D_MODEL) + gate_bias)
    w_rows = split_cols(w_branch, MIX_WIDTHS, axis=0)
    merged = jnp.zeros_like(x)
    for i, (o, w) in enumerate(zip((o_swa, o_rwkv, o_fox, o_dsa), w_rows)):
        merged = merged + gates[:, :, i] * (o @ w)
    return merged @ w_out


def swiglu(x, w1, w3, w2):
    return (jax.nn.silu(x @ w1) * (x @ w3)) @ w2


def moe_ffn(x, router_w, router_b, w1, w3, w2):
    logits = (x @ router_w).astype(jnp.float32) + router_b
    top_val, top_idx = lax.top_k(logits, TOP_K)
    weights = jax.nn.softmax(top_val, axis=-1)
    combine = jnp.sum(jax.nn.one_hot(top_idx, N_EXPERTS, dtype=jnp.float32) * weights[..., None], axis=-2)
    y = jnp.zeros_like(x)
    for e in range(N_EXPERTS):
        y = y + combine[..., e:e + 1].astype(x.dtype) * swiglu(x, w1[e], w3[e], w2[e])
    return y


def setup_inputs(seed: int = 0) -> dict:
    key = jax.random.key(seed)
    ks = iter(jax.random.split(key, 32))
    nrm = lambda shape, scale: scale * jax.random.normal(next(ks), shape, jnp.float32)
    unif = lambda shape, lo, hi: jax.random.uniform(next(ks), shape, jnp.float32, lo, hi)
    L = DEPTH
    return {
        'x': nrm((BATCH, SEQ, D_MODEL), 1.0),
        'w_in': nrm((L, D_MODEL, D_IN), D_MODEL ** -0.5),
        'swa_sinks': nrm((L, SWA_HEADS), 0.5),
        'rwkv_mu': unif((L, RWKV_IN), 0.0, 1.0),
        'rwkv_w0': unif((L, RWKV_W), -6.0, 1.0),
        'rwkv_w_up': nrm((L, RWKV_W_RANK, RWKV_W), 0.1),
        'rwkv_a0': nrm((L, RWKV_W), 0.1),
        'rwkv_a_up': nrm((L, RWKV_A_RANK, RWKV_W), RWKV_A_RANK ** -0.5),
        'rwkv_g_up': nrm((L, RWKV_G_RANK, RWKV_W), RWKV_G_RANK ** -0.5),
        'rwkv_k_k': 0.85 + nrm((L, RWKV_W), 0.05),
        'rwkv_k_a': 1.0 + nrm((L, RWKV_W), 0.05),
        'rwkv_r_k': nrm((L, RWKV_HEADS, HEAD_DIM), 0.1),
        'rwkv_gn_g': 1.0 + nrm((L, RWKV_W), 0.05),
        'rwkv_gn_b': nrm((L, RWKV_W), 0.02),
        'fox_f_bias': 2.0 + nrm((L, FOX_HEADS), 0.5),
        'gate_bias': nrm((L, N_BRANCHES, D_MODEL), 0.1),
        'w_branch': nrm((L, MIX_WIDTH, D_MODEL), (4 * HEAD_DIM) ** -0.5),
        'w_out': nrm((L, D_MODEL, D_MODEL), DN_BETA * D_MODEL ** -0.5),
        'ln_g': 1.0 + nrm((L, 2, D_MODEL), 0.05),
        'ln_b': nrm((L, 2, D_MODEL), 0.02),
        'ffn_w1': nrm((N_DENSE, D_MODEL, D_FF), D_MODEL ** -0.5),
        'ffn_w3': nrm((N_DENSE, D_MODEL, D_FF), D_MODEL ** -0.5),
        'ffn_w2': nrm((N_DENSE, D_FF, D_MODEL), DN_BETA * D_FF ** -0.5),
        'router_w': nrm((N_MOE, D_MODEL, N_EXPERTS), D_MODEL ** -0.5),
        'router_b': nrm((N_MOE, N_EXPERTS), 0.01),
        'exp_w1': nrm((N_MOE, N_EXPERTS, D_MODEL, D_FF_EXPERT), D_MODEL ** -0.5),
        'exp_w3': nrm((N_MOE, N_EXPERTS, D_MODEL, D_FF_EXPERT), D_MODEL ** -0.5),
        'exp_w2': nrm((N_MOE, N_EXPERTS, D_FF_EXPERT, D_MODEL), DN_BETA * D_FF_EXPERT ** -0.5),
    }


def reference(x, w_in, swa_sinks, rwkv_mu, rwkv_w0, rwkv_w_up, rwkv_a0, rwkv_a_up, rwkv_g_up,
              rwkv_k_k, rwkv_k_a, rwkv_r_k, rwkv_gn_g, rwkv_gn_b, fox_f_bias, gate_bias, w_branch,
              w_out, ln_g, ln_b, ffn_w1, ffn_w3, ffn_w2, router_w, router_b, exp_w1, exp_w3, exp_w2):
    for layer in range(DEPTH):
        y = mixer_block(x, w_in[layer], swa_sinks[layer], rwkv_mu[layer], rwkv_w0[layer],
                        rwkv_w_up[layer], rwkv_a0[layer], rwkv_a_up[layer], rwkv_g_up[layer],
                        rwkv_k_k[layer], rwkv_k_a[layer], rwkv_r_k[layer], rwkv_gn_g[layer],
                        rwkv_gn_b[layer], fox_f_bias[layer], gate_bias[layer], w_branch[layer],
                        w_out[layer])
        x = layer_norm(DN_ALPHA * x + y, ln_g[layer, 0], ln_b[layer, 0])
        j = layer // 2
        if layer % 2 == 0:
            y = swiglu(x, ffn_w1[j], ffn_w3[j], ffn_w2[j])
        else:
            y = moe_ffn(x, router_w[j], router_b[j], exp_w1[j], exp_w3[j], exp_w2[j])
        x = layer_norm(DN_ALPHA * x + y, ln_g[layer, 1], ln_b[layer, 1])
    return x
```

```python
import numpy as np
from contextlib import ExitStack
import concourse.bass as bass
import concourse.mybir as mybir
from concourse.bass_utils import run_bass_kernel_spmd

F32 = mybir.dt.float32
BF16 = mybir.dt.bfloat16
AF = mybir.ActivationFunctionType
ALU = mybir.AluOpType
AX = mybir.AxisListType

S = 2048
D = 1024
NT = 16
DEPTH = 2
D_IN = 7368
D_FF = 2816
NEXP = 8
D_FFE = 1408
ALPHA = (2 * DEPTH) ** 0.25
LN_EPS = 1e-5
GN_EPS = 64e-5
NEG = -1.0e30

ENGS = ['pe', 'act', 'dve', 'pool', 'sp']
BLK = {'pe': 'tensor', 'act': 'scalar', 'dve': 'vector', 'pool': 'gpsimd', 'sp': 'sync'}
NDSEM = 64
SAME_ENGINE_WAW = True
ARENA_BYTES = 204 * 1024


class Prog:
    def __init__(self, nc):
        self.nc = nc
        self.ops = []
        self.cnt = {e: 0 for e in ENGS}
        self.last_w = {}
        self.readers = {}
        self.known = {e: {} for e in ENGS}
        self.ndma = 0
        self.ndma_pool = 0
        self.dma_cnt = [0] * NDSEM
        self.nalloc = 0
        self.arena = None
        self.aoff = 0

    def sb(self, shape, dtype, name=None, persist=False):
        self.nalloc += 1
        if persist:
            return self.nc.alloc_sbuf_tensor(name or f"sb{self.nalloc}", list(shape), dtype)
        if self.arena is None:
            self.arena = self.nc.alloc_sbuf_tensor("arena", [128, ARENA_BYTES], mybir.dt.uint8)
            self.aoff = 0
        free = 1
        for d in shape[1:]:
            free *= d
        nb = free * (4 if dtype == F32 else 2)
        nb = (nb + 63) // 64 * 64
        assert self.aoff + nb <= ARENA_BYTES, f"arena overflow allocating {name} {shape}: {self.aoff}+{nb}"
        ap = self.arena[0:shape[0], self.aoff:self.aoff + free * (4 if dtype == F32 else 2)].bitcast(dtype)
        self.aoff += nb
        if len(shape) == 3:
            ap = ap.rearrange("p (a b) -> p a b", a=shape[1])
        elif len(shape) == 4:
            ap = ap.rearrange("p (a b c) -> p a b c", a=shape[1], b=shape[2])
        return ap

    def stage_begin(self):
        for e in ENGS:
            deps = set()
            for s in range(NDSEM):
                if self.dma_cnt[s]:
                    deps.add((('d', s), 16 * self.dma_cnt[s]))
            for o in ENGS:
                if o != e and self.cnt[o]:
                    deps.add((o, self.cnt[o]))
            waits = self._waits(e, deps)
            if waits:
                self.ops.append((e, None, waits, None, 0))
        self.aoff = 0

    def mark(self):
        return self.aoff

    def release(self, mark):
        off = mark
        self.stage_begin()
        self.aoff = off

    def ps(self, shape, dtype, name=None):
        self.nalloc += 1
        return self.nc.alloc_psum_tensor(name or f"ps{self.nalloc}", list(shape), dtype)

    def _deps(self, reads, writes, eng=None):
        deps = set()
        for k in reads:
            t = self.last_w.get(k)
            if t is not None and not (eng == 'pe' and t[0] == 'pe'):
                deps.add(t)
        for k in writes:
            t = self.last_w.get(k)
            if t is not None and (t[0] != eng or (SAME_ENGINE_WAW and eng != 'pe')):
                deps.add(t)
            for r in self.readers.get(k, ()):
                if not (eng == 'pe' and r[0] == 'pe'):
                    deps.add(r)
        return deps

    def _commit(self, tok, reads, writes):
        for k in reads:
            self.readers.setdefault(k, []).append(tok)
        for k in writes:
            self.last_w[k] = tok
            self.readers[k] = []

    def _waits(self, eng, deps):
        best = {}
        for (sk, v) in deps:
            if self.known[eng].get(sk, 0) >= v:
                continue
            if best.get(sk, 0) < v:
                best[sk] = v
        for sk, v in best.items():
            self.known[eng][sk] = v
        return list(best.items())

    def op(self, eng, fn, reads=(), writes=()):
        reads = tuple(reads)
        writes = tuple(writes)
        waits = self._waits(eng, self._deps(reads, writes, eng))
        self.cnt[eng] += 1
        tok = (eng, self.cnt[eng])
        self.ops.append((eng, fn, waits, eng, 1))
        self._commit(tok, reads, writes)
        return tok

    def dma(self, eng, out, in_, reads=(), writes=(), **kw):
        reads = tuple(reads)
        writes = tuple(writes)
        if eng == 'pool':
            slot = NDSEM // 2 + self.ndma_pool % (NDSEM // 2)
            self.ndma_pool += 1
        else:
            slot = self.ndma % (NDSEM // 2)
            self.ndma += 1
        deps = self._deps(reads, writes)
        if self.dma_cnt[slot] > 0:
            deps.add((('d', slot), 16 * self.dma_cnt[slot]))
        self.dma_cnt[slot] += 1
        tok = (('d', slot), 16 * self.dma_cnt[slot])
        waits = self._waits(eng, deps)
        self.ops.append((eng, lambda e: e.dma_start(out=out, in_=in_, **kw), waits, ('d', slot), 16))
        self._commit(tok, reads, writes)
        return tok

    def finish(self):
        deps = set()
        for s in range(NDSEM):
            if self.dma_cnt[s]:
                deps.add((('d', s), 16 * self.dma_cnt[s]))
        for e in ENGS:
            if e != 'sp' and self.cnt[e]:
                deps.add((e, self.cnt[e]))
        waits = self._waits('sp', deps)
        self.ops.append(('sp', None, waits, None, 0))

    def emit(self):
        nc = self.nc
        with ExitStack() as ctx:
            sems = {}
            for e in ENGS:
                sems[e] = ctx.enter_context(nc.semaphore(f"s_{e}"))
            for s in range(NDSEM):
                sems[('d', s)] = ctx.enter_context(nc.semaphore(f"s_d{s}"))
            block = ctx.enter_context(nc.Block())
            ops = self.ops

            def make(eng):
                def body(e):
                    for (oeng, fn, waits, isem, ival) in ops:
                        if oeng != eng:
                            continue
                        for (sk, v) in waits:
                            e.wait_ge(sems[sk], v)
                        if fn is None:
                            continue
                        fn(e).then_inc(sems[isem], ival)
                return body

            for eng in ENGS:
                getattr(block, BLK[eng])(make(eng))
        return nc


O_SWA, O_RW, O_FOX, O_DSA, O_GATE = 0, 768, 1792, 2564, 3272
H_COLS = ([O_SWA + 64 * h for h in range(8)] + [O_SWA + 512 + 64 * g for g in range(2)]
          + [O_FOX + 64 * h for h in range(4)] + [O_FOX + 256 + 64 * h for h in range(4)]
          + [O_DSA + 64 * h for h in range(4)] + [O_DSA + 256]
          + [O_DSA + 384 + 64 * h for h in range(4)] + [O_DSA + 640])
R_COLS = [O_RW + 64 * i for i in range(16)]
NSLAB = 44
T_COLS = (list(range(O_SWA + 640, O_SWA + 768)) + list(range(O_FOX + 512, O_FOX + 768))
          + list(range(O_DSA + 320, O_DSA + 384)) + list(range(O_FOX + 768, O_FOX + 772))
          + list(range(O_DSA + 704, O_DSA + 708)))
NTC = 456
HS_SWQ, HS_SWK, HS_FXQ, HS_FXK, HS_DSQ, HS_DSK, HS_DSQI, HS_DSKI = 0, 8, 10, 14, 18, 22, 23, 27


class Ctx:
    pass


def build_program(stages, dbg=(), layers=(0, 1)):
    nc = bass.Bass("TRN2", target_bir_lowering=False)
    P = Prog(nc)
    C = Ctx()
    C.nc, C.P = nc, P
    C.dbg = set(dbg)

    def ext_in(name, shape, dt=F32):
        return nc.dram_tensor(name, list(shape), dt, kind="ExternalInput").ap()

    def scratch(name, shape, dt):
        kind = "ExternalOutput" if name in C.dbg else "Internal"
        return nc.dram_tensor(name, list(shape), dt, kind=kind).ap()

    C.scratch = scratch
    C.x_in = ext_in("x", [S, D])
    C.consts = ext_in("consts", [128, 512])
    C.wA = ext_in("wA", [DEPTH, NSLAB // 2, 128, 8, 128])
    C.wT = ext_in("wT", [DEPTH, 128, 8, NTC])
    C.mu = ext_in("mu", [DEPTH, 128, 8])
    C.sinks = ext_in("sinks", [DEPTH, 8])
    C.fbias = ext_in("fbias", [DEPTH, 4])
    C.rwp = ext_in("rwp", [DEPTH, 64, 24])
    C.rwup = ext_in("rwup", [DEPTH, 64, 2, 256])
    C.rwgup = ext_in("rwgup", [DEPTH, 128, 256])
    C.rwgn = ext_in("rwgn", [DEPTH, 2, 256])
    C.cmask = ext_in("cmask", [128, 1024])
    C.wG = ext_in("wG", [DEPTH, 8, 128, 8, 4, 128])
    C.gbias = ext_in("gbias", [DEPTH, 128, 4, 8])
    C.w_branch = ext_in("w_branch", [DEPTH, 1280, D])
    C.w_out = ext_in("w_out", [DEPTH, D, D])
    C.ln_g = ext_in("ln_g", [DEPTH, 2, D])
    C.ln_b = ext_in("ln_b", [DEPTH, 2, D])
    C.w13d = ext_in("w13d", [D_FF // 128, 128, 8, 2, 128])
    C.w2d = ext_in("w2d", [D_FF // 128, 128, D])
    C.router_w = ext_in("router_w", [D, NEXP])
    C.router_b = ext_in("router_b", [NEXP])
    C.w13e = ext_in("w13e", [NEXP, D_FFE // 128, 128, 8, 2, 128])
    C.w2e = ext_in("w2e", [NEXP, D_FFE // 128, 128, D])
    C.xcur = scratch("xcur", [S, D], F32)
    C.out = nc.dram_tensor("out", [S, D], F32, kind="ExternalOutput").ap()

    C.cst = P.sb([128, 512], F32, "cst", persist=True)
    P.dma('sp', C.cst[:], C.consts, writes=['cst'])
    C.ident_bf = P.sb([128, 128], BF16, "ident_bf", persist=True)
    C.triu_bf = P.sb([128, 128], BF16, "triu_bf", persist=True)
    C.ones_bf = P.sb([128, 128], BF16, "ones_bf", persist=True)
    P.op('dve', lambda e: e.tensor_copy(out=C.ident_bf[:], in_=C.cst[:, 0:128]), reads=['cst'], writes=['ident_bf'])
    P.op('dve', lambda e: e.tensor_copy(out=C.triu_bf[:], in_=C.cst[:, 128:256]), reads=['cst'], writes=['triu_bf'])
    P.op('dve', lambda e: e.tensor_copy(out=C.ones_bf[:], in_=C.cst[:, 256:384]), reads=['cst'], writes=['ones_bf'])
    C.ident_f = C.cst[:, 0:128]
    C.triu_f = C.cst[:, 128:256]
    C.ones_f = C.cst[:, 256:384]
    C.pow2 = C.cst[:, 384:408]
    C.pb = [P.ps([128, 512], F32, f"pb{i}") for i in range(8)]

    for l in layers:
        L = Ctx()
        L.l = l
        L.xin = C.x_in if l == 0 else C.xcur
        L.headT = scratch(f"headT{l}", [28, 64, S], BF16)
        L.rwT = scratch(f"rwT{l}", [16, 64, S], F32)
        L.tokm = scratch(f"tokm{l}", [S, NTC], F32)
        L.xT = scratch(f"xT{l}", [128, 8, S], BF16)
        L.otok = scratch(f"otok{l}", [S, 1280], BF16)
        L.x1 = scratch(f"x1_{l}", [S, D], F32)
        L.xout = C.out if l == DEPTH - 1 else C.xcur
        if 'A' in stages:
            stage_A(C, L)
        if 'B' in stages:
            stage_swa(C, L)
        if 'C' in stages:
            stage_fox(C, L)
        if 'D' in stages:
            stage_dsa2(C, L)
        if 'd' in stages:
            stage_dsa(C, L)
        if 'E' in stages:
            stage_rwkv(C, L)
        if 'F' in stages:
            stage_merge(C, L)
        if 'G' in stages:
            if l % 2 == 0:
                stage_ffn_dense(C, L)
            else:
                stage_moe(C, L)
    P.finish()
    P.emit()
    return nc


def stage_A(C, L):
    C.P.stage_begin()
    P, l = C.P, L.l
    pb = C.pb
    xT = P.sb([128, 8, S], BF16, f"A_xT{l}")
    xs = [P.sb([128, D], F32, f"A_xs{l}_{i}") for i in range(2)]
    xb = [P.sb([128, D], BF16, f"A_xb{l}_{i}") for i in range(2)]
    for t in range(NT):
        i = t % 2
        P.dma('sp', xs[i][:], L.xin[t * 128:(t + 1) * 128, :], reads=['xin'], writes=[f'A_xs{i}'])
        P.op('dve', lambda e, i=i: e.tensor_copy(out=xb[i][:], in_=xs[i][:]), reads=[f'A_xs{i}'], writes=[f'A_xb{i}'])
        bank = pb[t % 2]
        pT = bank[:].bitcast(BF16)
        for kc in range(8):
            P.op('pe', lambda e, i=i, kc=kc, pT=pT: e.transpose(out=pT[:, kc * 128:(kc + 1) * 128], in_=xb[i][:, kc * 128:(kc + 1) * 128], identity=C.ident_bf[:]),
                 reads=[f'A_xb{i}', 'ident_bf'], writes=[f'pb{t % 2}'])
        P.op('act', lambda e, t=t, pT=pT: e.copy(out=xT[:, :, t * 128:(t + 1) * 128], in_=pT.rearrange("p (k c) -> p k c", k=8)),
             reads=[f'pb{t % 2}'], writes=['A_xT'])
    P.dma('act', L.xT, xT[:], reads=['A_xT'], writes=[f'xT{l}'])

    mu = P.sb([128, 8], F32, f"A_mu{l}")
    omu = P.sb([128, 8], F32, f"A_omu{l}")
    P.dma('sp', mu[:], C.mu[l], writes=['A_mu'])
    P.op('dve', lambda e: e.tensor_scalar(out=omu[:], in0=mu[:], scalar1=-1.0, scalar2=1.0, op0=ALU.mult, op1=ALU.add), reads=['A_mu'], writes=['A_omu'])

    wtf = P.sb([128, 8, NTC], F32, f"A_wtf{l}")
    wtb = P.sb([128, 8, NTC], BF16, f"A_wtb{l}")
    P.dma('sp', wtf[:], C.wT[l], writes=['A_wtf'])
    P.op('pool', lambda e: e.tensor_copy(out=wtb[:], in_=wtf[:]), reads=['A_wtf'], writes=['A_wtb'])
    tk = [P.sb([128, NTC], F32, f"A_tk{l}_{i}") for i in range(2)]
    for t in range(NT):
        b = 2 + t % 2
        for kc in range(8):
            P.op('pe', lambda e, t=t, kc=kc, b=b: e.matmul(pb[b][:, 0:NTC], lhsT=xT[:, kc, t * 128:(t + 1) * 128], rhs=wtb[:, kc, :], start=(kc == 0), stop=(kc == 7)),
                 reads=['A_xT', 'A_wtb'], writes=[f'pb{b}'])
        i = t % 2
        P.op('act', lambda e, b=b, i=i: e.copy(out=tk[i][:], in_=pb[b][:, 0:NTC]), reads=[f'pb{b}'], writes=[f'A_tk{i}'])
        P.dma('act', L.tokm[t * 128:(t + 1) * 128, :], tk[i][:], reads=[f'A_tk{i}'], writes=[f'tokm{l}'])

    wsf = [P.sb([128, 2, 8, 128], F32, f"A_wsf{l}_{i}") for i in range(2)]
    wsb = [P.sb([128, 2, 8, 128], BF16, f"A_wsb{l}_{i}") for i in range(2)]
    hsl = [P.sb([128, S], BF16, f"A_hsl{l}_{i}") for i in range(2)]
    pf = [P.sb([128, S + 1], F32, f"A_pf{l}_{i}") for i in range(2)]
    rtmp = [P.sb([128, S], F32, f"A_rtmp{l}_{i}") for i in range(2)]
    rout = [P.sb([128, S], F32, f"A_rout{l}_{i}") for i in range(2)]
    for i in range(2):
        P.op('pool', lambda e, i=i: e.memset(pf[i][:, 0:1], 0.0), writes=[f'A_pf{i}'])
    nb = 0
    NTILE = NSLAB // 2
    for g in range(NTILE // 2):
        gi = g % 2
        P.dma('sp', wsf[gi][:], C.wA[l, 2 * g:2 * g + 2].rearrange("s p k m -> p s k m"), writes=[f'A_wsf{gi}'])
        P.op('pool', lambda e, gi=gi: e.tensor_copy(out=wsb[gi][:], in_=wsf[gi][:]), reads=[f'A_wsf{gi}'], writes=[f'A_wsb{gi}'])
        for si in range(2):
            T = 2 * g + si
            i = T % 2
            for tb in range(4):
                b = 4 + nb % 4
                nb += 1
                for kc in range(8):
                    P.op('pe', lambda e, gi=gi, si=si, kc=kc, tb=tb, b=b: e.matmul(pb[b][:], lhsT=wsb[gi][:, si, kc, :], rhs=xT[:, kc, tb * 512:(tb + 1) * 512], start=(kc == 0), stop=(kc == 7)),
                         reads=['A_xT', f'A_wsb{gi}'], writes=[f'pb{b}'])
                if T < 14:
                    P.op('act', lambda e, i=i, tb=tb, b=b: e.copy(out=hsl[i][:, tb * 512:(tb + 1) * 512], in_=pb[b][:]), reads=[f'pb{b}'], writes=[f'A_hsl{i}'])
                else:
                    P.op('act', lambda e, i=i, tb=tb, b=b: e.copy(out=pf[i][:, 1 + tb * 512:1 + (tb + 1) * 512], in_=pb[b][:]), reads=[f'pb{b}'], writes=[f'A_pf{i}'])
            if T < 14:
                P.dma('act', L.headT[2 * T:2 * T + 2].rearrange("a p s -> (a p) s"), hsl[i][:], reads=[f'A_hsl{i}'], writes=[f'headT{l}'])
            else:
                r = T - 14
                P.op('dve', lambda e, i=i, r=r: e.tensor_scalar(out=rtmp[i][:], in0=pf[i][:, 0:S], scalar1=mu[:, r:r + 1], scalar2=None, op0=ALU.mult),
                     reads=[f'A_pf{i}', 'A_mu'], writes=[f'A_rtmp{i}'])
                P.op('dve', lambda e, i=i, r=r: e.scalar_tensor_tensor(out=rout[i][:], in0=pf[i][:, 1:S + 1], scalar=omu[:, r:r + 1], in1=rtmp[i][:], op0=ALU.mult, op1=ALU.add),
                     reads=[f'A_pf{i}', 'A_omu', f'A_rtmp{i}'], writes=[f'A_rout{i}'])
                P.dma('act', L.rwT[2 * r:2 * r + 2].rearrange("a p s -> (a p) s"), rout[i][:], reads=[f'A_rout{i}'], writes=[f'rwT{l}'])


def load_heads(C, L, name, s0, n):
    P = C.P
    t = P.sb([64, n, S], BF16, f"{name}{L.l}")
    P.dma('sp', t[:], L.headT[s0:s0 + n].rearrange("h p s -> p h s"), reads=[f'headT{L.l}'], writes=[name])
    return t


def load_vaug(C, L, name, c0, nh):
    P = C.P
    vf = P.sb([128, NT, nh * 64], F32, f"{name}f{L.l}")
    va = P.sb([128, NT, nh, 65], BF16, f"{name}a{L.l}")
    P.dma('sp', vf[:], L.tokm.rearrange("(t p) c -> p t c", p=128)[:, :, c0:c0 + nh * 64], reads=[f'tokm{L.l}'], writes=[name + 'f'])
    P.op('pool', lambda e: e.memset(va[:], 1.0), writes=[name])
    for h in range(nh):
        P.op('dve', lambda e, h=h: e.tensor_copy(out=va[:, :, h, 0:64], in_=vf[:, :, h * 64:(h + 1) * 64]), reads=[name + 'f', name], writes=[name])
    return va


def stage_swa(C, L):
    C.P.stage_begin()
    P, l = C.P, L.l
    pb = C.pb
    qT = load_heads(C, L, "sw_q", HS_SWQ, 8)
    kT = load_heads(C, L, "sw_k", HS_SWK, 2)
    va = load_vaug(C, L, "sw_v", 0, 2)
    esk = P.sb([128, 8], F32, f"sw_esk{l}")
    P.dma('sp', esk[:], C.sinks[l].partition_broadcast(128), writes=['sw_esk'])
    P.op('act', lambda e: e.activation(out=esk[:], in_=esk[:], func=AF.Exp), reads=['sw_esk'], writes=['sw_esk'])
    osw = P.sb([128, NT, 512], BF16, f"sw_o{l}")
    PA = [P.sb([128, 4, 128], BF16, f"sw_PA{l}_{i}") for i in range(2)]
    PB = [P.sb([128, 4, 128], BF16, f"sw_PB{l}_{i}") for i in range(2)]
    den = [P.sb([128, 4], F32, f"sw_den{l}_{i}") for i in range(2)]
    items = [(g, t) for g in range(2) for t in range(NT)]

    def qk(idx):
        g, t = items[idx]
        i = idx % 2
        bA, bB = pb[i], pb[2 + i]
        kA, kB = f'pb{i}', f'pb{2 + i}'
        rhs_q = qT[:, 4 * g:4 * g + 4, t * 128:(t + 1) * 128]
        if t > 0:
            P.op('pe', lambda e: e.matmul(bA[:].rearrange("p (h q) -> p h q", h=4), lhsT=kT[:, g, (t - 1) * 128:t * 128], rhs=rhs_q, start=True, stop=True),
                 reads=['sw_q', 'sw_k'], writes=[kA])
            P.op('act', lambda e: e.activation(out=PA[i][:], in_=bA[:].rearrange("p (h q) -> p h q", h=4), func=AF.Exp, scale=0.125), reads=[kA], writes=[f'sw_PA{i}'])
        P.op('pe', lambda e: e.matmul(bB[:].rearrange("p (h q) -> p h q", h=4), lhsT=kT[:, g, t * 128:(t + 1) * 128], rhs=rhs_q, start=True, stop=True),
             reads=['sw_q', 'sw_k'], writes=[kB])
        P.op('act', lambda e: e.activation(out=PB[i][:], in_=bB[:].rearrange("p (h q) -> p h q", h=4), func=AF.Exp, scale=0.125), reads=[kB], writes=[f'sw_PB{i}'])

    def pv(idx):
        g, t = items[idx]
        i = idx % 2
        bO, kO = pb[4 + i], f'pb{4 + i}'
        po = bO[:, 0:260].rearrange("p (h c) -> p h c", h=4)
        for hh in range(4):
            rd = ['sw_v', f'sw_PA{i}', f'sw_PB{i}']
            if t > 0:
                P.op('pe', lambda e, hh=hh: e.matmul(po[0:64, hh, :], lhsT=PA[i][:, hh, 0:64], rhs=va[:, t - 1, g, :], start=True, stop=False), reads=rd, writes=[kO])
                P.op('pe', lambda e, hh=hh: e.matmul(po[0:64, hh, :], lhsT=PB[i][0:64, hh, 0:64], rhs=va[0:64, t, g, :], start=False, stop=True), reads=rd, writes=[kO])
                P.op('pe', lambda e, hh=hh: e.matmul(po[64:128, hh, :], lhsT=PA[i][64:128, hh, 64:128], rhs=va[64:128, t - 1, g, :], start=True, stop=False), reads=rd, writes=[kO])
                P.op('pe', lambda e, hh=hh: e.matmul(po[64:128, hh, :], lhsT=PB[i][:, hh, 64:128], rhs=va[:, t, g, :], start=False, stop=True), reads=rd, writes=[kO])
            else:
                P.op('pe', lambda e, hh=hh: e.matmul(po[0:64, hh, :], lhsT=PB[i][0:64, hh, 0:64], rhs=va[0:64, t, g, :], start=True, stop=True), reads=rd, writes=[kO])
                P.op('pe', lambda e, hh=hh: e.matmul(po[64:128, hh, :], lhsT=PB[i][:, hh, 64:128], rhs=va[:, t, g, :], start=True, stop=True), reads=rd, writes=[kO])
        P.op('dve', lambda e: e.tensor_tensor(out=den[i][:], in0=po[:, :, 64], in1=esk[:, 4 * g:4 * g + 4], op=ALU.add), reads=[kO, 'sw_esk'], writes=[f'sw_den{i}'])
        P.op('dve', lambda e: e.reciprocal(out=den[i][:], in_=den[i][:]), reads=[f'sw_den{i}'], writes=[f'sw_den{i}'])
        for hh in range(4):
            P.op('dve', lambda e, hh=hh: e.tensor_scalar(out=osw[:, t, (4 * g + hh) * 64:(4 * g + hh + 1) * 64], in0=po[:, hh, 0:64], scalar1=den[i][:, hh:hh + 1], scalar2=None, op0=ALU.mult),
                 reads=[kO, f'sw_den{i}'], writes=['sw_o'])

    qk(0)
    for idx in range(len(items)):
        if idx + 1 < len(items):
            qk(idx + 1)
        pv(idx)
    P.dma('sp', L.otok.rearrange("(t p) c -> p t c", p=128)[:, :, 0:512], osw[:], reads=['sw_o'], writes=[f'otok{l}_sw'])


def stage_fox(C, L):
    C.P.stage_begin()
    P, l = C.P, L.l
    pb = C.pb
    qT = load_heads(C, L, "fx_q", HS_FXQ, 4)
    kT = load_heads(C, L, "fx_k", HS_FXK, 4)
    va = load_vaug(C, L, "fx_v", 128, 4)
    f = P.sb([128, NT, 4], F32, f"fx_f{l}")
    fb = P.sb([128, 4], F32, f"fx_fb{l}")
    P.dma('sp', f[:], L.tokm.rearrange("(t p) c -> p t c", p=128)[:, :, 448:452], reads=[f'tokm{l}'], writes=['fx_f'])
    P.dma('sp', fb[:], C.fbias[l].partition_broadcast(128), writes=['fx_fb'])
    P.op('dve', lambda e: e.tensor_tensor(out=f[:], in0=f[:], in1=fb[:].unsqueeze(1).to_broadcast([128, NT, 4]), op=ALU.add), reads=['fx_f', 'fx_fb'], writes=['fx_f'])
    P.op('act', lambda e: e.activation(out=f[:], in_=f[:], func=AF.Exp, scale=-1.0), reads=['fx_f'], writes=['fx_f'])
    P.op('act', lambda e: e.activation(out=f[:], in_=f[:], func=AF.Ln, bias=1.0), reads=['fx_f'], writes=['fx_f'])
    ff = f[:].rearrange("p t h -> p (t h)")
    P.op('pe', lambda e: e.matmul(pb[0][:, 0:64], lhsT=C.triu_f, rhs=ff, start=True, stop=True), reads=['fx_f', 'cst'], writes=['pb0'])
    P.op('pe', lambda e: e.matmul(pb[1][:, 0:64], lhsT=C.ones_f, rhs=ff, start=True, stop=True), reads=['fx_f', 'cst'], writes=['pb1'])
    tot = P.sb([128, NT, 4], F32, f"fx_tot{l}")
    pre = P.sb([128, NT, 4], F32, f"fx_pre{l}")
    cpos = P.sb([128, NT, 4], F32, f"fx_c{l}")
    P.op('dve', lambda e: e.tensor_copy(out=tot[:].rearrange("p t h -> p (t h)"), in_=pb[1][:, 0:64]), reads=['pb1'], writes=['fx_tot'])
    P.op('dve', lambda e: e.memset(pre[:, 0, :], 0.0), writes=['fx_pre'])
    for t in range(1, NT):
        P.op('dve', lambda e, t=t: e.tensor_tensor(out=pre[:, t, :], in0=pre[:, t - 1, :], in1=tot[:, t - 1, :], op=ALU.add), reads=['fx_pre', 'fx_tot'], writes=['fx_pre'])
    P.op('dve', lambda e: e.tensor_tensor(out=cpos[:].rearrange("p t h -> p (t h)"), in0=pb[0][:, 0:64], in1=pre[:].rearrange("p t h -> p (t h)"), op=ALU.add), reads=['pb0', 'fx_pre'], writes=['fx_c'])
    rrow = P.sb([1, 4, S], BF16, f"fx_rrow{l}")
    for h in range(4):
        for j in range(NT):
            P.op('dve', lambda e, h=h, j=j: e.tensor_scalar(out=rrow[0:1, h, j * 128:(j + 1) * 128], in0=C.ones_f[0:1, :], scalar1=pre[0:1, j, h:h + 1], scalar2=-8.0, op0=ALU.mult, op1=ALU.mult),
                 reads=['fx_pre', 'cst'], writes=['fx_rrow'])
    ofx = P.sb([128, NT, 256], BF16, f"fx_o{l}")
    PT = [P.sb([128, 512], BF16, f"fx_PT{l}_{i}") for i in range(2)]
    rec = P.sb([128, NT], F32, f"fx_rec{l}")
    accb = [pb[2], pb[3], pb[4]]
    acck = ['pb2', 'pb3', 'pb4']

    def acc_ap(j, lo, hi):
        return accb[j // 7][:, (j % 7) * 65 + lo:(j % 7) * 65 + hi]

    items = []
    for h in range(4):
        for kt in range(NT):
            q0 = kt * 128
            while q0 < S:
                n = min(512, S - q0)
                items.append((h, kt, q0, n))
                q0 += n

    def qk(idx):
        h, kt, q0, n = items[idx]
        i = idx % 2
        bs, ks = pb[5 + i], f'pb{5 + i}'
        P.op('pe', lambda e: e.matmul(bs[:, 0:n], lhsT=kT[:, h, kt * 128:(kt + 1) * 128], rhs=qT[:, h, q0:q0 + n], start=True, stop=False),
             reads=['fx_q', 'fx_k'], writes=[ks])
        P.op('pe', lambda e: e.matmul(bs[:, 0:n], lhsT=C.ones_bf[0:1, :], rhs=rrow[0:1, h, q0:q0 + n], start=False, stop=True),
             reads=['fx_rrow', 'ones_bf'], writes=[ks])
        P.op('act', lambda e: e.activation(out=PT[i][:, 0:n], in_=bs[:, 0:n], func=AF.Exp, scale=0.125, bias=cpos[:, kt, h:h + 1]),
             reads=[ks, 'fx_c'], writes=[f'fx_PT{i}'])
        if q0 == kt * 128:
            P.op('dve', lambda e: e.tensor_tensor(out=PT[i][:, 0:128], in0=PT[i][:, 0:128], in1=C.triu_bf[:], op=ALU.mult), reads=[f'fx_PT{i}', 'triu_bf'], writes=[f'fx_PT{i}'])

    def pv(idx):
        h, kt, q0, n = items[idx]
        i = idx % 2
        if kt == 0 and q0 == 0:
            for bi in range(3):
                P.op('dve', lambda e, bi=bi: e.memset(accb[bi][:], 0.0), writes=[acck[bi]])
        for jj in range(n // 128):
            j = q0 // 128 + jj
            P.op('pe', lambda e, jj=jj, j=j: e.matmul(acc_ap(j, 0, 65), lhsT=PT[i][:, jj * 128:(jj + 1) * 128], rhs=va[:, kt, h, :], start=False, stop=(kt == j), skip_group_check=True),
                 reads=[f'fx_PT{i}', 'fx_v'], writes=[acck[j // 7]])
        if kt == NT - 1:
            for j in range(NT):
                P.op('dve', lambda e, j=j: e.reciprocal(out=rec[:, j:j + 1], in_=acc_ap(j, 64, 65)), reads=[acck[j // 7]], writes=['fx_rec'])
                P.op('dve', lambda e, j=j: e.tensor_scalar(out=ofx[:, j, h * 64:(h + 1) * 64], in0=acc_ap(j, 0, 64), scalar1=rec[:, j:j + 1], scalar2=None, op0=ALU.mult),
                     reads=[acck[j // 7], 'fx_rec'], writes=['fx_o'])

    qk(0)
    for idx in range(len(items)):
        if idx + 1 < len(items):
            qk(idx + 1)
        pv(idx)
    P.dma('sp', L.otok.rearrange("(t p) c -> p t c", p=128)[:, :, 768:1024], ofx[:], reads=['fx_o'], writes=[f'otok{l}_fx'])


NBIS = 16


def stage_dsa(C, L):
    C.P.stage_begin()
    P, l = C.P, L.l
    pb = C.pb
    qT = load_heads(C, L, "ds_q", HS_DSQ, 4)
    kT = load_heads(C, L, "ds_k", HS_DSK, 1)
    qiT = load_heads(C, L, "ds_qi", HS_DSQI, 4)
    kiT = load_heads(C, L, "ds_ki", HS_DSKI, 1)
    va = load_vaug(C, L, "ds_v", 384, 1)
    wi = P.sb([128, NT, 4], F32, f"ds_wi{l}")
    P.dma('sp', wi[:], L.tokm.rearrange("(t p) c -> p t c", p=128)[:, :, 452:456], reads=[f'tokm{l}'], writes=['ds_wi'])
    sc = [P.sb([128, S], F32, f"ds_sc{l}_{i}") for i in range(2)]
    rl = [P.sb([128, 512], F32, f"ds_rl{l}_{i}") for i in range(4)]
    junk = P.sb([128, S], BF16, f"ds_junk{l}")
    tA = P.sb([128, S], F32, f"ds_tA{l}")
    tE = P.sb([128, S], F32, f"ds_tE{l}")
    mask = [P.sb([128, S], BF16, f"ds_mask{l}_{i}") for i in range(2)]
    maskT = [P.sb([128, NT, 128], BF16, f"ds_maskT{l}_{i}") for i in range(2)]
    PT = [P.sb([128, 4, 128], BF16, f"ds_PT{l}_{i}") for i in range(2)]
    sm = [P.sb([128, 8], F32, f"ds_sm{l}_{i}") for i in range(2)]
    wtab = [P.sb([128, NBIS], F32, f"ds_wtab{l}_{i}") for i in range(2)]
    rec = P.sb([128, 4], F32, f"ds_rec{l}")
    ods = P.sb([128, NT, 256], BF16, f"ds_o{l}")
    it = 0
    nr = 0
    if 'dsdbg' in C.dbg:
        C.dsdbg = C.scratch("dsdbg", [NT, 128, 8], F32)
    for j in range(NT):
        ji = j % 2
        n = (j + 1) * 128
        scj, ksc = sc[ji], f'ds_sc{ji}'
        for c0 in range(0, n, 512):
            nn = min(512, n - c0)
            for h in range(4):
                P.op('pe', lambda e, h=h, j=j, c0=c0, nn=nn: e.matmul(pb[h][:, 0:nn], lhsT=qiT[:, h, j * 128:(j + 1) * 128], rhs=kiT[:, 0, c0:c0 + nn], start=True, stop=True),
                     reads=['ds_qi', 'ds_ki'], writes=[f'pb{h}'])
                ri = nr % 4
                nr += 1
                P.op('act', lambda e, h=h, ri=ri, nn=nn: e.activation(out=rl[ri][:, 0:nn], in_=pb[h][:, 0:nn], func=AF.Relu), reads=[f'pb{h}'], writes=[f'ds_rl{ri}'])
                if h == 0:
                    P.op('dve', lambda e, ri=ri, nn=nn, c0=c0, j=j, scj=scj: e.tensor_scalar(out=scj[:, c0:c0 + nn], in0=rl[ri][:, 0:nn], scalar1=wi[:, j, 0:1], scalar2=None, op0=ALU.mult),
                         reads=[f'ds_rl{ri}', 'ds_wi'], writes=[ksc])
                else:
                    P.op('dve', lambda e, ri=ri, nn=nn, c0=c0, j=j, h=h, scj=scj: e.scalar_tensor_tensor(out=scj[:, c0:c0 + nn], in0=rl[ri][:, 0:nn], scalar=wi[:, j, h:h + 1], in1=scj[:, c0:c0 + nn], op0=ALU.mult, op1=ALU.add),
                         reads=[f'ds_rl{ri}', 'ds_wi', ksc], writes=[ksc])
        mk, kmk = mask[ji], f'ds_mask{ji}'
        smj, ksm = sm[ji], f'ds_sm{ji}'
        if j >= 2:
            P.op('dve', lambda e, scj=scj, smj=smj, n=n: e.tensor_reduce(out=smj[:, 0:1], in_=scj[:, 0:n - 64], axis=AX.X, op=ALU.min), reads=[ksc], writes=[ksm])
        P.op('dve', lambda e, scj=scj, n=n: e.memset(scj[0:64, n - 64:n], NEG), reads=[ksc], writes=[ksc])
        if j >= 2:
            P.op('dve', lambda e, scj=scj, smj=smj, n=n: e.tensor_reduce(out=smj[:, 5:6], in_=scj[:, 0:n], axis=AX.X, op=ALU.max), reads=[ksc], writes=[ksm])
            P.op('dve', lambda e, smj=smj: e.tensor_tensor(out=smj[:, 1:2], in0=smj[:, 5:6], in1=smj[:, 0:1], op=ALU.subtract), reads=[ksm], writes=[ksm])
            wt, kwt = wtab[ji], f'ds_wtab{ji}'
            P.op('dve', lambda e, wt=wt, smj=smj: e.tensor_scalar(out=wt[:], in0=C.pow2, scalar1=smj[:, 1:2], scalar2=None, op0=ALU.mult), reads=[ksm, 'cst'], writes=[kwt])
            for b in range(NBIS):
                P.op('dve', lambda e, smj=smj, wt=wt, b=b: e.tensor_tensor(out=smj[:, 2:3], in0=smj[:, 0:1], in1=wt[:, b:b + 1], op=ALU.add), reads=[ksm, kwt], writes=[ksm])
                P.op('dve', lambda e, smj=smj, scj=scj, n=n: e.tensor_scalar(out=junk[:, 0:n], in0=scj[:, 0:n], scalar1=smj[:, 2:3], scalar2=None, op0=ALU.is_ge, op1=ALU.add, accum_out=smj[:, 3:4]),
                     reads=[ksm, ksc], writes=[ksm, 'ds_junk'])
                P.op('dve', lambda e, smj=smj, wt=wt, b=b: e.tensor_scalar(out=smj[:, 4:5], in0=smj[:, 3:4], scalar1=255.5, scalar2=wt[:, b:b + 1], op0=ALU.is_ge, op1=ALU.mult), reads=[ksm, kwt], writes=[ksm])
                P.op('dve', lambda e, smj=smj: e.tensor_tensor(out=smj[:, 0:1], in0=smj[:, 0:1], in1=smj[:, 4:5], op=ALU.add), reads=[ksm], writes=[ksm])
            P.op('dve', lambda e, scj=scj, smj=smj, n=n: e.tensor_scalar(out=tA[:, 0:n], in0=scj[:, 0:n], scalar1=smj[:, 0:1], scalar2=1.0e37, op0=ALU.is_lt, op1=ALU.mult), reads=[ksc, ksm], writes=['ds_tA'])
            P.op('dve', lambda e, scj=scj, n=n: e.tensor_tensor(out=tA[:, 0:n], in0=tA[:, 0:n], in1=scj[:, 0:n], op=ALU.add), reads=[ksc, 'ds_tA'], writes=['ds_tA'])
            P.op('dve', lambda e, smj=smj, n=n: e.tensor_reduce(out=smj[:, 6:7], in_=tA[:, 0:n], axis=AX.X, op=ALU.min), reads=['ds_tA'], writes=[ksm])
            P.op('dve', lambda e, mk=mk, scj=scj, smj=smj, n=n: e.tensor_scalar(out=mk[:, 0:n], in0=scj[:, 0:n], scalar1=smj[:, 6:7], scalar2=None, op0=ALU.is_gt, op1=ALU.add, accum_out=smj[:, 7:8]), reads=[ksc, ksm], writes=[kmk, ksm])
            P.op('dve', lambda e, smj=smj: e.tensor_scalar(out=smj[:, 7:8], in0=smj[:, 7:8], scalar1=-1.0, scalar2=256.0, op0=ALU.mult, op1=ALU.add), reads=[ksm], writes=[ksm])
            P.op('dve', lambda e, scj=scj, smj=smj, n=n: e.tensor_scalar(out=tE[:, 0:n], in0=scj[:, 0:n], scalar1=smj[:, 6:7], scalar2=None, op0=ALU.is_equal), reads=[ksc, ksm], writes=['ds_tE'])
            P.op('dve', lambda e, n=n: e.tensor_tensor_scan(out=tA[:, 0:n], data0=C.ones_f[:, 0:1].to_broadcast([128, n]), data1=tE[:, 0:n], initial=0.0, op0=ALU.mult, op1=ALU.add), reads=['ds_tE', 'cst'], writes=['ds_tA'])
            P.op('dve', lambda e, smj=smj, n=n: e.scalar_tensor_tensor(out=tA[:, 0:n], in0=tA[:, 0:n], scalar=smj[:, 7:8], in1=tE[:, 0:n], op0=ALU.is_le, op1=ALU.mult), reads=['ds_tA', 'ds_tE', ksm], writes=['ds_tA'])
            P.op('dve', lambda e, mk=mk, n=n: e.tensor_tensor(out=mk[:, 0:n], in0=mk[:, 0:n], in1=tA[:, 0:n], op=ALU.add), reads=[kmk, 'ds_tA'], writes=[kmk])
        else:
            P.op('dve', lambda e, mk=mk, scj=scj, n=n: e.tensor_scalar(out=mk[:, 0:n], in0=scj[:, 0:n], scalar1=-1.0e29, scalar2=None, op0=ALU.is_ge), reads=[ksc], writes=[kmk])
        if 'dsdbg' in C.dbg:
            P.dma('sp', C.dsdbg[j], smj[:], reads=[ksm], writes=['dsdbg'])
        mT, kmT = maskT[ji], f'ds_maskT{ji}'
        for k0 in range(0, j + 1, 8):
            k1 = min(j + 1, k0 + 8)
            pT = pb[4][:].bitcast(BF16)
            for kt in range(k0, k1):
                P.op('pe', lambda e, pT=pT, mk=mk, kt=kt, k0=k0: e.transpose(out=pT[:, (kt - k0) * 128:(kt - k0 + 1) * 128], in_=mk[:, kt * 128:(kt + 1) * 128], identity=C.ident_bf[:]),
                     reads=[kmk, 'ident_bf'], writes=['pb4'])
            P.op('act', lambda e, pT=pT, mT=mT, k0=k0, k1=k1: e.copy(out=mT[:, k0:k1, :], in_=pT[:, 0:(k1 - k0) * 128].rearrange("p (k c) -> p k c", c=128)), reads=['pb4'], writes=[kmT])
        po = pb[7][:, 0:260].rearrange("p (h c) -> p h c", h=4)
        P.op('dve', lambda e: e.memset(pb[7][:, 0:260], 0.0), writes=['pb7'])
        for kt in range(j + 1):
            i = it % 2
            it += 1
            bs, ks = pb[5 + i], f'pb{5 + i}'
            P.op('pe', lambda e, bs=bs, kt=kt, j=j: e.matmul(bs[:].rearrange("p (h q) -> p h q", h=4), lhsT=kT[:, 0, kt * 128:(kt + 1) * 128], rhs=qT[:, :, j * 128:(j + 1) * 128], start=True, stop=True),
                 reads=['ds_q', 'ds_k'], writes=[ks])
            P.op('act', lambda e, bs=bs, i=i: e.activation(out=PT[i][:], in_=bs[:].rearrange("p (h q) -> p h q", h=4), func=AF.Exp, scale=0.125), reads=[ks], writes=[f'ds_PT{i}'])
            P.op('dve', lambda e, i=i, mT=mT, kt=kt: e.tensor_tensor(out=PT[i][:], in0=PT[i][:], in1=mT[:, kt, :].unsqueeze(1).to_broadcast([128, 4, 128]), op=ALU.mult), reads=[f'ds_PT{i}', kmT], writes=[f'ds_PT{i}'])
            for h in range(4):
                P.op('pe', lambda e, po=po, i=i, h=h, kt=kt, j=j: e.matmul(po[:, h, :], lhsT=PT[i][:, h, :], rhs=va[:, kt, 0, :], start=False, stop=(kt == j), skip_group_check=True),
                     reads=[f'ds_PT{i}', 'ds_v'], writes=['pb7'])
        P.op('dve', lambda e, po=po: e.reciprocal(out=rec[:], in_=po[:, :, 64]), reads=['pb7'], writes=['ds_rec'])
        for h in range(4):
            P.op('dve', lambda e, po=po, h=h, j=j: e.tensor_scalar(out=ods[:, j, h * 64:(h + 1) * 64], in0=po[:, h, 0:64], scalar1=rec[:, h:h + 1], scalar2=None, op0=ALU.mult), reads=['pb7', 'ds_rec'], writes=['ds_o'])
    P.dma('sp', L.otok.rearrange("(t p) c -> p t c", p=128)[:, :, 1024:1280], ods[:], reads=['ds_o'], writes=[f'otok{l}_ds'])


def make_consts():
    c = np.zeros((128, 512), np.float32)
    c[:, 0:128] = np.eye(128, dtype=np.float32)
    c[:, 128:256] = np.triu(np.ones((128, 128), np.float32))
    c[:, 256:384] = 1.0
    c[:, 384:408] = (0.5 ** np.arange(1, 25, dtype=np.float64)).astype(np.float32)[None, :]
    return c


def prep_shared(inp):
    w_in = np.asarray(inp['w_in'])
    sh = {}
    wA = np.empty((DEPTH, NSLAB // 2, 128, 8, 128), np.float32)
    wT = np.empty((DEPTH, 128, 8, NTC), np.float32)
    cols = H_COLS + R_COLS
    for l in range(DEPTH):
        w = w_in[l].reshape(8, 128, D_IN)
        for s, c0 in enumerate(cols):
            wA[l, s // 2, :, :, (s % 2) * 64:(s % 2) * 64 + 64] = w[:, :, c0:c0 + 64].transpose(1, 0, 2)
        wT[l] = w[:, :, T_COLS].transpose(1, 0, 2)
    sh['wA'] = wA
    sh['wT'] = wT
    sh['mu'] = np.ascontiguousarray(np.asarray(inp['rwkv_mu']).reshape(DEPTH, 8, 128).transpose(0, 2, 1))
    sh['sinks'] = np.asarray(inp['swa_sinks'])
    sh['fbias'] = np.asarray(inp['fox_f_bias'])
    sh['consts'] = make_consts()
    rwp = np.zeros((DEPTH, 64, 24), np.float32)
    for i, nm in enumerate(['rwkv_w0', 'rwkv_a0', 'rwkv_k_k', 'rwkv_k_a']):
        rwp[:, :, 4 * i:4 * i + 4] = np.asarray(inp[nm]).reshape(DEPTH, 4, 64).transpose(0, 2, 1)
    rwp[:, :, 16:20] = np.asarray(inp['rwkv_r_k']).transpose(0, 2, 1)
    sh['rwp'] = rwp
    sh['rwup'] = np.ascontiguousarray(np.stack([np.asarray(inp['rwkv_w_up']), np.asarray(inp['rwkv_a_up'])], axis=2))
    sh['rwgup'] = np.asarray(inp['rwkv_g_up'])
    sh['rwgn'] = np.ascontiguousarray(np.stack([np.asarray(inp['rwkv_gn_g']), np.asarray(inp['rwkv_gn_b'])], axis=1))
    cm = np.ones((128, 1024), np.float32)
    cm[:, ::64] = 0.0
    sh['cmask'] = cm
    wG = np.empty((DEPTH, 8, 128, 8, 4, 128), np.float32)
    for l in range(DEPTH):
        g = w_in[l][:, O_GATE:].reshape(8, 128, 4, 8, 128)
        wG[l] = g.transpose(3, 1, 0, 2, 4)
    sh['wG'] = wG
    sh['gbias'] = np.ascontiguousarray(np.asarray(inp['gate_bias']).reshape(DEPTH, 4, 8, 128).transpose(0, 3, 1, 2))
    for nm in ['w_branch', 'w_out', 'ln_g', 'ln_b']:
        sh[nm] = np.asarray(inp[nm])
    nf = D_FF // 128
    w1 = np.asarray(inp['ffn_w1'])[0].reshape(8, 128, nf, 128)
    w3 = np.asarray(inp['ffn_w3'])[0].reshape(8, 128, nf, 128)
    sh['w13d'] = np.ascontiguousarray(np.stack([w1, w3], axis=3).transpose(2, 1, 0, 3, 4))
    sh['w2d'] = np.asarray(inp['ffn_w2'])[0].reshape(nf, 128, D)
    sh['router_w'] = np.asarray(inp['router_w'])[0]
    sh['router_b'] = np.asarray(inp['router_b'])[0]
    nfe = D_FFE // 128
    e1 = np.asarray(inp['exp_w1'])[0].reshape(NEXP, 8, 128, nfe, 128)
    e3 = np.asarray(inp['exp_w3'])[0].reshape(NEXP, 8, 128, nfe, 128)
    sh['w13e'] = np.ascontiguousarray(np.stack([e1, e3], axis=4).transpose(0, 3, 2, 1, 4, 5))
    sh['w2e'] = np.asarray(inp['exp_w2'])[0].reshape(NEXP, nfe, 128, D)
    return sh


C0 = float(np.exp(-0.5))
HS2 = 1024
NCH = 16
RW_SCAN_CHUNKS = NCH


def stage_rwkv(C, L):
    P, l = C.P, L.l
    P.stage_begin()
    pb = C.pb
    rwp = P.sb([64, 24], F32, f"rw_p{l}")
    P.dma('sp', rwp[:], C.rwp[l], writes=['rw_p'])
    P.op('dve', lambda e: e.tensor_scalar(out=rwp[:, 20:24], in0=rwp[:, 12:16], scalar1=-1.0, scalar2=1.0, op0=ALU.mult, op1=ALU.add), reads=['rw_p'], writes=['rw_p'])
    upf = P.sb([64, 2, 256], F32, f"rw_upf{l}")
    upb = P.sb([64, 2, 256], BF16, f"rw_upb{l}")
    P.dma('sp', upf[:], C.rwup[l], writes=['rw_upf'])
    P.op('dve', lambda e: e.tensor_copy(out=upb[:], in_=upf[:]), reads=['rw_upf'], writes=['rw_upb'])
    guf = P.sb([128, 256], F32, f"rw_guf{l}")
    gub = P.sb([128, 256], BF16, f"rw_gub{l}")
    P.dma('sp', guf[:], C.rwgup[l], writes=['rw_guf'])
    P.op('dve', lambda e: e.tensor_copy(out=gub[:], in_=guf[:]), reads=['rw_guf'], writes=['rw_gub'])
    gng = P.sb([64, 256], F32, f"rw_gng{l}")
    gnb = P.sb([64, 256], F32, f"rw_gnb{l}")
    P.dma('sp', gng[:], C.rwgn[l, 0].partition_broadcast(64), writes=['rw_gng'])
    P.dma('sp', gnb[:], C.rwgn[l, 1].partition_broadcast(64), writes=['rw_gnb'])
    eps12 = P.sb([64, 2], F32, f"rw_eps{l}")
    P.op('dve', lambda e: e.memset(eps12[:], 1e-12), writes=['rw_eps'])
    cm = P.sb([64, HS2], F32, f"rw_cm{l}")
    P.dma('sp', cm[:], C.cmask[0:64, :], writes=['rw_cm'])
    sup = P.sb([64, 64], F32, f"rw_sup{l}")
    slo = P.sb([64, 64], F32, f"rw_slo{l}")
    P.op('dve', lambda e: e.tensor_tensor(out=sup[:], in0=C.triu_f[0:64, 0:64], in1=C.ident_f[0:64, 0:64], op=ALU.subtract), reads=['cst'], writes=['rw_sup'])
    P.op('dve', lambda e: e.tensor_scalar(out=slo[:], in0=C.triu_f[0:64, 0:64], scalar1=-1.0, scalar2=1.0, op0=ALU.mult, op1=ALU.add), reads=['cst'], writes=['rw_slo'])

    AT = P.sb([64, 4, HS2], BF16, f"rw_AT{l}")
    RT = P.sb([64, 4, HS2], BF16, f"rw_RT{l}")
    BT = P.sb([64, 4, HS2], BF16, f"rw_BT{l}")
    KT = P.sb([64, 4, HS2], BF16, f"rw_KT{l}")
    Vt = P.sb([64, 4, NCH, 64], BF16, f"rw_Vt{l}")
    Bh = P.sb([64, 4, NCH, 64], BF16, f"rw_Bh{l}")
    Kh = P.sb([64, 4, NCH, 64], BF16, f"rw_Kh{l}")
    TT = P.sb([64, 4, NCH, 64], BF16, f"rw_TT{l}")
    AakT = P.sb([64, 4, NCH, 64], BF16, f"rw_Aak{l}")
    ArbT = P.sb([64, 4, NCH, 64], BF16, f"rw_Arb{l}")
    ArkT = P.sb([64, 4, NCH, 64], BF16, f"rw_Ark{l}")
    WC = P.sb([64, 4, NCH], F32, f"rw_WC{l}")
    rkb = P.sb([64, NCH, 4], F32, f"rw_rkb{l}")
    ST = P.sb([64, 4, 64], F32, f"rw_ST{l}")
    STb = P.sb([64, 4, 64], BF16, f"rw_STb{l}")
    STw = P.sb([64, 4, 64], F32, f"rw_STw{l}")
    P.op('dve', lambda e: e.memset(ST[:], 0.0), writes=['rw_ST'])
    P.op('dve', lambda e: e.memset(STb[:], 0.0), writes=['rw_STb'])
    wlo = P.sb([64, HS2], F32, f"rw_wlo{l}")
    alo = P.sb([64, HS2], F32, f"rw_alo{l}")
    glo = P.sb([128, HS2], F32, f"rw_glo{l}")
    tw = P.sb([64, HS2], BF16, f"rw_tw{l}")
    alb = P.sb([64, HS2], BF16, f"rw_alb{l}")
    sg = P.sb([128, HS2], BF16, f"rw_sg{l}")
    tr = P.sb([64, HS2], F32, f"rw_tr{l}")
    tk = P.sb([64, HS2], F32, f"rw_tk{l}")
    tv = P.sb([64, HS2], F32, f"rw_tv{l}")
    cs = P.sb([64, HS2], F32, f"rw_cs{l}")
    sig = P.sb([64, HS2], F32, f"rw_sig{l}")
    ex = P.sb([64, HS2], F32, f"rw_ex{l}")
    ta = P.sb([64, HS2], F32, f"rw_ta{l}")
    kk = P.sb([64, HS2], F32, f"rw_kk{l}")
    kp = P.sb([64, HS2], F32, f"rw_kp{l}")
    tb = P.sb([64, HS2], F32, f"rw_tb{l}")
    t1 = P.sb([64, HS2], F32, f"rw_t1{l}")
    t2b = P.sb([64, HS2], BF16, f"rw_t2b{l}")
    Nb = [[P.sb([64, 8, 64], BF16, f"rw_Nb{l}_{g}{i}") for i in range(2)] for g in range(2)]
    Mb = [[P.sb([64, 8, 64], BF16, f"rw_Mb{l}_{g}{i}") for i in range(2)] for g in range(2)]
    Pb = [[P.sb([64, 8, 64], BF16, f"rw_Pb{l}_{g}{i}") for i in range(2)] for g in range(2)]
    Upb = P.sb([64, 4, 64], BF16, f"rw_Upb{l}")
    Ub = P.sb([64, 4, 64], BF16, f"rw_Ub{l}")
    Y = [P.sb([64, 4, 64], F32, f"rw_Y{l}_{i}") for i in range(2)]
    yc = P.sb([64, 4, 64], F32, f"rw_yc{l}")
    ysq = P.sb([64, 4, 64], F32, f"rw_ysq{l}")
    st4 = P.sb([64, 8], F32, f"rw_st4{l}")
    gt = P.sb([64, 256], F32, f"rw_gt{l}")
    ob = [P.sb([64, 256], BF16, f"rw_ob{l}_{i}") for i in range(2)]

    def bc3(ap2, n1, n2):
        return ap2.unsqueeze(2).to_broadcast([64, n1, n2])

    def mbc(m):
        return m.unsqueeze(1).to_broadcast([64, 8, 64])

    for half in range(2):
        t0 = half * HS2
        P.dma('sp', wlo[:], L.rwT[12, :, t0:t0 + HS2], reads=[f'rwT{l}'], writes=['rw_wlo'])
        P.dma('sp', alo[:], L.rwT[13, :, t0:t0 + HS2], reads=[f'rwT{l}'], writes=['rw_alo'])
        P.dma('sp', glo[:], L.rwT[14:16, :, t0:t0 + HS2].rearrange("a p s -> (a p) s"), reads=[f'rwT{l}'], writes=['rw_glo'])
        P.op('act', lambda e: e.activation(out=tw[:], in_=wlo[:], func=AF.Tanh), reads=['rw_wlo'], writes=['rw_tw'])
        P.op('act', lambda e: e.copy(out=alb[:], in_=alo[:]), reads=['rw_alo'], writes=['rw_alb'])
        P.op('act', lambda e: e.activation(out=sg[:], in_=glo[:], func=AF.Sigmoid), reads=['rw_glo'], writes=['rw_sg'])
        for h in range(4):
            P.dma('sp', tr[:], L.rwT[h, :, t0:t0 + HS2], reads=[f'rwT{l}'], writes=['rw_tr'])
            P.dma('sp', tk[:], L.rwT[4 + h, :, t0:t0 + HS2], reads=[f'rwT{l}'], writes=['rw_tk'])
            P.dma('sp', tv[:], L.rwT[8 + h, :, t0:t0 + HS2], reads=[f'rwT{l}'], writes=['rw_tv'])
            for blk in range(2):
                P.op('pe', lambda e, h=h, blk=blk: e.matmul(pb[blk][0:64, :], lhsT=upb[:, 0, h * 64:(h + 1) * 64], rhs=tw[:, blk * 512:(blk + 1) * 512], start=True, stop=True), reads=['rw_upb', 'rw_tw'], writes=[f'pb{blk}'])
                P.op('act', lambda e, h=h, blk=blk: e.activation(out=sig[:, blk * 512:(blk + 1) * 512], in_=pb[blk][0:64, :], func=AF.Sigmoid, bias=rwp[:, h:h + 1]), reads=[f'pb{blk}', 'rw_p'], writes=['rw_sig'])
            P.op('dve', lambda e: e.tensor_tensor_scan(out=cs[:], data0=cm[:], data1=sig[:], initial=0.0, op0=ALU.mult, op1=ALU.add), reads=['rw_cm', 'rw_sig'], writes=['rw_cs'])
            for blk in range(2):
                P.op('pe', lambda e, h=h, blk=blk: e.matmul(pb[2 + blk][0:64, :], lhsT=upb[:, 1, h * 64:(h + 1) * 64], rhs=alb[:, blk * 512:(blk + 1) * 512], start=True, stop=True), reads=['rw_upb', 'rw_alb'], writes=[f'pb{2 + blk}'])
                P.op('act', lambda e, h=h, blk=blk: e.activation(out=ta[:, blk * 512:(blk + 1) * 512], in_=pb[2 + blk][0:64, :], func=AF.Sigmoid, bias=rwp[:, 4 + h:5 + h]), reads=[f'pb{2 + blk}', 'rw_p'], writes=['rw_ta'])
            P.op('act', lambda e, h=h: e.activation(out=kk[:], in_=tk[:], func=AF.Copy, scale=rwp[:, 8 + h:9 + h]), reads=['rw_tk', 'rw_p'], writes=['rw_kk'])
            P.op('act', lambda e: e.activation(out=t1[:], in_=kk[:], func=AF.Square), reads=['rw_kk'], writes=['rw_t1'])
            for blk in range(2):
                P.op('pe', lambda e, blk=blk: e.matmul(pb[4 + blk][0:64, :], lhsT=C.ones_f[0:64, 0:64], rhs=t1[:, blk * 512:(blk + 1) * 512], start=True, stop=True), reads=['cst', 'rw_t1'], writes=[f'pb{4 + blk}'])
                P.op('act', lambda e, blk=blk: e.activation(out=tb[:, blk * 512:(blk + 1) * 512], in_=pb[4 + blk][0:64, :], func=AF.Sqrt, bias=eps12[:, 0:1]), reads=[f'pb{4 + blk}', 'rw_eps'], writes=['rw_tb'])
            P.op('dve', lambda e: e.reciprocal(out=tb[:], in_=tb[:]), reads=['rw_tb'], writes=['rw_tb'])
            P.op('dve', lambda e: e.tensor_tensor(out=kk[:], in0=kk[:], in1=tb[:], op=ALU.mult), reads=['rw_kk', 'rw_tb'], writes=['rw_kk'])
            P.op('dve', lambda e, h=h: e.tensor_scalar(out=kp[:], in0=ta[:], scalar1=rwp[:, 12 + h:13 + h], scalar2=rwp[:, 20 + h:21 + h], op0=ALU.mult, op1=ALU.add), reads=['rw_ta', 'rw_p'], writes=['rw_kp'])
            P.op('dve', lambda e: e.tensor_tensor(out=kp[:], in0=kp[:], in1=tk[:], op=ALU.mult), reads=['rw_kp', 'rw_tk'], writes=['rw_kp'])
            P.op('dve', lambda e: e.tensor_tensor(out=tb[:], in0=kk[:], in1=ta[:], op=ALU.mult), reads=['rw_kk', 'rw_ta'], writes=['rw_tb'])
            P.op('dve', lambda e, h=h: e.scalar_tensor_tensor(out=t1[:], in0=tr[:], scalar=rwp[:, 16 + h:17 + h], in1=kp[:], op0=ALU.mult, op1=ALU.mult), reads=['rw_tr', 'rw_kp', 'rw_p'], writes=['rw_t1'])
            for c in range(NCH):
                P.op('pe', lambda e, c=c, h=h: e.matmul(pb[6][0:64, c * 4 + h:c * 4 + h + 1], lhsT=t1[:, c * 64:(c + 1) * 64], rhs=C.ones_f[0:64, 0:1], start=True, stop=True), reads=['rw_t1', 'cst'], writes=['pb6'])
            P.op('act', lambda e: e.activation(out=ex[:], in_=cs[:], func=AF.Exp, scale=-C0), reads=['rw_cs'], writes=['rw_ex'])
            P.op('dve', lambda e, h=h: e.tensor_tensor(out=RT[:, h, :], in0=tr[:], in1=ex[:], op=ALU.mult), reads=['rw_tr', 'rw_ex'], writes=['rw_RT'])
            P.op('dve', lambda e: e.tensor_tensor(out=t1[:], in0=cs[:], in1=sig[:], op=ALU.subtract), reads=['rw_cs', 'rw_sig', 'pb6' if False else 'rw_t1'], writes=['rw_t1'])
            P.op('act', lambda e: e.activation(out=ex[:], in_=t1[:], func=AF.Exp, scale=-C0), reads=['rw_t1', 'rw_RT'], writes=['rw_ex'])
            P.op('dve', lambda e, h=h: e.scalar_tensor_tensor(out=AT[:, h, :], in0=kk[:], scalar=-1.0, in1=ex[:], op0=ALU.mult, op1=ALU.mult), reads=['rw_kk', 'rw_ex'], writes=['rw_AT'])
            P.op('act', lambda e: e.activation(out=ex[:], in_=cs[:], func=AF.Exp, scale=C0), reads=['rw_cs', 'rw_AT'], writes=['rw_ex'])
            P.op('dve', lambda e, h=h: e.tensor_tensor(out=BT[:, h, :], in0=tb[:], in1=ex[:], op=ALU.mult), reads=['rw_tb', 'rw_ex'], writes=['rw_BT'])
            P.op('dve', lambda e, h=h: e.tensor_tensor(out=KT[:, h, :], in0=kp[:], in1=ex[:], op=ALU.mult), reads=['rw_kp', 'rw_ex'], writes=['rw_KT'])
            cs3 = cs[:].rearrange("p (c t) -> p c t", t=64)
            P.op('dve', lambda e, cs3=cs3: e.tensor_tensor(out=t1[:].rearrange("p (c t) -> p c t", t=64), in0=bc3(cs3[:, :, 63], NCH, 64), in1=cs3, op=ALU.subtract), reads=['rw_cs'], writes=['rw_t1'])
            P.op('act', lambda e: e.activation(out=ex[:], in_=t1[:], func=AF.Exp, scale=-C0), reads=['rw_t1', 'rw_BT', 'rw_KT'], writes=['rw_ex'])
            P.op('act', lambda e, h=h, cs3=cs3: e.activation(out=WC[:, h, :], in_=cs3[:, :, 63], func=AF.Exp, scale=-C0), reads=['rw_cs'], writes=['rw_WC'])
            pT = pb[7][:].bitcast(BF16)
            for (src, dst, nm) in ((tb, Bh, 'rw_Bh'), (kp, Kh, 'rw_Kh'), (tv, Vt, 'rw_Vt')):
                if nm == 'rw_Vt':
                    P.op('dve', lambda e, src=src: e.tensor_copy(out=t2b[:], in_=src[:]), reads=['rw_tv', 'pb7'], writes=['rw_t2b'])
                else:
                    P.op('dve', lambda e, src=src: e.tensor_tensor(out=t2b[:], in0=src[:], in1=ex[:], op=ALU.mult), reads=['rw_tb', 'rw_kp', 'rw_ex', 'pb7'], writes=['rw_t2b'])
                for c in range(NCH):
                    P.op('pe', lambda e, c=c: e.transpose(out=pT[0:64, c * 64:(c + 1) * 64], in_=t2b[:, c * 64:(c + 1) * 64], identity=C.ident_bf[0:64, 0:64]), reads=['rw_t2b', 'ident_bf'], writes=['pb7'])
                P.op('act', lambda e, dst=dst, h=h: e.copy(out=dst[:, h, :, :], in_=pT[0:64, :].rearrange("p (c k) -> p c k", k=64)), reads=['pb7'], writes=[nm])
            v3 = lambda bank: bank[0:64, :].rearrange("p (c k) -> p c k", k=64)
            for g in range(2):
                cs_ = range(g * 8, g * 8 + 8)
                for (bank, kb, lt, rt_, msk, dst, nm) in ((pb[3], 'pb3', KT, AT, sup, AakT, 'rw_Aak'), (pb[4], 'pb4', BT, RT, C.triu_f[0:64, 0:64], ArbT, 'rw_Arb'), (pb[5], 'pb5', KT, RT, C.triu_f[0:64, 0:64], ArkT, 'rw_Ark')):
                    for ci, c in enumerate(cs_):
                        P.op('pe', lambda e, bank=bank, lt=lt, rt_=rt_, ci=ci, c=c, h=h: e.matmul(bank[0:64, ci * 64:(ci + 1) * 64], lhsT=lt[:, h, c * 64:(c + 1) * 64], rhs=rt_[:, h, c * 64:(c + 1) * 64], start=True, stop=True),
                             reads=['rw_AT', 'rw_RT', 'rw_BT', 'rw_KT'], writes=[kb])
                    P.op('dve', lambda e, bank=bank, msk=msk, dst=dst, h=h, g=g: e.tensor_tensor(out=dst[:, h, g * 8:g * 8 + 8, :], in0=v3(bank), in1=mbc(msk), op=ALU.mult), reads=[kb, 'rw_sup', 'cst'], writes=[nm])
            BK = {0: (0, 1, 2), 1: (3, 4, 5)}
            for g in range(2):
                bn, bm, _ = BK[g]
                for ci in range(8):
                    c = g * 8 + ci
                    P.op('pe', lambda e, ci=ci, c=c, h=h, bn=bn: e.matmul(pb[bn][0:64, ci * 64:(ci + 1) * 64], lhsT=BT[:, h, c * 64:(c + 1) * 64], rhs=AT[:, h, c * 64:(c + 1) * 64], start=True, stop=True), reads=['rw_AT', 'rw_BT'], writes=[f'pb{bn}'])
                    P.op('pe', lambda e, ci=ci, c=c, h=h, bm=bm: e.matmul(pb[bm][0:64, ci * 64:(ci + 1) * 64], lhsT=AT[:, h, c * 64:(c + 1) * 64], rhs=BT[:, h, c * 64:(c + 1) * 64], start=True, stop=True), reads=['rw_AT', 'rw_BT'], writes=[f'pb{bm}'])
                P.op('dve', lambda e, g=g, bn=bn: e.tensor_tensor(out=Nb[g][0][:], in0=v3(pb[bn]), in1=mbc(sup), op=ALU.mult), reads=[f'pb{bn}', 'rw_sup'], writes=[f'rw_Nb{g}0'])
                P.op('dve', lambda e, g=g, bm=bm: e.tensor_tensor(out=Mb[g][0][:], in0=v3(pb[bm]), in1=mbc(slo), op=ALU.mult), reads=[f'pb{bm}', 'rw_slo'], writes=[f'rw_Mb{g}0'])
                P.op('dve', lambda e, g=g: e.tensor_tensor(out=Pb[g][0][:], in0=Nb[g][0][:], in1=mbc(C.ident_f[0:64, 0:64]), op=ALU.add), reads=[f'rw_Nb{g}0', 'cst'], writes=[f'rw_Pb{g}0'])
            for i in range(1, 6):
                a, b_ = (i - 1) % 2, i % 2
                for g in range(2):
                    bn, bm, bp = BK[g]
                    for ci in range(8):
                        if i < 5:
                            P.op('pe', lambda e, ci=ci, a=a, g=g, bn=bn: e.matmul(pb[bn][0:64, ci * 64:(ci + 1) * 64], lhsT=Mb[g][a][:, ci, :], rhs=Nb[g][a][:, ci, :], start=True, stop=True), reads=[f'rw_Nb{g}{a}', f'rw_Mb{g}{a}'], writes=[f'pb{bn}'])
                        P.op('pe', lambda e, ci=ci, a=a, g=g, bm=bm: e.matmul(pb[bm][0:64, ci * 64:(ci + 1) * 64], lhsT=Nb[g][a][:, ci, :], rhs=Mb[g][a][:, ci, :], start=True, stop=True), reads=[f'rw_Nb{g}{a}', f'rw_Mb{g}{a}'], writes=[f'pb{bm}'])
                    if i < 5:
                        P.op('act', lambda e, b_=b_, g=g, bn=bn: e.copy(out=Nb[g][b_][:], in_=v3(pb[bn])), reads=[f'pb{bn}'], writes=[f'rw_Nb{g}{b_}'])
                    P.op('act', lambda e, b_=b_, g=g, bm=bm: e.copy(out=Mb[g][b_][:], in_=v3(pb[bm])), reads=[f'pb{bm}'], writes=[f'rw_Mb{g}{b_}'])
                for g in range(2):
                    bn, bm, bp = BK[g]
                    for ci in range(8):
                        P.op('pe', lambda e, ci=ci, a=a, b_=b_, g=g, bp=bp: e.matmul(pb[bp][0:64, ci * 64:(ci + 1) * 64], lhsT=Mb[g][b_][:, ci, :], rhs=Pb[g][a][:, ci, :], start=True, stop=True), reads=[f'rw_Mb{g}{b_}', f'rw_Pb{g}{a}'], writes=[f'pb{bp}'])
                    if i < 5:
                        P.op('dve', lambda e, a=a, b_=b_, g=g, bp=bp: e.tensor_tensor(out=Pb[g][b_][:], in0=v3(pb[bp]), in1=Pb[g][a][:], op=ALU.add), reads=[f'pb{bp}', f'rw_Pb{g}{a}'], writes=[f'rw_Pb{g}{b_}'])
                    else:
                        P.op('dve', lambda e, a=a, h=h, g=g, bp=bp: e.tensor_tensor(out=TT[:, h, g * 8:g * 8 + 8, :], in0=v3(pb[bp]), in1=Pb[g][a][:], op=ALU.add), reads=[f'pb{bp}', f'rw_Pb{g}{a}'], writes=['rw_TT'])
            P.op('dve', lambda e, h=h: e.tensor_copy(out=rkb[:, :, h], in_=pb[6][0:64, 0:64].rearrange("p (c h) -> p c h", h=4)[:, :, h]), reads=['pb6'], writes=['rw_rkb'])

        P.op('dve', lambda e: e.tensor_tensor(out=STw[:], in0=ST[:], in1=bc3(WC[:, :, 0], 4, 64), op=ALU.mult), reads=['rw_ST', 'rw_WC'], writes=['rw_STw'])
        for c in range(RW_SCAN_CHUNKS):
            sl = slice(c * 64, (c + 1) * 64)
            pU, pY, pS = pb[0][0:64, 0:256].rearrange("p (h v) -> p h v", h=4), pb[1][0:64, 0:256].rearrange("p (h v) -> p h v", h=4), pb[2][0:64, 0:256].rearrange("p (h v) -> p h v", h=4)
            pU2 = pb[3][0:64, 0:256].rearrange("p (h v) -> p h v", h=4)
            for h in range(4):
                P.op('pe', lambda e, h=h, sl=sl, pU=pU: e.matmul(pU[:, h, :], lhsT=AT[:, h, sl], rhs=STb[:, h, :], start=True, stop=False), reads=['rw_AT', 'rw_STb'], writes=['pb0'])
                P.op('pe', lambda e, h=h, c=c, pU=pU: e.matmul(pU[:, h, :], lhsT=AakT[:, h, c, :], rhs=Vt[:, h, c, :], start=False, stop=True), reads=['rw_Aak', 'rw_Vt'], writes=['pb0'])
            P.op('act', lambda e, pU=pU: e.copy(out=Upb[:], in_=pU), reads=['pb0'], writes=['rw_Upb'])
            for h in range(4):
                P.op('pe', lambda e, h=h, c=c, pU2=pU2: e.matmul(pU2[:, h, :], lhsT=TT[:, h, c, :], rhs=Upb[:, h, :], start=True, stop=True), reads=['rw_TT', 'rw_Upb'], writes=['pb3'])
            P.op('act', lambda e, pU2=pU2: e.copy(out=Ub[:], in_=pU2), reads=['pb3'], writes=['rw_Ub'])
            for h in range(4):
                P.op('pe', lambda e, h=h, sl=sl, pY=pY: e.matmul(pY[:, h, :], lhsT=RT[:, h, sl], rhs=STb[:, h, :], start=True, stop=False), reads=['rw_RT', 'rw_STb'], writes=['pb1'])
                P.op('pe', lambda e, h=h, c=c, pY=pY: e.matmul(pY[:, h, :], lhsT=ArbT[:, h, c, :], rhs=Ub[:, h, :], start=False, stop=False), reads=['rw_Arb', 'rw_Ub'], writes=['pb1'])
                P.op('pe', lambda e, h=h, c=c, pY=pY: e.matmul(pY[:, h, :], lhsT=ArkT[:, h, c, :], rhs=Vt[:, h, c, :], start=False, stop=True), reads=['rw_Ark', 'rw_Vt'], writes=['pb1'])
            for h in range(4):
                P.op('pe', lambda e, h=h, c=c, pS=pS: e.matmul(pS[:, h, :], lhsT=Bh[:, h, c, :], rhs=Ub[:, h, :], start=True, stop=False), reads=['rw_Bh', 'rw_Ub'], writes=['pb2'])
                P.op('pe', lambda e, h=h, c=c, pS=pS: e.matmul(pS[:, h, :], lhsT=Kh[:, h, c, :], rhs=Vt[:, h, c, :], start=False, stop=True), reads=['rw_Kh', 'rw_Vt'], writes=['pb2'])
            P.op('dve', lambda e, pS=pS: e.tensor_tensor(out=STb[:], in0=STw[:], in1=pS, op=ALU.add), reads=['rw_STw', 'pb2'], writes=['rw_STb'])
            P.op('dve', lambda e, pS=pS: e.tensor_tensor(out=ST[:], in0=STw[:], in1=pS, op=ALU.add), reads=['rw_STw', 'pb2'], writes=['rw_ST'])
            if not (half == 1 and c == NCH - 1):
                cn, hn = (c + 1) % NCH, (c + 1) // NCH
                if hn == 0:
                    P.op('dve', lambda e, cn=cn: e.tensor_tensor(out=STw[:], in0=ST[:], in1=bc3(WC[:, :, cn], 4, 64), op=ALU.mult), reads=['rw_ST', 'rw_WC'], writes=['rw_STw'])
            yi = c % 2
            Yc, kY = Y[yi], f'rw_Y{yi}'
            P.op('act', lambda e, Yc=Yc, pY=pY: e.copy(out=Yc[:], in_=pY), reads=['pb1'], writes=[kY])
            P.op('pe', lambda e, sl=sl: e.matmul(pb[4][0:64, 0:256], lhsT=sg[:, sl], rhs=gub[:], start=True, stop=True), reads=['rw_sg', 'rw_gub'], writes=['pb4'])
            P.op('dve', lambda e, Yc=Yc: e.tensor_reduce(out=st4[:, 0:4], in_=Yc[:], axis=AX.X, op=ALU.add), reads=[kY], writes=['rw_st4'])
            P.op('dve', lambda e: e.tensor_scalar(out=st4[:, 0:4], in0=st4[:, 0:4], scalar1=1.0 / 64, scalar2=None, op0=ALU.mult), reads=['rw_st4'], writes=['rw_st4'])
            P.op('dve', lambda e, Yc=Yc: e.tensor_tensor(out=yc[:], in0=Yc[:], in1=bc3(st4[:, 0:4], 4, 64), op=ALU.subtract), reads=[kY, 'rw_st4'], writes=['rw_yc'])
            P.op('dve', lambda e: e.tensor_tensor(out=ysq[:], in0=yc[:], in1=yc[:], op=ALU.mult), reads=['rw_yc'], writes=['rw_ysq'])
            P.op('dve', lambda e: e.tensor_reduce(out=st4[:, 4:8], in_=ysq[:], axis=AX.X, op=ALU.add), reads=['rw_ysq'], writes=['rw_st4'])
            P.op('dve', lambda e: e.tensor_scalar(out=st4[:, 4:8], in0=st4[:, 4:8], scalar1=1.0 / 64, scalar2=GN_EPS, op0=ALU.mult, op1=ALU.add), reads=['rw_st4'], writes=['rw_st4'])
            P.op('act', lambda e: e.activation(out=st4[:, 4:8], in_=st4[:, 4:8], func=AF.Sqrt), reads=['rw_st4'], writes=['rw_st4'])
            P.op('dve', lambda e: e.reciprocal(out=st4[:, 4:8], in_=st4[:, 4:8]), reads=['rw_st4'], writes=['rw_st4'])
            P.op('dve', lambda e: e.tensor_tensor(out=yc[:], in0=yc[:], in1=bc3(st4[:, 4:8], 4, 64), op=ALU.mult), reads=['rw_yc', 'rw_st4'], writes=['rw_yc'])
            ycf = yc[:].rearrange("p h v -> p (h v)")
            P.op('dve', lambda e, ycf=ycf: e.tensor_tensor(out=ycf, in0=ycf, in1=gng[:], op=ALU.mult), reads=['rw_yc', 'rw_gng'], writes=['rw_yc'])
            P.op('dve', lambda e, ycf=ycf: e.tensor_tensor(out=ycf, in0=ycf, in1=gnb[:], op=ALU.add), reads=['rw_yc', 'rw_gnb'], writes=['rw_yc'])
            P.op('dve', lambda e, c=c: e.tensor_tensor(out=ysq[:], in0=Vt[:, :, c, :], in1=bc3(rkb[:, c, :], 4, 64), op=ALU.mult), reads=['rw_Vt', 'rw_rkb'], writes=['rw_ysq'])
            P.op('dve', lambda e: e.tensor_tensor(out=yc[:], in0=yc[:], in1=ysq[:], op=ALU.add), reads=['rw_yc', 'rw_ysq'], writes=['rw_yc'])
            oi = c % 2
            P.op('dve', lambda e, oi=oi, ycf=ycf: e.tensor_tensor(out=ob[oi][:], in0=ycf, in1=pb[4][0:64, 0:256], op=ALU.mult), reads=['rw_yc', 'pb4'], writes=[f'rw_ob{oi}'])
            P.dma('sp', L.otok[t0 + c * 64:t0 + (c + 1) * 64, 512:768], ob[oi][:], reads=[f'rw_ob{oi}'], writes=[f'otok{l}_rw'])


def ln_consts(C, L, which, tag):
    P, l = C.P, L.l
    g = P.sb([128, D], F32, f"{tag}_lng{l}")
    b = P.sb([128, D], F32, f"{tag}_lnb{l}")
    P.dma('sp', g[:], C.ln_g[l, which].partition_broadcast(128), writes=[f'{tag}_lng'])
    P.dma('sp', b[:], C.ln_b[l, which].partition_broadcast(128), writes=[f'{tag}_lnb'])
    eps = P.sb([128, 2], F32, f"{tag}_eps{l}")
    P.op('dve', lambda e: e.memset(eps[:], LN_EPS), writes=[f'{tag}_eps'])
    tmp = dict(g=g, b=b, eps=eps, tag=tag,
               c=[P.sb([128, D], F32, f"{tag}_lc{l}_{i}") for i in range(2)],
               sq=P.sb([128, D], F32, f"{tag}_lsq{l}"),
               st=[P.sb([128, 8], F32, f"{tag}_lst{l}_{i}") for i in range(2)])
    return tmp


def layer_norm_tile(C, LN, i, h, kh, out_dram, kout):
    P = C.P
    tag = LN['tag']
    c, sq, st = LN['c'][i], LN['sq'], LN['st'][i]
    kc, ksq, kst = f'{tag}_lc{i}', f'{tag}_lsq', f'{tag}_lst{i}'
    P.op('dve', lambda e: e.tensor_tensor(out=st[:, 0:1], in0=st[:, 4:5], in1=st[:, 5:6], op=ALU.add), reads=[kst], writes=[kst])
    P.op('dve', lambda e: e.tensor_scalar(out=st[:, 0:1], in0=st[:, 0:1], scalar1=-1.0 / D, scalar2=None, op0=ALU.mult), reads=[kst], writes=[kst])
    P.op('act', lambda e: e.activation(out=c[:], in_=h, func=AF.Identity, bias=st[:, 0:1]), reads=[kh, kst], writes=[kc])
    P.op('act', lambda e: e.activation(out=sq[:], in_=c[:], func=AF.Square, accum_out=st[:, 1:2]), reads=[kc], writes=[ksq, kst])
    P.op('act', lambda e: e.activation(out=st[:, 2:3], in_=st[:, 1:2], func=AF.Sqrt, scale=1.0 / D, bias=LN['eps'][:, 0:1]), reads=[kst, f'{tag}_eps'], writes=[kst])
    P.op('dve', lambda e: e.reciprocal(out=st[:, 2:3], in_=st[:, 2:3]), reads=[kst], writes=[kst])
    P.op('dve', lambda e: e.scalar_tensor_tensor(out=c[:], in0=c[:], scalar=st[:, 2:3], in1=LN['g'][:], op0=ALU.mult, op1=ALU.mult), reads=[kc, kst, f'{tag}_lng'], writes=[kc])
    P.op('pool', lambda e: e.tensor_tensor(out=c[:], in0=c[:], in1=LN['b'][:], op=ALU.add), reads=[kc, f'{tag}_lnb'], writes=[kc])
    P.dma('act', out_dram, c[:], reads=[kc], writes=[kout])


def stage_merge(C, L):
    P, l = C.P, L.l
    P.stage_begin()
    pb = C.pb
    mT = P.sb([128, 8, S], BF16, f"mg_mT{l}")
    m0 = P.mark()
    oT = P.sb([128, 10, S], BF16, f"mg_oT{l}")
    xT = P.sb([128, 8, S], BF16, f"mg_xT{l}")
    P.dma('sp', xT[:], L.xT, reads=[f'xT{l}'], writes=['mg_xT'])
    ot = [P.sb([128, 1280], BF16, f"mg_ot{l}_{i}") for i in range(2)]
    wbf = [P.sb([128, 1024], F32, f"mg_wbf{l}_{i}") for i in range(2)]
    wbb = P.sb([128, 10, 1024], BF16, f"mg_wbb{l}")
    wgf = [P.sb([128, 8, 4, 128], F32, f"mg_wgf{l}_{i}") for i in range(2)]
    wgb = [P.sb([128, 8, 4, 128], BF16, f"mg_wgb{l}_{i}") for i in range(2)]
    P.dma('sp', wgf[0][:], C.wG[l, 0], writes=['mg_wgf0'])
    P.op('pool', lambda e: e.tensor_copy(out=wgb[0][:], in_=wgf[0][:]), reads=['mg_wgf0'], writes=['mg_wgb0'])
    for t in range(NT):
        i = t % 2
        if t < 10:
            cc = t
            P.dma('sp', wbf[cc % 2][:], C.w_branch[l, cc * 128:(cc + 1) * 128, :], writes=[f'mg_wbf{cc % 2}'])
            P.op('pool', lambda e, cc=cc: e.tensor_copy(out=wbb[:, cc, :], in_=wbf[cc % 2][:]), reads=[f'mg_wbf{cc % 2}'], writes=['mg_wbb'])
        P.dma('sp', ot[i][:], L.otok[t * 128:(t + 1) * 128, :], reads=[f'otok{l}_sw', f'otok{l}_rw', f'otok{l}_fx', f'otok{l}_ds'], writes=[f'mg_ot{i}'])
        for (c0, c1, bk) in ((0, 8, 0), (8, 10, 1)):
            pT = pb[bk][:].bitcast(BF16)
            for cc in range(c0, c1):
                P.op('pe', lambda e, i=i, cc=cc, c0=c0, pT=pT: e.transpose(out=pT[:, (cc - c0) * 128:(cc - c0 + 1) * 128], in_=ot[i][:, cc * 128:(cc + 1) * 128], identity=C.ident_bf[:]), reads=[f'mg_ot{i}', 'ident_bf'], writes=[f'pb{bk}'])
            P.op('act', lambda e, t=t, c0=c0, c1=c1, pT=pT: e.copy(out=oT[:, c0:c1, t * 128:(t + 1) * 128], in_=pT[:, 0:(c1 - c0) * 128].rearrange("p (k c) -> p k c", c=128)), reads=[f'pb{bk}'], writes=['mg_oT'])
    gb = P.sb([128, 4, 8], F32, f"mg_gb{l}")
    P.dma('sp', gb[:], C.gbias[l], writes=['mg_gb'])
    gt = [P.sb([128, 512], F32, f"mg_gt{l}_{i}") for i in range(2)]
    macc = P.sb([128, 512], F32, f"mg_macc{l}")
    mtmp = P.sb([128, 512], F32, f"mg_mtmp{l}")
    BR = ((0, 4), (4, 6), (6, 8), (8, 10))
    n = 0
    for f in range(8):
        fi = f % 2
        if f > 0:
            P.dma('sp', wgf[fi][:], C.wG[l, f], writes=[f'mg_wgf{fi}'])
            P.op('pool', lambda e, fi=fi: e.tensor_copy(out=wgb[fi][:], in_=wgf[fi][:]), reads=[f'mg_wgf{fi}'], writes=[f'mg_wgb{fi}'])
        for tb in range(4):
            ts_ = slice(tb * 512, (tb + 1) * 512)
            for br in range(4):
                i = n % 2
                n += 1
                bg, bp = 2 + i, 4 + i
                for kc in range(8):
                    P.op('pe', lambda e, fi=fi, kc=kc, br=br, ts_=ts_, bg=bg: e.matmul(pb[bg][:], lhsT=wgb[fi][:, kc, br, :], rhs=xT[:, kc, ts_], start=(kc == 0), stop=(kc == 7)), reads=[f'mg_wgb{fi}', 'mg_xT'], writes=[f'pb{bg}'])
                P.op('act', lambda e, i=i, bg=bg, br=br, f=f: e.activation(out=gt[i][:], in_=pb[bg][:], func=AF.Sigmoid, bias=gb[:, br, f:f + 1]), reads=[f'pb{bg}', 'mg_gb'], writes=[f'mg_gt{i}'])
                c0, c1 = BR[br]
                for cc in range(c0, c1):
                    P.op('pe', lambda e, cc=cc, f=f, ts_=ts_, bp=bp, c0=c0, c1=c1: e.matmul(pb[bp][:], lhsT=wbb[:, cc, f * 128:(f + 1) * 128], rhs=oT[:, cc, ts_], start=(cc == c0), stop=(cc == c1 - 1)), reads=['mg_wbb', 'mg_oT'], writes=[f'pb{bp}'])
                if br == 0:
                    P.op('dve', lambda e, i=i, bp=bp: e.tensor_tensor(out=macc[:], in0=gt[i][:], in1=pb[bp][:], op=ALU.mult), reads=[f'mg_gt{i}', f'pb{bp}'], writes=['mg_macc'])
                else:
                    P.op('dve', lambda e, i=i, bp=bp: e.tensor_tensor(out=mtmp[:], in0=gt[i][:], in1=pb[bp][:], op=ALU.mult), reads=[f'mg_gt{i}', f'pb{bp}'], writes=['mg_mtmp'])
                    if br < 3:
                        P.op('dve', lambda e: e.tensor_tensor(out=macc[:], in0=macc[:], in1=mtmp[:], op=ALU.add), reads=['mg_macc', 'mg_mtmp'], writes=['mg_macc'])
                    else:
                        P.op('dve', lambda e, f=f, ts_=ts_: e.tensor_tensor(out=mT[:, f, ts_], in0=macc[:], in1=mtmp[:], op=ALU.add), reads=['mg_macc', 'mg_mtmp'], writes=['mg_mT'])
    P.release(m0)
    wbf2 = P.sb([128, 1024], F32, f"mg_wbf2{l}")
    wob = P.sb([128, 8, 1024], BF16, f"mg_wob{l}")
    for cc in range(8):
        P.dma('sp', wbf2[:], C.w_out[l, cc * 128:(cc + 1) * 128, :], writes=['mg_wbf2'])
        P.op('pool', lambda e, cc=cc: e.tensor_copy(out=wob[:, cc, :], in_=wbf2[:]), reads=['mg_wbf2'], writes=['mg_wob'])
    LN = ln_consts(C, L, 0, "mg")
    xr = [P.sb([128, D], F32, f"mg_xr{l}_{i}") for i in range(2)]
    for t in range(NT):
        i = t % 2
        P.dma('sp', xr[i][:], L.xin[t * 128:(t + 1) * 128, :], reads=['xin'], writes=[f'mg_xr{i}'])
        for hf in range(2):
            bk = 6 + hf
            for f in range(8):
                P.op('pe', lambda e, f=f, t=t, hf=hf, bk=bk: e.matmul(pb[bk][:], lhsT=mT[:, f, t * 128:(t + 1) * 128], rhs=wob[:, f, hf * 512:(hf + 1) * 512], start=(f == 0), stop=(f == 7)), reads=['mg_mT', 'mg_wob'], writes=[f'pb{bk}'])
            P.op('dve', lambda e, i=i, hf=hf, bk=bk: e.scalar_tensor_tensor(out=xr[i][:, hf * 512:(hf + 1) * 512], in0=xr[i][:, hf * 512:(hf + 1) * 512], scalar=ALPHA, in1=pb[bk][:], op0=ALU.mult, op1=ALU.add, accum_out=LN['st'][i][:, 4 + hf:5 + hf]), reads=[f'mg_xr{i}', f'pb{bk}'], writes=[f'mg_xr{i}', f'mg_lst{i}'])
        layer_norm_tile(C, LN, i, xr[i][:], f'mg_xr{i}', L.x1[t * 128:(t + 1) * 128, :], f'x1_{l}')


def make_xT(C, L, tag, src, ksrc, want_f32_router=None, xT=None):
    P, l = C.P, L.l
    pb = C.pb
    if xT is None:
        xT = P.sb([128, 8, S], BF16, f"{tag}_xT{l}")
    xs = [P.sb([128, D], F32, f"{tag}_xs{l}_{i}") for i in range(2)]
    xb = [P.sb([128, D], BF16, f"{tag}_xb{l}_{i}") for i in range(2)]
    if want_f32_router is not None:
        rwf, lg = want_f32_router
        xtf = [P.sb([128, 8, 128], F32, f"{tag}_xtf{l}_{i}") for i in range(2)]
    for t in range(NT):
        i = t % 2
        P.dma('sp', xs[i][:], src[t * 128:(t + 1) * 128, :], reads=[ksrc], writes=[f'{tag}_xs{i}'])
        P.op('dve', lambda e, i=i: e.tensor_copy(out=xb[i][:], in_=xs[i][:]), reads=[f'{tag}_xs{i}'], writes=[f'{tag}_xb{i}'])
        pT = pb[t % 2][:].bitcast(BF16)
        for kc in range(8):
            P.op('pe', lambda e, i=i, kc=kc, pT=pT: e.transpose(out=pT[:, kc * 128:(kc + 1) * 128], in_=xb[i][:, kc * 128:(kc + 1) * 128], identity=C.ident_bf[:]), reads=[f'{tag}_xb{i}', 'ident_bf'], writes=[f'pb{t % 2}'])
        P.op('act', lambda e, t=t, pT=pT: e.copy(out=xT[:, :, t * 128:(t + 1) * 128], in_=pT.rearrange("p (k c) -> p k c", k=8)), reads=[f'pb{t % 2}'], writes=[f'{tag}_xT'])
        if want_f32_router is not None:
            for hf in range(2):
                bk = 2 + hf
                for kk_ in range(4):
                    kc = hf * 4 + kk_
                    P.op('pe', lambda e, i=i, kc=kc, kk_=kk_, bk=bk: e.transpose(out=pb[bk][:, kk_ * 128:(kk_ + 1) * 128], in_=xs[i][:, kc * 128:(kc + 1) * 128], identity=C.ident_f), reads=[f'{tag}_xs{i}', 'cst'], writes=[f'pb{bk}'])
                P.op('act', lambda e, i=i, hf=hf, bk=bk: e.copy(out=xtf[i][:, hf * 4:hf * 4 + 4, :], in_=pb[bk][:].rearrange("p (k c) -> p k c", c=128)), reads=[f'pb{bk}'], writes=[f'{tag}_xtf{i}'])
            for kc in range(8):
                P.op('pe', lambda e, i=i, kc=kc: e.matmul(pb[4][:, 0:8], lhsT=xtf[i][:, kc, :], rhs=rwf[:, kc, :], start=(kc == 0), stop=(kc == 7)), reads=[f'{tag}_xtf{i}', 'moe_rwf'], writes=['pb4'])
            P.op('dve', lambda e, t=t: e.tensor_copy(out=lg[:, t, :], in_=pb[4][:, 0:8]), reads=['pb4'], writes=['moe_lg'])
    return xT


def ffn_phase1(C, L, tag, xT, w13, nf, hT, w13f, w13b, w2=None, w2b=None, w2f=None):
    P, l = C.P, L.l
    pb = C.pb
    st = C.ffn_sil
    n = 0
    for f in range(nf):
        fi = f % 2
        P.dma('sp', w13f[fi][:], w13[f], writes=[f'{tag}_w13f{fi}'])
        P.op('pool', lambda e, fi=fi: e.tensor_copy(out=w13b[fi][:], in_=w13f[fi][:]), reads=[f'{tag}_w13f{fi}'], writes=[f'{tag}_w13b{fi}'])
        if w2 is not None:
            P.dma('sp', w2f[fi][:], w2[f], writes=[f'{tag}_w2f{fi}'])
            P.op('pool', lambda e, fi=fi, f=f: e.tensor_copy(out=w2b[:, f, :], in_=w2f[fi][:]), reads=[f'{tag}_w2f{fi}'], writes=[f'{tag}_w2b{f}'])
        for tb in range(4):
            ts_ = slice(tb * 512, (tb + 1) * 512)
            i = n % 2
            n += 1
            ba, bb = i, 2 + i
            for kc in range(8):
                P.op('pe', lambda e, fi=fi, kc=kc, ts_=ts_, ba=ba: e.matmul(pb[ba][:], lhsT=w13b[fi][:, kc, 0, :], rhs=xT[:, kc, ts_], start=(kc == 0), stop=(kc == 7)), reads=[f'{tag}_w13b{fi}', f'{tag}_xT'], writes=[f'pb{ba}'])
            for kc in range(8):
                P.op('pe', lambda e, fi=fi, kc=kc, ts_=ts_, bb=bb: e.matmul(pb[bb][:], lhsT=w13b[fi][:, kc, 1, :], rhs=xT[:, kc, ts_], start=(kc == 0), stop=(kc == 7)), reads=[f'{tag}_w13b{fi}', f'{tag}_xT'], writes=[f'pb{bb}'])
            P.op('act', lambda e, i=i, ba=ba: e.activation(out=st[i][:], in_=pb[ba][:], func=AF.Silu), reads=[f'pb{ba}'], writes=[f'ffn_sil{i}'])
            P.op('dve', lambda e, i=i, bb=bb, f=f, ts_=ts_: e.tensor_tensor(out=hT[:, f, ts_], in0=st[i][:], in1=pb[bb][:], op=ALU.mult), reads=[f'ffn_sil{i}', f'pb{bb}'], writes=[f'{tag}_hT'])


def ffn_phase2(C, L, tag, w2, nf, hT, w2b, w2f, y_cb):
    P, l = C.P, L.l
    pb = C.pb
    for t in range(NT):
        for hf in range(2):
            bk = 4 + (2 * t + hf) % 4
            for f in range(nf):
                P.op('pe', lambda e, f=f, t=t, hf=hf, bk=bk: e.matmul(pb[bk][:], lhsT=hT[:, f, t * 128:(t + 1) * 128], rhs=w2b[:, f, hf * 512:(hf + 1) * 512], start=(f == 0), stop=(f == nf - 1)), reads=[f'{tag}_hT', f'{tag}_w2b{f}'], writes=[f'pb{bk}'])
            y_cb(t, hf, pb[bk], f'pb{bk}')


def stage_ffn_dense(C, L):
    P, l = C.P, L.l
    P.stage_begin()
    nf = D_FF // 128
    hT = P.sb([128, nf, S], BF16, f"ff_hT{l}")
    w2b = P.sb([128, nf, D], BF16, f"ff_w2b{l}")
    w2f = [P.sb([128, D], F32, f"ff_w2f{l}_{i}") for i in range(2)]
    m0 = P.mark()
    xT = P.sb([128, 8, S], BF16, f"ff_xT{l}")
    m1 = P.mark()
    make_xT(C, L, "ff", L.x1, f'x1_{l}', xT=xT)
    P.release(m1)
    w13f = [P.sb([128, 8, 2, 128], F32, f"ff_w13f{l}_{i}") for i in range(2)]
    w13b = [P.sb([128, 8, 2, 128], BF16, f"ff_w13b{l}_{i}") for i in range(2)]
    C.ffn_sil = [P.sb([128, 512], F32, f"ff_sil{l}_{i}") for i in range(2)]
    ffn_phase1(C, L, "ff", xT, C.w13d, nf, hT, w13f, w13b, C.w2d, w2b, w2f)
    P.release(m0)
    LN = ln_consts(C, L, 1, "ff")
    xr = [P.sb([128, D], F32, f"ff_xr{l}_{i}") for i in range(2)]
    dst = L.xout

    def y_cb(t, hf, bank, kb):
        i = t % 2
        if hf == 0:
            P.dma('sp', xr[i][:], L.x1[t * 128:(t + 1) * 128, :], reads=[f'x1_{l}'], writes=[f'ff_xr{i}'])
        P.op('dve', lambda e: e.scalar_tensor_tensor(out=xr[i][:, hf * 512:(hf + 1) * 512], in0=xr[i][:, hf * 512:(hf + 1) * 512], scalar=ALPHA, in1=bank[:], op0=ALU.mult, op1=ALU.add, accum_out=LN['st'][i][:, 4 + hf:5 + hf]), reads=[f'ff_xr{i}', kb], writes=[f'ff_xr{i}', f'ff_lst{i}'])
        if hf == 1:
            layer_norm_tile(C, LN, i, xr[i][:], f'ff_xr{i}', dst[t * 128:(t + 1) * 128, :], f'xout{l}')

    ffn_phase2(C, L, "ff", C.w2d, nf, hT, w2b, w2f, y_cb)


def stage_moe(C, L):
    P, l = C.P, L.l
    P.stage_begin()
    pb = C.pb
    rwf = P.sb([128, 8, NEXP], F32, f"moe_rwf{l}")
    P.dma('sp', rwf[:], C.router_w.rearrange("(k p) e -> p k e", p=128), writes=['moe_rwf'])
    rb = P.sb([128, NEXP], F32, f"moe_rb{l}")
    P.dma('sp', rb[:], C.router_b.partition_broadcast(128), writes=['moe_rb'])
    lg = P.sb([128, NT, NEXP], F32, f"moe_lg{l}")
    m1 = P.sb([128, NT], F32, f"moe_m1{l}")
    m2 = P.sb([128, NT], F32, f"moe_m2{l}")
    eq1 = P.sb([128, NT, NEXP], F32, f"moe_eq1{l}")
    eq2 = P.sb([128, NT, NEXP], F32, f"moe_eq2{l}")
    lg2 = P.sb([128, NT, NEXP], F32, f"moe_lg2{l}")
    comb = P.sb([128, NT, NEXP], F32, f"moe_comb{l}")
    yacc = P.sb([128, NT, D], F32, f"moe_yacc{l}")
    xT = P.sb([128, 8, S], BF16, f"moe_xT{l}")
    m0 = P.mark()
    make_xT(C, L, "moe", L.x1, f'x1_{l}', want_f32_router=(rwf, lg), xT=xT)
    bcx = lambda a: a.unsqueeze(2).to_broadcast([128, NT, NEXP])
    P.op('dve', lambda e: e.tensor_tensor(out=lg[:], in0=lg[:], in1=rb[:].unsqueeze(1).to_broadcast([128, NT, NEXP]), op=ALU.add), reads=['moe_lg', 'moe_rb'], writes=['moe_lg'])
    P.op('dve', lambda e: e.tensor_reduce(out=m1[:], in_=lg[:], axis=AX.X, op=ALU.max), reads=['moe_lg'], writes=['moe_m1'])
    P.op('dve', lambda e: e.tensor_tensor(out=eq1[:], in0=lg[:], in1=bcx(m1[:]), op=ALU.is_equal), reads=['moe_lg', 'moe_m1'], writes=['moe_eq1'])
    P.op('dve', lambda e: e.scalar_tensor_tensor(out=lg2[:], in0=eq1[:], scalar=-1.0e30, in1=lg[:], op0=ALU.mult, op1=ALU.add), reads=['moe_eq1', 'moe_lg'], writes=['moe_lg2'])
    P.op('dve', lambda e: e.tensor_reduce(out=m2[:], in_=lg2[:], axis=AX.X, op=ALU.max), reads=['moe_lg2'], writes=['moe_m2'])
    P.op('dve', lambda e: e.tensor_tensor(out=eq2[:], in0=lg2[:], in1=bcx(m2[:]), op=ALU.is_equal), reads=['moe_lg2', 'moe_m2'], writes=['moe_eq2'])
    P.op('dve', lambda e: e.tensor_tensor(out=m2[:], in0=m2[:], in1=m1[:], op=ALU.subtract), reads=['moe_m1', 'moe_m2'], writes=['moe_m2'])
    P.op('act', lambda e: e.activation(out=m2[:], in_=m2[:], func=AF.Exp), reads=['moe_m2'], writes=['moe_m2'])
    P.op('dve', lambda e: e.tensor_scalar(out=m1[:], in0=m2[:], scalar1=1.0, scalar2=None, op0=ALU.add), reads=['moe_m2'], writes=['moe_m1'])
    P.op('dve', lambda e: e.reciprocal(out=m1[:], in_=m1[:]), reads=['moe_m1'], writes=['moe_m1'])
    P.op('dve', lambda e: e.tensor_tensor(out=m2[:], in0=m2[:], in1=m1[:], op=ALU.mult), reads=['moe_m1', 'moe_m2'], writes=['moe_m2'])
    P.op('dve', lambda e: e.tensor_tensor(out=comb[:], in0=eq1[:], in1=bcx(m1[:]), op=ALU.mult), reads=['moe_eq1', 'moe_m1'], writes=['moe_comb'])
    P.op('dve', lambda e: e.tensor_tensor(out=eq2[:], in0=eq2[:], in1=bcx(m2[:]), op=ALU.mult), reads=['moe_eq2', 'moe_m2'], writes=['moe_eq2'])
    P.op('dve', lambda e: e.tensor_tensor(out=comb[:], in0=comb[:], in1=eq2[:], op=ALU.add), reads=['moe_comb', 'moe_eq2'], writes=['moe_comb'])
    if 'moecomb' in C.dbg:
        P.dma('sp', C.scratch("moecomb", [128, NT, NEXP], F32), comb[:], reads=['moe_comb'], writes=['moecomb'])

    P.release(m0)
    nf = D_FFE // 128
    hT = P.sb([128, nf, S], BF16, f"moe_hT{l}")
    w2b = P.sb([128, nf, D], BF16, f"moe_w2b{l}")
    w13f = [P.sb([128, 8, 2, 128], F32, f"moe_w13f{l}_{i}") for i in range(2)]
    w13b = [P.sb([128, 8, 2, 128], BF16, f"moe_w13b{l}_{i}") for i in range(2)]
    w2f = [P.sb([128, D], F32, f"moe_w2f{l}_{i}") for i in range(2)]
    C.ffn_sil = [P.sb([128, 512], F32, f"moe_sil{l}_{i}") for i in range(2)]
    for ex in range(NEXP):
        def y_cb(t, hf, bank, kb, ex=ex):
            sl = slice(hf * 512, (hf + 1) * 512)
            if ex == 0:
                P.op('dve', lambda e: e.tensor_scalar(out=yacc[:, t, sl], in0=bank[:], scalar1=comb[:, t, ex:ex + 1], scalar2=None, op0=ALU.mult), reads=[kb, 'moe_comb'], writes=[f'moe_yacc{t}'])
            else:
                P.op('dve', lambda e: e.scalar_tensor_tensor(out=yacc[:, t, sl], in0=bank[:], scalar=comb[:, t, ex:ex + 1], in1=yacc[:, t, sl], op0=ALU.mult, op1=ALU.add), reads=[kb, 'moe_comb', f'moe_yacc{t}'], writes=[f'moe_yacc{t}'])
        ffn_phase1(C, L, "moe", xT, C.w13e[ex], nf, hT, w13f, w13b, C.w2e[ex], w2b, w2f)
        ffn_phase2(C, L, "moe", C.w2e[ex], nf, hT, w2b, w2f, y_cb)
    P.release(m0)
    LN = ln_consts(C, L, 1, "mo")
    xr = [P.sb([128, D], F32, f"mo_xr{l}_{i}") for i in range(2)]
    for t in range(NT):
        i = t % 2
        P.dma('sp', xr[i][:], L.x1[t * 128:(t + 1) * 128, :], reads=[f'x1_{l}'], writes=[f'mo_xr{i}'])
        for hf in range(2):
            P.op('dve', lambda e, i=i, t=t, hf=hf: e.scalar_tensor_tensor(out=xr[i][:, hf * 512:(hf + 1) * 512], in0=xr[i][:, hf * 512:(hf + 1) * 512], scalar=ALPHA, in1=yacc[:, t, hf * 512:(hf + 1) * 512], op0=ALU.mult, op1=ALU.add, accum_out=LN['st'][i][:, 4 + hf:5 + hf]), reads=[f'mo_xr{i}', f'moe_yacc{t}'], writes=[f'mo_xr{i}', f'mo_lst{i}'])
        layer_norm_tile(C, LN, i, xr[i][:], f'mo_xr{i}', L.xout[t * 128:(t + 1) * 128, :], f'xout{l}')


def kernel(**inputs):
    sh = prep_shared(inputs)
    nc = build_program("ABCDEFG", layers=(0, 1))
    x = np.asarray(inputs['x'])
    n = x.shape[0]
    in_maps = [dict(sh, x=np.ascontiguousarray(x[b])) for b in range(n)]
    res = run_bass_kernel_spmd(nc, in_maps, core_ids=list(range(n)))
    return np.stack([np.asarray(r['out']) for r in res.results]).astype(np.float32)


BIGM = 32768.0
DSA_ACT_J = tuple(range(11, 16))


def stage_dsa2(C, L):
    P, l = C.P, L.l
    P.stage_begin()
    pb = C.pb
    qT = load_heads(C, L, "ds_q", HS_DSQ, 4)
    kT = load_heads(C, L, "ds_k", HS_DSK, 1)
    va = load_vaug(C, L, "ds_v", 384, 1)
    wi = P.sb([128, NT, 4], F32, f"ds_wi{l}")
    P.dma('sp', wi[:], L.tokm.rearrange("(t p) c -> p t c", p=128)[:, :, 452:456], reads=[f'tokm{l}'], writes=['ds_wi'])
    OFF = [128 * j * (j + 1) // 2 for j in range(NT + 1)]
    sc = P.sb([128, OFF[NT]], F32, f"ds_sc{l}")
    mpm = P.sb([128, OFF[NT]], BF16, f"ds_mpm{l}")
    ods = P.sb([128, NT, 256], BF16, f"ds_o{l}")
    identB = P.sb([128, 128], BF16, f"ds_identB{l}")
    P.op('dve', lambda e: e.tensor_scalar(out=identB[:], in0=C.ident_f, scalar1=BIGM, scalar2=None, op0=ALU.mult), reads=['cst'], writes=['ds_identB'])
    stt = {nm: P.sb([128, NT], F32, f"ds_{nm}{l}") for nm in ('lo', 'rng', 'mid', 'wc', 'sel', 'mx', 'thr', 'need')}
    cnt = P.sb([128, NT], F32, f"ds_cnt{l}")
    rec = P.sb([128, 4], F32, f"ds_rec{l}")
    m0 = P.mark()
    qiT = load_heads(C, L, "ds_qi", HS_DSQI, 4)
    kiT = load_heads(C, L, "ds_ki", HS_DSKI, 1)
    rl = [P.sb([128, 512], F32, f"ds_rl{l}_{i}") for i in range(4)]
    nr = 0
    for j in range(NT):
        n = (j + 1) * 128
        o = OFF[j]
        ksc = f'ds_sc{j}'
        for c0 in range(0, n, 512):
            nn = min(512, n - c0)
            for h in range(4):
                bk = nr % 8
                ri = nr % 4
                nr += 1
                P.op('pe', lambda e, h=h, j=j, c0=c0, nn=nn, bk=bk: e.matmul(pb[bk][:, 0:nn], lhsT=qiT[:, h, j * 128:(j + 1) * 128], rhs=kiT[:, 0, c0:c0 + nn], start=True, stop=True),
                     reads=['ds_qi', 'ds_ki'], writes=[f'pb{bk}'])
                P.op('act', lambda e, bk=bk, ri=ri, nn=nn: e.activation(out=rl[ri][:, 0:nn], in_=pb[bk][:, 0:nn], func=AF.Relu), reads=[f'pb{bk}'], writes=[f'ds_rl{ri}'])
                if h == 0:
                    P.op('dve', lambda e, ri=ri, nn=nn, c0=c0, j=j, o=o: e.tensor_scalar(out=sc[:, o + c0:o + c0 + nn], in0=rl[ri][:, 0:nn], scalar1=wi[:, j, 0:1], scalar2=None, op0=ALU.mult),
                         reads=[f'ds_rl{ri}', 'ds_wi'], writes=[ksc])
                else:
                    P.op('dve', lambda e, ri=ri, nn=nn, c0=c0, j=j, h=h, o=o: e.scalar_tensor_tensor(out=sc[:, o + c0:o + c0 + nn], in0=rl[ri][:, 0:nn], scalar=wi[:, j, h:h + 1], in1=sc[:, o + c0:o + c0 + nn], op0=ALU.mult, op1=ALU.add),
                         reads=[f'ds_rl{ri}', 'ds_wi', ksc], writes=[ksc])
        if j >= 2:
            P.op('dve', lambda e, j=j, n=n, o=o: e.tensor_reduce(out=stt['lo'][:, j:j + 1], in_=sc[:, o:o + n - 64], axis=AX.X, op=ALU.min), reads=[ksc], writes=[f'ds_lo{j}'])
        P.op('dve', lambda e, n=n, o=o: e.memset(sc[0:64, o + n - 64:o + n], NEG), reads=[ksc], writes=[ksc])
        if j >= 2:
            P.op('dve', lambda e, j=j, n=n, o=o: e.tensor_reduce(out=stt['mx'][:, j:j + 1], in_=sc[:, o:o + n], axis=AX.X, op=ALU.max), reads=[ksc], writes=[f'ds_mx{j}'])
    P.release(m0)
    junk = {eng: P.sb([128, S], BF16, f"ds_junk{l}_{eng}") for eng in ('dve', 'act')}
    junkr = {eng: [P.sb([128, S], BF16, f"ds_junkr{l}_{eng}{i}") for i in range(3)] for eng in ('dve', 'act')}
    tA = {eng: P.sb([128, S], F32, f"ds_tA{l}_{eng}") for eng in ('dve',)}
    tE = {eng: P.sb([128, S], F32, f"ds_tE{l}_{eng}") for eng in ('dve',)}
    halfn = P.sb([128, NT], F32, f"ds_halfn{l}")
    ssum = P.sb([128, NT], F32, f"ds_ssum{l}")
    nmid = P.sb([128, NT], F32, f"ds_nmid{l}")
    for j in DSA_ACT_J:
        P.op('dve', lambda e, j=j: e.memset(halfn[:, j:j + 1], float((j + 1) * 64)), writes=['ds_halfn'])
    PT = [P.sb([128, 4, 128], BF16, f"ds_PT{l}_{i}") for i in range(2)]
    JS = list(range(2, NT))
    ENG = {j: ('act' if j in DSA_ACT_J else 'dve') for j in JS}
    allk = lambda nm: [f'ds_{nm}{j}' for j in JS]
    c2 = slice(2, NT)
    P.op('dve', lambda e: e.tensor_tensor(out=stt['rng'][:, c2], in0=stt['mx'][:, c2], in1=stt['lo'][:, c2], op=ALU.subtract), reads=allk('mx') + allk('lo'), writes=['ds_rng'])
    for b in range(NBIS):
        P.op('dve', lambda e, b=b: e.tensor_scalar(out=stt['wc'][:, c2], in0=stt['rng'][:, c2], scalar1=float(0.5 ** (b + 1)), scalar2=None, op0=ALU.mult), reads=['ds_rng'], writes=['ds_wc'])
        P.op('dve', lambda e: e.tensor_tensor(out=stt['mid'][:, c2], in0=stt['lo'][:, c2], in1=stt['wc'][:, c2], op=ALU.add), reads=allk('lo') + ['ds_wc'], writes=['ds_mid'])
        ca = slice(DSA_ACT_J[0], DSA_ACT_J[-1] + 1)
        P.op('dve', lambda e, ca=ca: e.scalar_tensor_tensor(out=nmid[:, ca], in0=stt['lo'][:, ca], scalar=-1.0, in1=stt['wc'][:, ca], op0=ALU.mult, op1=ALU.subtract), reads=allk('lo') + ['ds_wc'], writes=['ds_nmid'])
        for j in JS:
            n, o, eng = (j + 1) * 128, OFF[j], ENG[j]
            if eng == 'dve':
                P.op(eng, lambda e, j=j, n=n, o=o, eng=eng: e.tensor_scalar(out=junkr[eng][j % 3][:, 0:n], in0=sc[:, o:o + n], scalar1=stt['mid'][:, j:j + 1], scalar2=None, op0=ALU.is_ge, op1=ALU.add, accum_out=cnt[:, j:j + 1]),
                     reads=['ds_mid', f'ds_sc{j}'], writes=[f'ds_cnt{j}', f'ds_junkr_{eng}{j % 3}'])
            else:
                P.op(eng, lambda e, j=j, n=n, o=o, eng=eng: e.activation(out=junkr[eng][j % 3][:, 0:n], in_=sc[:, o:o + n], func=AF.Sign, bias=nmid[:, j:j + 1], accum_out=ssum[:, j:j + 1]),
                     reads=['ds_nmid', f'ds_sc{j}'], writes=[f'ds_ssum{j}', f'ds_junkr_{eng}{j % 3}'])
        P.op('dve', lambda e, ca=ca: e.scalar_tensor_tensor(out=cnt[:, ca], in0=ssum[:, ca], scalar=0.5, in1=halfn[:, ca], op0=ALU.mult, op1=ALU.add), reads=[f'ds_ssum{j}' for j in DSA_ACT_J] + ['ds_halfn'], writes=[f'ds_cnt{j}' for j in DSA_ACT_J])
        P.op('dve', lambda e: e.scalar_tensor_tensor(out=stt['sel'][:, c2], in0=cnt[:, c2], scalar=255.5, in1=stt['wc'][:, c2], op0=ALU.is_ge, op1=ALU.mult), reads=allk('cnt') + ['ds_wc'], writes=['ds_sel'])
        P.op('dve', lambda e: e.tensor_tensor(out=stt['lo'][:, c2], in0=stt['lo'][:, c2], in1=stt['sel'][:, c2], op=ALU.add), reads=allk('lo') + ['ds_sel'], writes=allk('lo'))
    for j in range(NT):
        n, o = (j + 1) * 128, OFF[j]
        ksc, kmk = f'ds_sc{j}', f'ds_mpm{j}'
        if j < 2:
            P.op('dve', lambda e, n=n, o=o: e.tensor_scalar(out=mpm[:, o:o + n], in0=sc[:, o:o + n], scalar1=-1.0e29, scalar2=-1.0, op0=ALU.is_lt, op1=ALU.mult), reads=[ksc], writes=[kmk])
            continue
        eng = 'dve'
        a, t_, jk = tA[eng], tE[eng], junk[eng]
        ka, ke, kj = f'ds_tA_{eng}', f'ds_tE_{eng}', f'ds_junk_{eng}'
        thr, need = stt['thr'][:, j:j + 1], stt['need'][:, j:j + 1]
        lo = stt['lo'][:, j:j + 1]
        P.op(eng, lambda e, a=a, n=n, o=o, lo=lo: e.tensor_scalar(out=a[:, 0:n], in0=sc[:, o:o + n], scalar1=lo, scalar2=1.0e37, op0=ALU.is_lt, op1=ALU.mult), reads=[ksc, f'ds_lo{j}'], writes=[ka])
        P.op(eng, lambda e, a=a, n=n, o=o: e.tensor_tensor(out=a[:, 0:n], in0=a[:, 0:n], in1=sc[:, o:o + n], op=ALU.add), reads=[ksc, ka], writes=[ka])
        if eng == 'dve':
            P.op(eng, lambda e, a=a, n=n, thr=thr: e.tensor_reduce(out=thr, in_=a[:, 0:n], axis=AX.X, op=ALU.min), reads=[ka], writes=[f'ds_thr{j}'])
        else:
            P.op(eng, lambda e, a=a, n=n, thr=thr, t_=t_: e.tensor_scalar(out=t_[:, 0:n], in0=a[:, 0:n], scalar1=0.0, scalar2=None, op0=ALU.add, op1=ALU.min, accum_out=thr), reads=[ka], writes=[f'ds_thr{j}', ke])
        P.op(eng, lambda e, jk=jk, n=n, o=o, thr=thr, need=need: e.tensor_scalar(out=jk[:, 0:n], in0=sc[:, o:o + n], scalar1=thr, scalar2=None, op0=ALU.is_le, op1=ALU.add, accum_out=need), reads=[ksc, f'ds_thr{j}'], writes=[kj, f'ds_need{j}'])
        P.op(eng, lambda e, need=need, n=n: e.tensor_scalar(out=need, in0=need, scalar1=float(256 - n), scalar2=None, op0=ALU.add), reads=[f'ds_need{j}'], writes=[f'ds_need{j}'])
        P.op(eng, lambda e, t_=t_, n=n, o=o, thr=thr: e.tensor_scalar(out=t_[:, 0:n], in0=sc[:, o:o + n], scalar1=thr, scalar2=None, op0=ALU.is_equal), reads=[ksc, f'ds_thr{j}'], writes=[ke])
        P.op(eng, lambda e, a=a, t_=t_, n=n: e.tensor_tensor_scan(out=a[:, 0:n], data0=C.ones_f[:, 0:1].to_broadcast([128, n]), data1=t_[:, 0:n], initial=0.0, op0=ALU.mult, op1=ALU.add), reads=[ke, 'cst'], writes=[ka])
        P.op(eng, lambda e, a=a, t_=t_, n=n, need=need: e.scalar_tensor_tensor(out=a[:, 0:n], in0=a[:, 0:n], scalar=need, in1=t_[:, 0:n], op0=ALU.is_le, op1=ALU.mult), reads=[ka, ke, f'ds_need{j}'], writes=[ka])
        P.op(eng, lambda e, a=a, jk=jk, n=n, o=o: e.tensor_tensor(out=mpm[:, o:o + n], in0=a[:, 0:n], in1=jk[:, 0:n], op=ALU.subtract), reads=[ka, kj], writes=[kmk])
    items = [(j, kt) for j in range(NT) for kt in range(j + 1)]
    po = pb[7][:, 0:260].rearrange("p (h c) -> p h c", h=4)

    def qk(idx):
        j, kt = items[idx]
        i = idx % 4
        o = OFF[j]
        bs4 = pb[i][:].rearrange("p (h q) -> p h q", h=4)
        ks = f'pb{i}'
        P.op('pe', lambda e: e.matmul(bs4, lhsT=kT[:, 0, kt * 128:(kt + 1) * 128], rhs=qT[:, :, j * 128:(j + 1) * 128], start=True, stop=False),
             reads=['ds_q', 'ds_k'], writes=[ks])
        P.op('pe', lambda e: e.matmul(bs4, lhsT=mpm[:, o + kt * 128:o + (kt + 1) * 128], rhs=identB[:].unsqueeze(1).to_broadcast([128, 4, 128]), start=False, stop=True),
             reads=[f'ds_mpm{j}', 'ds_identB'], writes=[ks])
        pi = idx % 2
        P.op('act', lambda e: e.activation(out=PT[pi][:], in_=bs4, func=AF.Exp, scale=0.125), reads=[ks], writes=[f'ds_PT{pi}'])

    def pv(idx):
        j, kt = items[idx]
        pi = idx % 2
        if kt == 0:
            P.op('dve', lambda e: e.memset(pb[7][:, 0:260], 0.0), writes=['pb7'])
        for h in range(4):
            P.op('pe', lambda e, h=h: e.matmul(po[:, h, :], lhsT=PT[pi][:, h, :], rhs=va[:, kt, 0, :], start=False, stop=(kt == j), skip_group_check=True),
                 reads=[f'ds_PT{pi}', 'ds_v'], writes=['pb7'])
        if kt == j:
            P.op('dve', lambda e: e.reciprocal(out=rec[:], in_=po[:, :, 64]), reads=['pb7'], writes=['ds_rec'])
            for h in range(4):
                P.op('dve', lambda e, h=h: e.tensor_scalar(out=ods[:, j, h * 64:(h + 1) * 64], in0=po[:, h, 0:64], scalar1=rec[:, h:h + 1], scalar2=None, op0=ALU.mult), reads=['pb7', 'ds_rec'], writes=['ds_o'])

    qk(0)
    for idx in range(len(items)):
        if idx + 1 < len(items):
            qk(idx + 1)
        pv(idx)
    P.dma('sp', L.otok.rearrange("(t p) c -> p t c", p=128)[:, :, 1024:1280], ods[:], reads=['ds_o'], writes=[f'otok{l}_ds'])
```

```python
import numpy as np
from contextlib import ExitStack
import concourse.bass as bass
import concourse.mybir as mybir
from concourse.bass_utils import run_bass_kernel_spmd

F32 = mybir.dt.float32
BF16 = mybir.dt.bfloat16
AF = mybir.ActivationFunctionType
ALU = mybir.AluOpType
AX = mybir.AxisListType

S = 2048
D = 1024
NT = 16
DEPTH = 2
D_IN = 7368
D_FF = 2816
NEXP = 8
D_FFE = 1408
ALPHA = (2 * DEPTH) ** 0.25
LN_EPS = 1e-5
GN_EPS = 64e-5
NEG = -1.0e30

ENGS = ['pe', 'act', 'dve', 'pool', 'sp']
BLK = {'pe': 'tensor', 'act': 'scalar', 'dve': 'vector', 'pool': 'gpsimd', 'sp': 'sync'}
NDSEM = 64
SAME_ENGINE_WAW = True
ARENA_BYTES = 204 * 1024


class Prog:
    def __init__(self, nc):
        self.nc = nc
        self.ops = []
        self.cnt = {e: 0 for e in ENGS}
        self.last_w = {}
        self.readers = {}
        self.known = {e: {} for e in ENGS}
        self.ndma = 0
        self.ndma_pool = 0
        self.dma_cnt = [0] * NDSEM
        self.nalloc = 0
        self.arena = None
        self.aoff = 0

    def sb(self, shape, dtype, name=None, persist=False):
        self.nalloc += 1
        if persist:
            return self.nc.alloc_sbuf_tensor(name or f"sb{self.nalloc}", list(shape), dtype)
        if self.arena is None:
            self.arena = self.nc.alloc_sbuf_tensor("arena", [128, ARENA_BYTES], mybir.dt.uint8)
            self.aoff = 0
        free = 1
        for d in shape[1:]:
            free *= d
        nb = free * (4 if dtype == F32 else 2)
        nb = (nb + 63) // 64 * 64
        assert self.aoff + nb <= ARENA_BYTES, f"arena overflow allocating {name} {shape}: {self.aoff}+{nb}"
        ap = self.arena[0:shape[0], self.aoff:self.aoff + free * (4 if dtype == F32 else 2)].bitcast(dtype)
        self.aoff += nb
        if len(shape) == 3:
            ap = ap.rearrange("p (a b) -> p a b", a=shape[1])
        elif len(shape) == 4:
            ap = ap.rearrange("p (a b c) -> p a b c", a=shape[1], b=shape[2])
        return ap

    def stage_begin(self):
        for e in ENGS:
            deps = set()
            for s in range(NDSEM):
                if self.dma_cnt[s]:
                    deps.add((('d', s), 16 * self.dma_cnt[s]))
            for o in ENGS:
                if o != e and self.cnt[o]:
                    deps.add((o, self.cnt[o]))
            waits = self._waits(e, deps)
            if waits:
                self.ops.append((e, None, waits, None, 0))
        self.aoff = 0

    def mark(self):
        return self.aoff

    def release(self, mark):
        off = mark
        self.stage_begin()
        self.aoff = off

    def ps(self, shape, dtype, name=None):
        self.nalloc += 1
        return self.nc.alloc_psum_tensor(name or f"ps{self.nalloc}", list(shape), dtype)

    def _deps(self, reads, writes, eng=None):
        deps = set()
        for k in reads:
            t = self.last_w.get(k)
            if t is not None and not (eng == 'pe' and t[0] == 'pe'):
                deps.add(t)
        for k in writes:
            t = self.last_w.get(k)
            if t is not None and (t[0] != eng or (SAME_ENGINE_WAW and eng != 'pe')):
                deps.add(t)
            for r in self.readers.get(k, ()):
                if not (eng == 'pe' and r[0] == 'pe'):
                    deps.add(r)
        return deps

    def _commit(self, tok, reads, writes):
        for k in reads:
            self.readers.setdefault(k, []).append(tok)
        for k in writes:
            self.last_w[k] = tok
            self.readers[k] = []

    def _waits(self, eng, deps):
        best = {}
        for (sk, v) in deps:
            if self.known[eng].get(sk, 0) >= v:
                continue
            if best.get(sk, 0) < v:
                best[sk] = v
        for sk, v in best.items():
            self.known[eng][sk] = v
        return list(best.items())

    def op(self, eng, fn, reads=(), writes=()):
        reads = tuple(reads)
        writes = tuple(writes)
        waits = self._waits(eng, self._deps(reads, writes, eng))
        self.cnt[eng] += 1
        tok = (eng, self.cnt[eng])
        self.ops.append((eng, fn, waits, eng, 1))
        self._commit(tok, reads, writes)
        return tok

    def dma(self, eng, out, in_, reads=(), writes=(), **kw):
        reads = tuple(reads)
        writes = tuple(writes)
        if eng == 'pool':
            slot = NDSEM // 2 + self.ndma_pool % (NDSEM // 2)
            self.ndma_pool += 1
        else:
            slot = self.ndma % (NDSEM // 2)
            self.ndma += 1
        deps = self._deps(reads, writes)
        if self.dma_cnt[slot] > 0:
            deps.add((('d', slot), 16 * self.dma_cnt[slot]))
        self.dma_cnt[slot] += 1
        tok = (('d', slot), 16 * self.dma_cnt[slot])
        waits = self._waits(eng, deps)
        self.ops.append((eng, lambda e: e.dma_start(out=out, in_=in_, **kw), waits, ('d', slot), 16))
        self._commit(tok, reads, writes)
        return tok

    def finish(self):
        deps = set()
        for s in range(NDSEM):
            if self.dma_cnt[s]:
                deps.add((('d', s), 16 * self.dma_cnt[s]))
        for e in ENGS:
            if e != 'sp' and self.cnt[e]:
                deps.add((e, self.cnt[e]))
        waits = self._waits('sp', deps)
        self.ops.append(('sp', None, waits, None, 0))

    def emit(self):
        nc = self.nc
        with ExitStack() as ctx:
            sems = {}
            for e in ENGS:
                sems[e] = ctx.enter_context(nc.semaphore(f"s_{e}"))
            for s in range(NDSEM):
                sems[('d', s)] = ctx.enter_context(nc.semaphore(f"s_d{s}"))
            block = ctx.enter_context(nc.Block())
            ops = self.ops

            def make(eng):
                def body(e):
                    for (oeng, fn, waits, isem, ival) in ops:
                        if oeng != eng:
                            continue
                        for (sk, v) in waits:
                            e.wait_ge(sems[sk], v)
                        if fn is None:
                            continue
                        fn(e).then_inc(sems[isem], ival)
                return body

            for eng in ENGS:
                getattr(block, BLK[eng])(make(eng))
        return nc


O_SWA, O_RW, O_FOX, O_DSA, O_GATE = 0, 768, 1792, 2564, 3272
H_COLS = ([O_SWA + 64 * h for h in range(8)] + [O_SWA + 512 + 64 * g for g in range(2)]
          + [O_FOX + 64 * h for h in range(4)] + [O_FOX + 256 + 64 * h for h in range(4)]
          + [O_DSA + 64 * h for h in range(4)] + [O_DSA + 256]
          + [O_DSA + 384 + 64 * h for h in range(4)] + [O_DSA + 640])
R_COLS = [O_RW + 64 * i for i in range(16)]
NSLAB = 44
T_COLS = (list(range(O_SWA + 640, O_SWA + 768)) + list(range(O_FOX + 512, O_FOX + 768))
          + list(range(O_DSA + 320, O_DSA + 384)) + list(range(O_FOX + 768, O_FOX + 772))
          + list(range(O_DSA + 704, O_DSA + 708)))
NTC = 456
HS_SWQ, HS_SWK, HS_FXQ, HS_FXK, HS_DSQ, HS_DSK, HS_DSQI, HS_DSKI = 0, 8, 10, 14, 18, 22, 23, 27


class Ctx:
    pass


def build_program(stages, dbg=(), layers=(0, 1)):
    nc = bass.Bass("TRN2", target_bir_lowering=False)
    P = Prog(nc)
    C = Ctx()
    C.nc, C.P = nc, P
    C.dbg = set(dbg)

    def ext_in(name, shape, dt=F32):
        return nc.dram_tensor(name, list(shape), dt, kind="ExternalInput").ap()

    def scratch(name, shape, dt):
        kind = "ExternalOutput" if name in C.dbg else "Internal"
        return nc.dram_tensor(name, list(shape), dt, kind=kind).ap()

    C.scratch = scratch
    C.x_in = ext_in("x", [S, D])
    C.consts = ext_in("consts", [128, 512])
    C.wA = ext_in("wA", [DEPTH, NSLAB // 2, 128, 8, 128])
    C.wT = ext_in("wT", [DEPTH, 128, 8, NTC])
    C.mu = ext_in("mu", [DEPTH, 128, 8])
    C.sinks = ext_in("sinks", [DEPTH, 8])
    C.fbias = ext_in("fbias", [DEPTH, 4])
    C.rwp = ext_in("rwp", [DEPTH, 64, 24])
    C.rwup = ext_in("rwup", [DEPTH, 64, 2, 256])
    C.rwgup = ext_in("rwgup", [DEPTH, 128, 256])
    C.rwgn = ext_in("rwgn", [DEPTH, 2, 256])
    C.cmask = ext_in("cmask", [128, 1024])
    C.wG = ext_in("wG", [DEPTH, 8, 128, 8, 4, 128])
    C.gbias = ext_in("gbias", [DEPTH, 128, 4, 8])
    C.w_branch = ext_in("w_branch", [DEPTH, 1280, D])
    C.w_out = ext_in("w_out", [DEPTH, D, D])
    C.ln_g = ext_in("ln_g", [DEPTH, 2, D])
    C.ln_b = ext_in("ln_b", [DEPTH, 2, D])
    C.w13d = ext_in("w13d", [D_FF // 128, 128, 8, 2, 128])
    C.w2d = ext_in("w2d", [D_FF // 128, 128, D])
    C.router_w = ext_in("router_w", [D, NEXP])
    C.router_b = ext_in("router_b", [NEXP])
    C.w13e = ext_in("w13e", [NEXP, D_FFE // 128, 128, 8, 2, 128])
    C.w2e = ext_in("w2e", [NEXP, D_FFE // 128, 128, D])
    C.xcur = scratch("xcur", [S, D], F32)
    C.out = nc.dram_tensor("out", [S, D], F32, kind="ExternalOutput").ap()

    C.cst = P.sb([128, 512], F32, "cst", persist=True)
    P.dma('sp', C.cst[:], C.consts, writes=['cst'])
    C.ident_bf = P.sb([128, 128], BF16, "ident_bf", persist=True)
    C.triu_bf = P.sb([128, 128], BF16, "triu_bf", persist=True)
    C.ones_bf = P.sb([128, 128], BF16, "ones_bf", persist=True)
    P.op('dve', lambda e: e.tensor_copy(out=C.ident_bf[:], in_=C.cst[:, 0:128]), reads=['cst'], writes=['ident_bf'])
    P.op('dve', lambda e: e.tensor_copy(out=C.triu_bf[:], in_=C.cst[:, 128:256]), reads=['cst'], writes=['triu_bf'])
    P.op('dve', lambda e: e.tensor_copy(out=C.ones_bf[:], in_=C.cst[:, 256:384]), reads=['cst'], writes=['ones_bf'])
    C.ident_f = C.cst[:, 0:128]
    C.triu_f = C.cst[:, 128:256]
    C.ones_f = C.cst[:, 256:384]
    C.pow2 = C.cst[:, 384:408]
    C.pb = [P.ps([128, 512], F32, f"pb{i}") for i in range(8)]

    for l in layers:
        L = Ctx()
        L.l = l
        L.xin = C.x_in if l == 0 else C.xcur
        L.headT = scratch(f"headT{l}", [28, 64, S], BF16)
        L.rwT = scratch(f"rwT{l}", [16, 64, S], F32)
        L.tokm = scratch(f"tokm{l}", [S, NTC], F32)
        L.xT = scratch(f"xT{l}", [128, 8, S], BF16)
        L.otok = scratch(f"otok{l}", [S, 1280], BF16)
        L.x1 = scratch(f"x1_{l}", [S, D], F32)
        L.xout = C.out if l == DEPTH - 1 else C.xcur
        if 'A' in stages:
            stage_A(C, L)
        if 'B' in stages:
            stage_swa(C, L)
        if 'C' in stages:
            stage_fox(C, L)
        if 'D' in stages:
            stage_dsa2(C, L)
        if 'd' in stages:
            stage_dsa(C, L)
        if 'E' in stages:
            stage_rwkv(C, L)
        if 'F' in stages:
            stage_merge(C, L)
        if 'G' in stages:
            if l % 2 == 0:
                stage_ffn_dense(C, L)
            else:
                stage_moe(C, L)
    P.finish()
    P.emit()
    return nc


def stage_A(C, L):
    C.P.stage_begin()
    P, l = C.P, L.l
    pb = C.pb
    xT = P.sb([128, 8, S], BF16, f"A_xT{l}")
    xs = [P.sb([128, D], F32, f"A_xs{l}_{i}") for i in range(2)]
    xb = [P.sb([128, D], BF16, f"A_xb{l}_{i}") for i in range(2)]
    for t in range(NT):
        i = t % 2
        P.dma('sp', xs[i][:], L.xin[t * 128:(t + 1) * 128, :], reads=['xin'], writes=[f'A_xs{i}'])
        P.op('dve', lambda e, i=i: e.tensor_copy(out=xb[i][:], in_=xs[i][:]), reads=[f'A_xs{i}'], writes=[f'A_xb{i}'])
        bank = pb[t % 2]
        pT = bank[:].bitcast(BF16)
        for kc in range(8):
            P.op('pe', lambda e, i=i, kc=kc, pT=pT: e.transpose(out=pT[:, kc * 128:(kc + 1) * 128], in_=xb[i][:, kc * 128:(kc + 1) * 128], identity=C.ident_bf[:]),
                 reads=[f'A_xb{i}', 'ident_bf'], writes=[f'pb{t % 2}'])
        P.op('act', lambda e, t=t, pT=pT: e.copy(out=xT[:, :, t * 128:(t + 1) * 128], in_=pT.rearrange("p (k c) -> p k c", k=8)),
             reads=[f'pb{t % 2}'], writes=['A_xT'])
    P.dma('act', L.xT, xT[:], reads=['A_xT'], writes=[f'xT{l}'])

    mu = P.sb([128, 8], F32, f"A_mu{l}")
    omu = P.sb([128, 8], F32, f"A_omu{l}")
    P.dma('sp', mu[:], C.mu[l], writes=['A_mu'])
    P.op('dve', lambda e: e.tensor_scalar(out=omu[:], in0=mu[:], scalar1=-1.0, scalar2=1.0, op0=ALU.mult, op1=ALU.add), reads=['A_mu'], writes=['A_omu'])

    wtf = P.sb([128, 8, NTC], F32, f"A_wtf{l}")
    wtb = P.sb([128, 8, NTC], BF16, f"A_wtb{l}")
    P.dma('sp', wtf[:], C.wT[l], writes=['A_wtf'])
    P.op('pool', lambda e: e.tensor_copy(out=wtb[:], in_=wtf[:]), reads=['A_wtf'], writes=['A_wtb'])
    tk = [P.sb([128, NTC], F32, f"A_tk{l}_{i}") for i in range(2)]
    for t in range(NT):
        b = 2 + t % 2
        for kc in range(8):
            P.op('pe', lambda e, t=t, kc=kc, b=b: e.matmul(pb[b][:, 0:NTC], lhsT=xT[:, kc, t * 128:(t + 1) * 128], rhs=wtb[:, kc, :], start=(kc == 0), stop=(kc == 7)),
                 reads=['A_xT', 'A_wtb'], writes=[f'pb{b}'])
        i = t % 2
        P.op('act', lambda e, b=b, i=i: e.copy(out=tk[i][:], in_=pb[b][:, 0:NTC]), reads=[f'pb{b}'], writes=[f'A_tk{i}'])
        P.dma('act', L.tokm[t * 128:(t + 1) * 128, :], tk[i][:], reads=[f'A_tk{i}'], writes=[f'tokm{l}'])

    wsf = [P.sb([128, 2, 8, 128], F32, f"A_wsf{l}_{i}") for i in range(2)]
    wsb = [P.sb([128, 2, 8, 128], BF16, f"A_wsb{l}_{i}") for i in range(2)]
    hsl = [P.sb([128, S], BF16, f"A_hsl{l}_{i}") for i in range(2)]
    pf = [P.sb([128, S + 1], F32, f"A_pf{l}_{i}") for i in range(2)]
    rtmp = [P.sb([128, S], F32, f"A_rtmp{l}_{i}") for i in range(2)]
    rout = [P.sb([128, S], F32, f"A_rout{l}_{i}") for i in range(2)]
    for i in range(2):
        P.op('pool', lambda e, i=i: e.memset(pf[i][:, 0:1], 0.0), writes=[f'A_pf{i}'])
    nb = 0
    NTILE = NSLAB // 2
    for g in range(NTILE // 2):
        gi = g % 2
        P.dma('sp', wsf[gi][:], C.wA[l, 2 * g:2 * g + 2].rearrange("s p k m -> p s k m"), writes=[f'A_wsf{gi}'])
        P.op('pool', lambda e, gi=gi: e.tensor_copy(out=wsb[gi][:], in_=wsf[gi][:]), reads=[f'A_wsf{gi}'], writes=[f'A_wsb{gi}'])
        for si in range(2):
            T = 2 * g + si
            i = T % 2
            for tb in range(4):
                b = 4 + nb % 4
                nb += 1
                for kc in range(8):
                    P.op('pe', lambda e, gi=gi, si=si, kc=kc, tb=tb, b=b: e.matmul(pb[b][:], lhsT=wsb[gi][:, si, kc, :], rhs=xT[:, kc, tb * 512:(tb + 1) * 512], start=(kc == 0), stop=(kc == 7)),
                         reads=['A_xT', f'A_wsb{gi}'], writes=[f'pb{b}'])
                if T < 14:
                    P.op('act', lambda e, i=i, tb=tb, b=b: e.copy(out=hsl[i][:, tb * 512:(tb + 1) * 512], in_=pb[b][:]), reads=[f'pb{b}'], writes=[f'A_hsl{i}'])
                else:
                    P.op('act', lambda e, i=i, tb=tb, b=b: e.copy(out=pf[i][:, 1 + tb * 512:1 + (tb + 1) * 512], in_=pb[b][:]), reads=[f'pb{b}'], writes=[f'A_pf{i}'])
            if T < 14:
                P.dma('act', L.headT[2 * T:2 * T + 2].rearrange("a p s -> (a p) s"), hsl[i][:], reads=[f'A_hsl{i}'], writes=[f'headT{l}'])
            else:
                r = T - 14
                P.op('dve', lambda e, i=i, r=r: e.tensor_scalar(out=rtmp[i][:], in0=pf[i][:, 0:S], scalar1=mu[:, r:r + 1], scalar2=None, op0=ALU.mult),
                     reads=[f'A_pf{i}', 'A_mu'], writes=[f'A_rtmp{i}'])
                P.op('dve', lambda e, i=i, r=r: e.scalar_tensor_tensor(out=rout[i][:], in0=pf[i][:, 1:S + 1], scalar=omu[:, r:r + 1], in1=rtmp[i][:], op0=ALU.mult, op1=ALU.add),
                     reads=[f'A_pf{i}', 'A_omu', f'A_rtmp{i}'], writes=[f'A_rout{i}'])
                P.dma('act', L.rwT[2 * r:2 * r + 2].rearrange("a p s -> (a p) s"), rout[i][:], reads=[f'A_rout{i}'], writes=[f'rwT{l}'])


def load_heads(C, L, name, s0, n):
    P = C.P
    t = P.sb([64, n, S], BF16, f"{name}{L.l}")
    P.dma('sp', t[:], L.headT[s0:s0 + n].rearrange("h p s -> p h s"), reads=[f'headT{L.l}'], writes=[name])
    return t


def load_vaug(C, L, name, c0, nh):
    P = C.P
    vf = P.sb([128, NT, nh * 64], F32, f"{name}f{L.l}")
    va = P.sb([128, NT, nh, 65], BF16, f"{name}a{L.l}")
    P.dma('sp', vf[:], L.tokm.rearrange("(t p) c -> p t c", p=128)[:, :, c0:c0 + nh * 64], reads=[f'tokm{L.l}'], writes=[name + 'f'])
    P.op('pool', lambda e: e.memset(va[:], 1.0), writes=[name])
    for h in range(nh):
        P.op('dve', lambda e, h=h: e.tensor_copy(out=va[:, :, h, 0:64], in_=vf[:, :, h * 64:(h + 1) * 64]), reads=[name + 'f', name], writes=[name])
    return va


def stage_swa(C, L):
    C.P.stage_begin()
    P, l = C.P, L.l
    pb = C.pb
    qT = load_heads(C, L, "sw_q", HS_SWQ, 8)
    kT = load_heads(C, L, "sw_k", HS_SWK, 2)
    va = load_vaug(C, L, "sw_v", 0, 2)
    esk = P.sb([128, 8], F32, f"sw_esk{l}")
    P.dma('sp', esk[:], C.sinks[l].partition_broadcast(128), writes=['sw_esk'])
    P.op('act', lambda e: e.activation(out=esk[:], in_=esk[:], func=AF.Exp), reads=['sw_esk'], writes=['sw_esk'])
    osw = P.sb([128, NT, 512], BF16, f"sw_o{l}")
    PA = [P.sb([128, 4, 128], BF16, f"sw_PA{l}_{i}") for i in range(2)]
    PB = [P.sb([128, 4, 128], BF16, f"sw_PB{l}_{i}") for i in range(2)]
    den = [P.sb([128, 4], F32, f"sw_den{l}_{i}") for i in range(2)]
    items = [(g, t) for g in range(2) for t in range(NT)]

    def qk(idx):
        g, t = items[idx]
        i = idx % 2
        bA, bB = pb[i], pb[2 + i]
        kA, kB = f'pb{i}', f'pb{2 + i}'
        rhs_q = qT[:, 4 * g:4 * g + 4, t * 128:(t + 1) * 128]
        if t > 0:
            P.op('pe', lambda e: e.matmul(bA[:].rearrange("p (h q) -> p h q", h=4), lhsT=kT[:, g, (t - 1) * 128:t * 128], rhs=rhs_q, start=True, stop=True),
                 reads=['sw_q', 'sw_k'], writes=[kA])
            P.op('act', lambda e: e.activation(out=PA[i][:], in_=bA[:].rearrange("p (h q) -> p h q", h=4), func=AF.Exp, scale=0.125), reads=[kA], writes=[f'sw_PA{i}'])
        P.op('pe', lambda e: e.matmul(bB[:].rearrange("p (h q) -> p h q", h=4), lhsT=kT[:, g, t * 128:(t + 1) * 128], rhs=rhs_q, start=True, stop=True),
             reads=['sw_q', 'sw_k'], writes=[kB])
        P.op('act', lambda e: e.activation(out=PB[i][:], in_=bB[:].rearrange("p (h q) -> p h q", h=4), func=AF.Exp, scale=0.125), reads=[kB], writes=[f'sw_PB{i}'])

    def pv(idx):
        g, t = items[idx]
        i = idx % 2
        bO, kO = pb[4 + i], f'pb{4 + i}'
        po = bO[:, 0:260].rearrange("p (h c) -> p h c", h=4)
        for hh in range(4):
            rd = ['sw_v', f'sw_PA{i}', f'sw_PB{i}']
            if t > 0:
                P.op('pe', lambda e, hh=hh: e.matmul(po[0:64, hh, :], lhsT=PA[i][:, hh, 0:64], rhs=va[:, t - 1, g, :], start=True, stop=False), reads=rd, writes=[kO])
                P.op('pe', lambda e, hh=hh: e.matmul(po[0:64, hh, :], lhsT=PB[i][0:64, hh, 0:64], rhs=va[0:64, t, g, :], start=False, stop=True), reads=rd, writes=[kO])
                P.op('pe', lambda e, hh=hh: e.matmul(po[64:128, hh, :], lhsT=PA[i][64:128, hh, 64:128], rhs=va[64:128, t - 1, g, :], start=True, stop=False), reads=rd, writes=[kO])
                P.op('pe', lambda e, hh=hh: e.matmul(po[64:128, hh, :], lhsT=PB[i][:, hh, 64:128], rhs=va[:, t, g, :], start=False, stop=True), reads=rd, writes=[kO])
            else:
                P.op('pe', lambda e, hh=hh: e.matmul(po[0:64, hh, :], lhsT=PB[i][0:64, hh, 0:64], rhs=va[0:64, t, g, :], start=True, stop=True), reads=rd, writes=[kO])
                P.op('pe', lambda e, hh=hh: e.matmul(po[64:128, hh, :], lhsT=PB[i][:, hh, 64:128], rhs=va[:, t, g, :], start=True, stop=True), reads=rd, writes=[kO])
        P.op('dve', lambda e: e.tensor_tensor(out=den[i][:], in0=po[:, :, 64], in1=esk[:, 4 * g:4 * g + 4], op=ALU.add), reads=[kO, 'sw_esk'], writes=[f'sw_den{i}'])
        P.op('dve', lambda e: e.reciprocal(out=den[i][:], in_=den[i][:]), reads=[f'sw_den{i}'], writes=[f'sw_den{i}'])
        for hh in range(4):
            P.op('dve', lambda e, hh=hh: e.tensor_scalar(out=osw[:, t, (4 * g + hh) * 64:(4 * g + hh + 1) * 64], in0=po[:, hh, 0:64], scalar1=den[i][:, hh:hh + 1], scalar2=None, op0=ALU.mult),
                 reads=[kO, f'sw_den{i}'], writes=['sw_o'])

    qk(0)
    for idx in range(len(items)):
        if idx + 1 < len(items):
            qk(idx + 1)
        pv(idx)
    P.dma('sp', L.otok.rearrange("(t p) c -> p t c", p=128)[:, :, 0:512], osw[:], reads=['sw_o'], writes=[f'otok{l}_sw'])


def stage_fox(C, L):
    C.P.stage_begin()
    P, l = C.P, L.l
    pb = C.pb
    qT = load_heads(C, L, "fx_q", HS_FXQ, 4)
    kT = load_heads(C, L, "fx_k", HS_FXK, 4)
    va = load_vaug(C, L, "fx_v", 128, 4)
    f = P.sb([128, NT, 4], F32, f"fx_f{l}")
    fb = P.sb([128, 4], F32, f"fx_fb{l}")
    P.dma('sp', f[:], L.tokm.rearrange("(t p) c -> p t c", p=128)[:, :, 448:452], reads=[f'tokm{l}'], writes=['fx_f'])
    P.dma('sp', fb[:], C.fbias[l].partition_broadcast(128), writes=['fx_fb'])
    P.op('dve', lambda e: e.tensor_tensor(out=f[:], in0=f[:], in1=fb[:].unsqueeze(1).to_broadcast([128, NT, 4]), op=ALU.add), reads=['fx_f', 'fx_fb'], writes=['fx_f'])
    P.op('act', lambda e: e.activation(out=f[:], in_=f[:], func=AF.Exp, scale=-1.0), reads=['fx_f'], writes=['fx_f'])
    P.op('act', lambda e: e.activation(out=f[:], in_=f[:], func=AF.Ln, bias=1.0), reads=['fx_f'], writes=['fx_f'])
    ff = f[:].rearrange("p t h -> p (t h)")
    P.op('pe', lambda e: e.matmul(pb[0][:, 0:64], lhsT=C.triu_f, rhs=ff, start=True, stop=True), reads=['fx_f', 'cst'], writes=['pb0'])
    P.op('pe', lambda e: e.matmul(pb[1][:, 0:64], lhsT=C.ones_f, rhs=ff, start=True, stop=True), reads=['fx_f', 'cst'], writes=['pb1'])
    tot = P.sb([128, NT, 4], F32, f"fx_tot{l}")
    pre = P.sb([128, NT, 4], F32, f"fx_pre{l}")
    cpos = P.sb([128, NT, 4], F32, f"fx_c{l}")
    P.op('dve', lambda e: e.tensor_copy(out=tot[:].rearrange("p t h -> p (t h)"), in_=pb[1][:, 0:64]), reads=['pb1'], writes=['fx_tot'])
    P.op('dve', lambda e: e.memset(pre[:, 0, :], 0.0), writes=['fx_pre'])
    for t in range(1, NT):
        P.op('dve', lambda e, t=t: e.tensor_tensor(out=pre[:, t, :], in0=pre[:, t - 1, :], in1=tot[:, t - 1, :], op=ALU.add), reads=['fx_pre', 'fx_tot'], writes=['fx_pre'])
    P.op('dve', lambda e: e.tensor_tensor(out=cpos[:].rearrange("p t h -> p (t h)"), in0=pb[0][:, 0:64], in1=pre[:].rearrange("p t h -> p (t h)"), op=ALU.add), reads=['pb0', 'fx_pre'], writes=['fx_c'])
    rrow = P.sb([1, 4, S], BF16, f"fx_rrow{l}")
    for h in range(4):
        for j in range(NT):
            P.op('dve', lambda e, h=h, j=j: e.tensor_scalar(out=rrow[0:1, h, j * 128:(j + 1) * 128], in0=C.ones_f[0:1, :], scalar1=pre[0:1, j, h:h + 1], scalar2=-8.0, op0=ALU.mult, op1=ALU.mult),
                 reads=['fx_pre', 'cst'], writes=['fx_rrow'])
    ofx = P.sb([128, NT, 256], BF16, f"fx_o{l}")
    PT = [P.sb([128, 512], BF16, f"fx_PT{l}_{i}") for i in range(2)]
    rec = P.sb([128, NT], F32, f"fx_rec{l}")
    accb = [pb[2], pb[3], pb[4]]
    acck = ['pb2', 'pb3', 'pb4']

    def acc_ap(j, lo, hi):
        return accb[j // 7][:, (j % 7) * 65 + lo:(j % 7) * 65 + hi]

    items = []
    for h in range(4):
        for kt in range(NT):
            q0 = kt * 128
            while q0 < S:
                n = min(512, S - q0)
                items.append((h, kt, q0, n))
                q0 += n

    def qk(idx):
        h, kt, q0, n = items[idx]
        i = idx % 2
        bs, ks = pb[5 + i], f'pb{5 + i}'
        P.op('pe', lambda e: e.matmul(bs[:, 0:n], lhsT=kT[:, h, kt * 128:(kt + 1) * 128], rhs=qT[:, h, q0:q0 + n], start=True, stop=False),
             reads=['fx_q', 'fx_k'], writes=[ks])
        P.op('pe', lambda e: e.matmul(bs[:, 0:n], lhsT=C.ones_bf[0:1, :], rhs=rrow[0:1, h, q0:q0 + n], start=False, stop=True),
             reads=['fx_rrow', 'ones_bf'], writes=[ks])
        P.op('act', lambda e: e.activation(out=PT[i][:, 0:n], in_=bs[:, 0:n], func=AF.Exp, scale=0.125, bias=cpos[:, kt, h:h + 1]),
             reads=[ks, 'fx_c'], writes=[f'fx_PT{i}'])
        if q0 == kt * 128:
            P.op('dve', lambda e: e.tensor_tensor(out=PT[i][:, 0:128], in0=PT[i][:, 0:128], in1=C.triu_bf[:], op=ALU.mult), reads=[f'fx_PT{i}', 'triu_bf'], writes=[f'fx_PT{i}'])

    def pv(idx):
        h, kt, q0, n = items[idx]
        i = idx % 2
        if kt == 0 and q0 == 0:
            for bi in range(3):
                P.op('dve', lambda e, bi=bi: e.memset(accb[bi][:], 0.0), writes=[acck[bi]])
        for jj in range(n // 128):
            j = q0 // 128 + jj
            P.op('pe', lambda e, jj=jj, j=j: e.matmul(acc_ap(j, 0, 65), lhsT=PT[i][:, jj * 128:(jj + 1) * 128], rhs=va[:, kt, h, :], start=False, stop=(kt == j), skip_group_check=True),
                 reads=[f'fx_PT{i}', 'fx_v'], writes=[acck[j // 7]])
        if kt == NT - 1:
            for j in range(NT):
                P.op('dve', lambda e, j=j: e.reciprocal(out=rec[:, j:j + 1], in_=acc_ap(j, 64, 65)), reads=[acck[j // 7]], writes=['fx_rec'])
                P.op('dve', lambda e, j=j: e.tensor_scalar(out=ofx[:, j, h * 64:(h + 1) * 64], in0=acc_ap(j, 0, 64), scalar1=rec[:, j:j + 1], scalar2=None, op0=ALU.mult),
                     reads=[acck[j // 7], 'fx_rec'], writes=['fx_o'])

    qk(0)
    for idx in range(len(items)):
        if idx + 1 < len(items):
            qk(idx + 1)
        pv(idx)
    P.dma('sp', L.otok.rearrange("(t p) c -> p t c", p=128)[:, :, 768:1024], ofx[:], reads=['fx_o'], writes=[f'otok{l}_fx'])


NBIS = 16


def stage_dsa(C, L):
    C.P.stage_begin()
    P, l = C.P, L.l
    pb = C.pb
    qT = load_heads(C, L, "ds_q", HS_DSQ, 4)
    kT = load_heads(C, L, "ds_k", HS_DSK, 1)
    qiT = load_heads(C, L, "ds_qi", HS_DSQI, 4)
    kiT = load_heads(C, L, "ds_ki", HS_DSKI, 1)
    va = load_vaug(C, L, "ds_v", 384, 1)
    wi = P.sb([128, NT, 4], F32, f"ds_wi{l}")
    P.dma('sp', wi[:], L.tokm.rearrange("(t p) c -> p t c", p=128)[:, :, 452:456], reads=[f'tokm{l}'], writes=['ds_wi'])
    sc = [P.sb([128, S], F32, f"ds_sc{l}_{i}") for i in range(2)]
    rl = [P.sb([128, 512], F32, f"ds_rl{l}_{i}") for i in range(4)]
    junk = P.sb([128, S], BF16, f"ds_junk{l}")
    tA = P.sb([128, S], F32, f"ds_tA{l}")
    tE = P.sb([128, S], F32, f"ds_tE{l}")
    mask = [P.sb([128, S], BF16, f"ds_mask{l}_{i}") for i in range(2)]
    maskT = [P.sb([128, NT, 128], BF16, f"ds_maskT{l}_{i}") for i in range(2)]
    PT = [P.sb([128, 4, 128], BF16, f"ds_PT{l}_{i}") for i in range(2)]
    sm = [P.sb([128, 8], F32, f"ds_sm{l}_{i}") for i in range(2)]
    wtab = [P.sb([128, NBIS], F32, f"ds_wtab{l}_{i}") for i in range(2)]
    rec = P.sb([128, 4], F32, f"ds_rec{l}")
    ods = P.sb([128, NT, 256], BF16, f"ds_o{l}")
    it = 0
    nr = 0
    if 'dsdbg' in C.dbg:
        C.dsdbg = C.scratch("dsdbg", [NT, 128, 8], F32)
    for j in range(NT):
        ji = j % 2
        n = (j + 1) * 128
        scj, ksc = sc[ji], f'ds_sc{ji}'
        for c0 in range(0, n, 512):
            nn = min(512, n - c0)
            for h in range(4):
                P.op('pe', lambda e, h=h, j=j, c0=c0, nn=nn: e.matmul(pb[h][:, 0:nn], lhsT=qiT[:, h, j * 128:(j + 1) * 128], rhs=kiT[:, 0, c0:c0 + nn], start=True, stop=True),
                     reads=['ds_qi', 'ds_ki'], writes=[f'pb{h}'])
                ri = nr % 4
                nr += 1
                P.op('act', lambda e, h=h, ri=ri, nn=nn: e.activation(out=rl[ri][:, 0:nn], in_=pb[h][:, 0:nn], func=AF.Relu), reads=[f'pb{h}'], writes=[f'ds_rl{ri}'])
                if h == 0:
                    P.op('dve', lambda e, ri=ri, nn=nn, c0=c0, j=j, scj=scj: e.tensor_scalar(out=scj[:, c0:c0 + nn], in0=rl[ri][:, 0:nn], scalar1=wi[:, j, 0:1], scalar2=None, op0=ALU.mult),
                         reads=[f'ds_rl{ri}', 'ds_wi'], writes=[ksc])
                else:
                    P.op('dve', lambda e, ri=ri, nn=nn, c0=c0, j=j, h=h, scj=scj: e.scalar_tensor_tensor(out=scj[:, c0:c0 + nn], in0=rl[ri][:, 0:nn], scalar=wi[:, j, h:h + 1], in1=scj[:, c0:c0 + nn], op0=ALU.mult, op1=ALU.add),
                         reads=[f'ds_rl{ri}', 'ds_wi', ksc], writes=[ksc])
        mk, kmk = mask[ji], f'ds_mask{ji}'
        smj, ksm = sm[ji], f'ds_sm{ji}'
        if j >= 2:
            P.op('dve', lambda e, scj=scj, smj=smj, n=n: e.tensor_reduce(out=smj[:, 0:1], in_=scj[:, 0:n - 64], axis=AX.X, op=ALU.min), reads=[ksc], writes=[ksm])
        P.op('dve', lambda e, scj=scj, n=n: e.memset(scj[0:64, n - 64:n], NEG), reads=[ksc], writes=[ksc])
        if j >= 2:
            P.op('dve', lambda e, scj=scj, smj=smj, n=n: e.tensor_reduce(out=smj[:, 5:6], in_=scj[:, 0:n], axis=AX.X, op=ALU.max), reads=[ksc], writes=[ksm])
            P.op('dve', lambda e, smj=smj: e.tensor_tensor(out=smj[:, 1:2], in0=smj[:, 5:6], in1=smj[:, 0:1], op=ALU.subtract), reads=[ksm], writes=[ksm])
            wt, kwt = wtab[ji], f'ds_wtab{ji}'
            P.op('dve', lambda e, wt=wt, smj=smj: e.tensor_scalar(out=wt[:], in0=C.pow2, scalar1=smj[:, 1:2], scalar2=None, op0=ALU.mult), reads=[ksm, 'cst'], writes=[kwt])
            for b in range(NBIS):
                P.op('dve', lambda e, smj=smj, wt=wt, b=b: e.tensor_tensor(out=smj[:, 2:3], in0=smj[:, 0:1], in1=wt[:, b:b + 1], op=ALU.add), reads=[ksm, kwt], writes=[ksm])
                P.op('dve', lambda e, smj=smj, scj=scj, n=n: e.tensor_scalar(out=junk[:, 0:n], in0=scj[:, 0:n], scalar1=smj[:, 2:3], scalar2=None, op0=ALU.is_ge, op1=ALU.add, accum_out=smj[:, 3:4]),
                     reads=[ksm, ksc], writes=[ksm, 'ds_junk'])
                P.op('dve', lambda e, smj=smj, wt=wt, b=b: e.tensor_scalar(out=smj[:, 4:5], in0=smj[:, 3:4], scalar1=255.5, scalar2=wt[:, b:b + 1], op0=ALU.is_ge, op1=ALU.mult), reads=[ksm, kwt], writes=[ksm])
                P.op('dve', lambda e, smj=smj: e.tensor_tensor(out=smj[:, 0:1], in0=smj[:, 0:1], in1=smj[:, 4:5], op=ALU.add), reads=[ksm], writes=[ksm])
            P.op('dve', lambda e, scj=scj, smj=smj, n=n: e.tensor_scalar(out=tA[:, 0:n], in0=scj[:, 0:n], scalar1=smj[:, 0:1], scalar2=1.0e37, op0=ALU.is_lt, op1=ALU.mult), reads=[ksc, ksm], writes=['ds_tA'])
            P.op('dve', lambda e, scj=scj, n=n: e.tensor_tensor(out=tA[:, 0:n], in0=tA[:, 0:n], in1=scj[:, 0:n], op=ALU.add), reads=[ksc, 'ds_tA'], writes=['ds_tA'])
            P.op('dve', lambda e, smj=smj, n=n: e.tensor_reduce(out=smj[:, 6:7], in_=tA[:, 0:n], axis=AX.X, op=ALU.min), reads=['ds_tA'], writes=[ksm])
            P.op('dve', lambda e, mk=mk, scj=scj, smj=smj, n=n: e.tensor_scalar(out=mk[:, 0:n], in0=scj[:, 0:n], scalar1=smj[:, 6:7], scalar2=None, op0=ALU.is_gt, op1=ALU.add, accum_out=smj[:, 7:8]), reads=[ksc, ksm], writes=[kmk, ksm])
            P.op('dve', lambda e, smj=smj: e.tensor_scalar(out=smj[:, 7:8], in0=smj[:, 7:8], scalar1=-1.0, scalar2=256.0, op0=ALU.mult, op1=ALU.add), reads=[ksm], writes=[ksm])
            P.op('dve', lambda e, scj=scj, smj=smj, n=n: e.tensor_scalar(out=tE[:, 0:n], in0=scj[:, 0:n], scalar1=smj[:, 6:7], scalar2=None, op0=ALU.is_equal), reads=[ksc, ksm], writes=['ds_tE'])
            P.op('dve', lambda e, n=n: e.tensor_tensor_scan(out=tA[:, 0:n], data0=C.ones_f[:, 0:1].to_broadcast([128, n]), data1=tE[:, 0:n], initial=0.0, op0=ALU.mult, op1=ALU.add), reads=['ds_tE', 'cst'], writes=['ds_tA'])
            P.op('dve', lambda e, smj=smj, n=n: e.scalar_tensor_tensor(out=tA[:, 0:n], in0=tA[:, 0:n], scalar=smj[:, 7:8], in1=tE[:, 0:n], op0=ALU.is_le, op1=ALU.mult), reads=['ds_tA', 'ds_tE', ksm], writes=['ds_tA'])
            P.op('dve', lambda e, mk=mk, n=n: e.tensor_tensor(out=mk[:, 0:n], in0=mk[:, 0:n], in1=tA[:, 0:n], op=ALU.add), reads=[kmk, 'ds_tA'], writes=[kmk])
        else:
            P.op('dve', lambda e, mk=mk, scj=scj, n=n: e.tensor_scalar(out=mk[:, 0:n], in0=scj[:, 0:n], scalar1=-1.0e29, scalar2=None, op0=ALU.is_ge), reads=[ksc], writes=[kmk])
        if 'dsdbg' in C.dbg:
            P.dma('sp', C.dsdbg[j], smj[:], reads=[ksm], writes=['dsdbg'])
        mT, kmT = maskT[ji], f'ds_maskT{ji}'
        for k0 in range(0, j + 1, 8):
            k1 = min(j + 1, k0 + 8)
            pT = pb[4][:].bitcast(BF16)
            for kt in range(k0, k1):
                P.op('pe', lambda e, pT=pT, mk=mk, kt=kt, k0=k0: e.transpose(out=pT[:, (kt - k0) * 128:(kt - k0 + 1) * 128], in_=mk[:, kt * 128:(kt + 1) * 128], identity=C.ident_bf[:]),
                     reads=[kmk, 'ident_bf'], writes=['pb4'])
            P.op('act', lambda e, pT=pT, mT=mT, k0=k0, k1=k1: e.copy(out=mT[:, k0:k1, :], in_=pT[:, 0:(k1 - k0) * 128].rearrange("p (k c) -> p k c", c=128)), reads=['pb4'], writes=[kmT])
        po = pb[7][:, 0:260].rearrange("p (h c) -> p h c", h=4)
        P.op('dve', lambda e: e.memset(pb[7][:, 0:260], 0.0), writes=['pb7'])
        for kt in range(j + 1):
            i = it % 2
            it += 1
            bs, ks = pb[5 + i], f'pb{5 + i}'
            P.op('pe', lambda e, bs=bs, kt=kt, j=j: e.matmul(bs[:].rearrange("p (h q) -> p h q", h=4), lhsT=kT[:, 0, kt * 128:(kt + 1) * 128], rhs=qT[:, :, j * 128:(j + 1) * 128], start=True, stop=True),
                 reads=['ds_q', 'ds_k'], writes=[ks])
            P.op('act', lambda e, bs=bs, i=i: e.activation(out=PT[i][:], in_=bs[:].rearrange("p (h q) -> p h q", h=4), func=AF.Exp, scale=0.125), reads=[ks], writes=[f'ds_PT{i}'])
            P.op('dve', lambda e, i=i, mT=mT, kt=kt: e.tensor_tensor(out=PT[i][:], in0=PT[i][:], in1=mT[:, kt, :].unsqueeze(1).to_broadcast([128, 4, 128]), op=ALU.mult), reads=[f'ds_PT{i}', kmT], writes=[f'ds_PT{i}'])
            for h in range(4):
                P.op('pe', lambda e, po=po, i=i, h=h, kt=kt, j=j: e.matmul(po[:, h, :], lhsT=PT[i][:, h, :], rhs=va[:, kt, 0, :], start=False, stop=(kt == j), skip_group_check=True),
                     reads=[f'ds_PT{i}', 'ds_v'], writes=['pb7'])
        P.op('dve', lambda e, po=po: e.reciprocal(out=rec[:], in_=po[:, :, 64]), reads=['pb7'], writes=['ds_rec'])
        for h in range(4):
            P.op('dve', lambda e, po=po, h=h, j=j: e.tensor_scalar(out=ods[:, j, h * 64:(h + 1) * 64], in0=po[:, h, 0:64], scalar1=rec[:, h:h + 1], scalar2=None, op0=ALU.mult), reads=['pb7', 'ds_rec'], writes=['ds_o'])
    P.dma('sp', L.otok.rearrange("(t p) c -> p t c", p=128)[:, :, 1024:1280], ods[:], reads=['ds_o'], writes=[f'otok{l}_ds'])


def make_consts():
    c = np.zeros((128, 512), np.float32)
    c[:, 0:128] = np.eye(128, dtype=np.float32)
    c[:, 128:256] = np.triu(np.ones((128, 128), np.float32))
    c[:, 256:384] = 1.0
    c[:, 384:408] = (0.5 ** np.arange(1, 25, dtype=np.float64)).astype(np.float32)[None, :]
    return c


def prep_shared(inp):
    w_in = np.asarray(inp['w_in'])
    sh = {}
    wA = np.empty((DEPTH, NSLAB // 2, 128, 8, 128), np.float32)
    wT = np.empty((DEPTH, 128, 8, NTC), np.float32)
    cols = H_COLS + R_COLS
    for l in range(DEPTH):
        w = w_in[l].reshape(8, 128, D_IN)
        for s, c0 in enumerate(cols):
            wA[l, s // 2, :, :, (s % 2) * 64:(s % 2) * 64 + 64] = w[:, :, c0:c0 + 64].transpose(1, 0, 2)
        wT[l] = w[:, :, T_COLS].transpose(1, 0, 2)
    sh['wA'] = wA
    sh['wT'] = wT
    sh['mu'] = np.ascontiguousarray(np.asarray(inp['rwkv_mu']).reshape(DEPTH, 8, 128).transpose(0, 2, 1))
    sh['sinks'] = np.asarray(inp['swa_sinks'])
    sh['fbias'] = np.asarray(inp['fox_f_bias'])
    sh['consts'] = make_consts()
    rwp = np.zeros((DEPTH, 64, 24), np.float32)
    for i, nm in enumerate(['rwkv_w0', 'rwkv_a0', 'rwkv_k_k', 'rwkv_k_a']):
        rwp[:, :, 4 * i:4 * i + 4] = np.asarray(inp[nm]).reshape(DEPTH, 4, 64).transpose(0, 2, 1)
    rwp[:, :, 16:20] = np.asarray(inp['rwkv_r_k']).transpose(0, 2, 1)
    sh['rwp'] = rwp
    sh['rwup'] = np.ascontiguousarray(np.stack([np.asarray(inp['rwkv_w_up']), np.asarray(inp['rwkv_a_up'])], axis=2))
    sh['rwgup'] = np.asarray(inp['rwkv_g_up'])
    sh['rwgn'] = np.ascontiguousarray(np.stack([np.asarray(inp['rwkv_gn_g']), np.asarray(inp['rwkv_gn_b'])], axis=1))
    cm = np.ones((128, 1024), np.float32)
    cm[:, ::64] = 0.0
    sh['cmask'] = cm
    wG = np.empty((DEPTH, 8, 128, 8, 4, 128), np.float32)
    for l in range(DEPTH):
        g = w_in[l][:, O_GATE:].reshape(8, 128, 4, 8, 128)
        wG[l] = g.transpose(3, 1, 0, 2, 4)
    sh['wG'] = wG
    sh['gbias'] = np.ascontiguousarray(np.asarray(inp['gate_bias']).reshape(DEPTH, 4, 8, 128).transpose(0, 3, 1, 2))
    for nm in ['w_branch', 'w_out', 'ln_g', 'ln_b']:
        sh[nm] = np.asarray(inp[nm])
    nf = D_FF // 128
    w1 = np.asarray(inp['ffn_w1'])[0].reshape(8, 128, nf, 128)
    w3 = np.asarray(inp['ffn_w3'])[0].reshape(8, 128, nf, 128)
    sh['w13d'] = np.ascontiguousarray(np.stack([w1, w3], axis=3).transpose(2, 1, 0, 3, 4))
    sh['w2d'] = np.asarray(inp['ffn_w2'])[0].reshape(nf, 128, D)
    sh['router_w'] = np.asarray(inp['router_w'])[0]
    sh['router_b'] = np.asarray(inp['router_b'])[0]
    nfe = D_FFE // 128
    e1 = np.asarray(inp['exp_w1'])[0].reshape(NEXP, 8, 128, nfe, 128)
    e3 = np.asarray(inp['exp_w3'])[0].reshape(NEXP, 8, 128, nfe, 128)
    sh['w13e'] = np.ascontiguousarray(np.stack([e1, e3], axis=4).transpose(0, 3, 2, 1, 4, 5))
    sh['w2e'] = np.asarray(inp['exp_w2'])[0].reshape(NEXP, nfe, 128, D)
    return sh


C0 = float(np.exp(-0.5))
HS2 = 1024
NCH = 16
RW_SCAN_CHUNKS = NCH


def stage_rwkv(C, L):
    P, l = C.P, L.l
    P.stage_begin()
    pb = C.pb
    rwp = P.sb([64, 24], F32, f"rw_p{l}")
    P.dma('sp', rwp[:], C.rwp[l], writes=['rw_p'])
    P.op('dve', lambda e: e.tensor_scalar(out=rwp[:, 20:24], in0=rwp[:, 12:16], scalar1=-1.0, scalar2=1.0, op0=ALU.mult, op1=ALU.add), reads=['rw_p'], writes=['rw_p'])
    upf = P.sb([64, 2, 256], F32, f"rw_upf{l}")
    upb = P.sb([64, 2, 256], BF16, f"rw_upb{l}")
    P.dma('sp', upf[:], C.rwup[l], writes=['rw_upf'])
    P.op('dve', lambda e: e.tensor_copy(out=upb[:], in_=upf[:]), reads=['rw_upf'], writes=['rw_upb'])
    guf = P.sb([128, 256], F32, f"rw_guf{l}")
    gub = P.sb([128, 256], BF16, f"rw_gub{l}")
    P.dma('sp', guf[:], C.rwgup[l], writes=['rw_guf'])
    P.op('dve', lambda e: e.tensor_copy(out=gub[:], in_=guf[:]), reads=['rw_guf'], writes=['rw_gub'])
    gng = P.sb([64, 256], F32, f"rw_gng{l}")
    gnb = P.sb([64, 256], F32, f"rw_gnb{l}")
    P.dma('sp', gng[:], C.rwgn[l, 0].partition_broadcast(64), writes=['rw_gng'])
    P.dma('sp', gnb[:], C.rwgn[l, 1].partition_broadcast(64), writes=['rw_gnb'])
    eps12 = P.sb([64, 2], F32, f"rw_eps{l}")
    P.op('dve', lambda e: e.memset(eps12[:], 1e-12), writes=['rw_eps'])
    cm = P.sb([64, HS2], F32, f"rw_cm{l}")
    P.dma('sp', cm[:], C.cmask[0:64, :], writes=['rw_cm'])
    sup = P.sb([64, 64], F32, f"rw_sup{l}")
    slo = P.sb([64, 64], F32, f"rw_slo{l}")
    P.op('dve', lambda e: e.tensor_tensor(out=sup[:], in0=C.triu_f[0:64, 0:64], in1=C.ident_f[0:64, 0:64], op=ALU.subtract), reads=['cst'], writes=['rw_sup'])
    P.op('dve', lambda e: e.tensor_scalar(out=slo[:], in0=C.triu_f[0:64, 0:64], scalar1=-1.0, scalar2=1.0, op0=ALU.mult, op1=ALU.add), reads=['cst'], writes=['rw_slo'])

    AT = P.sb([64, 4, HS2], BF16, f"rw_AT{l}")
    RT = P.sb([64, 4, HS2], BF16, f"rw_RT{l}")
    BT = P.sb([64, 4, HS2], BF16, f"rw_BT{l}")
    KT = P.sb([64, 4, HS2], BF16, f"rw_KT{l}")
    Vt = P.sb([64, 4, NCH, 64], BF16, f"rw_Vt{l}")
    Bh = P.sb([64, 4, NCH, 64], BF16, f"rw_Bh{l}")
    Kh = P.sb([64, 4, NCH, 64], BF16, f"rw_Kh{l}")
    TT = P.sb([64, 4, NCH, 64], BF16, f"rw_TT{l}")
    AakT = P.sb([64, 4, NCH, 64], BF16, f"rw_Aak{l}")
    ArbT = P.sb([64, 4, NCH, 64], BF16, f"rw_Arb{l}")
    ArkT = P.sb([64, 4, NCH, 64], BF16, f"rw_Ark{l}")
    WC = P.sb([64, 4, NCH], F32, f"rw_WC{l}")
    rkb = P.sb([64, NCH, 4], F32, f"rw_rkb{l}")
    ST = P.sb([64, 4, 64], F32, f"rw_ST{l}")
    STb = P.sb([64, 4, 64], BF16, f"rw_STb{l}")
    STw = P.sb([64, 4, 64], F32, f"rw_STw{l}")
    P.op('dve', lambda e: e.memset(ST[:], 0.0), writes=['rw_ST'])
    P.op('dve', lambda e: e.memset(STb[:], 0.0), writes=['rw_STb'])
    wlo = P.sb([64, HS2], F32, f"rw_wlo{l}")
    alo = P.sb([64, HS2], F32, f"rw_alo{l}")
    glo = P.sb([128, HS2], F32, f"rw_glo{l}")
    tw = P.sb([64, HS2], BF16, f"rw_tw{l}")
    alb = P.sb([64, HS2], BF16, f"rw_alb{l}")
    sg = P.sb([128, HS2], BF16, f"rw_sg{l}")
    tr = P.sb([64, HS2], F32, f"rw_tr{l}")
    tk = P.sb([64, HS2], F32, f"rw_tk{l}")
    tv = P.sb([64, HS2], F32, f"rw_tv{l}")
    cs = P.sb([64, HS2], F32, f"rw_cs{l}")
    sig = P.sb([64, HS2], F32, f"rw_sig{l}")
    ex = P.sb([64, HS2], F32, f"rw_ex{l}")
    ta = P.sb([64, HS2], F32, f"rw_ta{l}")
    kk = P.sb([64, HS2], F32, f"rw_kk{l}")
    kp = P.sb([64, HS2], F32, f"rw_kp{l}")
    tb = P.sb([64, HS2], F32, f"rw_tb{l}")
    t1 = P.sb([64, HS2], F32, f"rw_t1{l}")
    t2b = P.sb([64, HS2], BF16, f"rw_t2b{l}")
    Nb = [[P.sb([64, 8, 64], BF16, f"rw_Nb{l}_{g}{i}") for i in range(2)] for g in range(2)]
    Mb = [[P.sb([64, 8, 64], BF16, f"rw_Mb{l}_{g}{i}") for i in range(2)] for g in range(2)]
    Pb = [[P.sb([64, 8, 64], BF16, f"rw_Pb{l}_{g}{i}") for i in range(2)] for g in range(2)]
    Upb = P.sb([64, 4, 64], BF16, f"rw_Upb{l}")
    Ub = P.sb([64, 4, 64], BF16, f"rw_Ub{l}")
    Y = [P.sb([64, 4, 64], F32, f"rw_Y{l}_{i}") for i in range(2)]
    yc = P.sb([64, 4, 64], F32, f"rw_yc{l}")
    ysq = P.sb([64, 4, 64], F32, f"rw_ysq{l}")
    st4 = P.sb([64, 8], F32, f"rw_st4{l}")
    gt = P.sb([64, 256], F32, f"rw_gt{l}")
    ob = [P.sb([64, 256], BF16, f"rw_ob{l}_{i}") for i in range(2)]

    def bc3(ap2, n1, n2):
        return ap2.unsqueeze(2).to_broadcast([64, n1, n2])

    def mbc(m):
        return m.unsqueeze(1).to_broadcast([64, 8, 64])

    for half in range(2):
        t0 = half * HS2
        P.dma('sp', wlo[:], L.rwT[12, :, t0:t0 + HS2], reads=[f'rwT{l}'], writes=['rw_wlo'])
        P.dma('sp', alo[:], L.rwT[13, :, t0:t0 + HS2], reads=[f'rwT{l}'], writes=['rw_alo'])
        P.dma('sp', glo[:], L.rwT[14:16, :, t0:t0 + HS2].rearrange("a p s -> (a p) s"), reads=[f'rwT{l}'], writes=['rw_glo'])
        P.op('act', lambda e: e.activation(out=tw[:], in_=wlo[:], func=AF.Tanh), reads=['rw_wlo'], writes=['rw_tw'])
        P.op('act', lambda e: e.copy(out=alb[:], in_=alo[:]), reads=['rw_alo'], writes=['rw_alb'])
        P.op('act', lambda e: e.activation(out=sg[:], in_=glo[:], func=AF.Sigmoid), reads=['rw_glo'], writes=['rw_sg'])
        for h in range(4):
            P.dma('sp', tr[:], L.rwT[h, :, t0:t0 + HS2], reads=[f'rwT{l}'], writes=['rw_tr'])
            P.dma('sp', tk[:], L.rwT[4 + h, :, t0:t0 + HS2], reads=[f'rwT{l}'], writes=['rw_tk'])
            P.dma('sp', tv[:], L.rwT[8 + h, :, t0:t0 + HS2], reads=[f'rwT{l}'], writes=['rw_tv'])
            for blk in range(2):
                P.op('pe', lambda e, h=h, blk=blk: e.matmul(pb[blk][0:64, :], lhsT=upb[:, 0, h * 64:(h + 1) * 64], rhs=tw[:, blk * 512:(blk + 1) * 512], start=True, stop=True), reads=['rw_upb', 'rw_tw'], writes=[f'pb{blk}'])
                P.op('act', lambda e, h=h, blk=blk: e.activation(out=sig[:, blk * 512:(blk + 1) * 512], in_=pb[blk][0:64, :], func=AF.Sigmoid, bias=rwp[:, h:h + 1]), reads=[f'pb{blk}', 'rw_p'], writes=['rw_sig'])
            P.op('dve', lambda e: e.tensor_tensor_scan(out=cs[:], data0=cm[:], data1=sig[:], initial=0.0, op0=ALU.mult, op1=ALU.add), reads=['rw_cm', 'rw_sig'], writes=['rw_cs'])
            for blk in range(2):
                P.op('pe', lambda e, h=h, blk=blk: e.matmul(pb[2 + blk][0:64, :], lhsT=upb[:, 1, h * 64:(h + 1) * 64], rhs=alb[:, blk * 512:(blk + 1) * 512], start=True, stop=True), reads=['rw_upb', 'rw_alb'], writes=[f'pb{2 + blk}'])
                P.op('act', lambda e, h=h, blk=blk: e.activation(out=ta[:, blk * 512:(blk + 1) * 512], in_=pb[2 + blk][0:64, :], func=AF.Sigmoid, bias=rwp[:, 4 + h:5 + h]), reads=[f'pb{2 + blk}', 'rw_p'], writes=['rw_ta'])
            P.op('act', lambda e, h=h: e.activation(out=kk[:], in_=tk[:], func=AF.Copy, scale=rwp[:, 8 + h:9 + h]), reads=['rw_tk', 'rw_p'], writes=['rw_kk'])
            P.op('act', lambda e: e.activation(out=t1[:], in_=kk[:], func=AF.Square), reads=['rw_kk'], writes=['rw_t1'])
            for blk in range(2):
                P.op('pe', lambda e, blk=blk: e.matmul(pb[4 + blk][0:64, :], lhsT=C.ones_f[0:64, 0:64], rhs=t1[:, blk * 512:(blk + 1) * 512], start=True, stop=True), reads=['cst', 'rw_t1'], writes=[f'pb{4 + blk}'])
                P.op('act', lambda e, blk=blk: e.activation(out=tb[:, blk * 512:(blk + 1) * 512], in_=pb[4 + blk][0:64, :], func=AF.Sqrt, bias=eps12[:, 0:1]), reads=[f'pb{4 + blk}', 'rw_eps'], writes=['rw_tb'])
            P.op('dve', lambda e: e.reciprocal(out=tb[:], in_=tb[:]), reads=['rw_tb'], writes=['rw_tb'])
            P.op('dve', lambda e: e.tensor_tensor(out=kk[:], in0=kk[:], in1=tb[:], op=ALU.mult), reads=['rw_kk', 'rw_tb'], writes=['rw_kk'])
            P.op('act', lambda e, h=h: e.activation(out=kp[:], in_=ta[:], func=AF.Identity, scale=rwp[:, 12 + h:13 + h], bias=rwp[:, 20 + h:21 + h]), reads=['rw_ta', 'rw_p'], writes=['rw_kp'])
            P.op('dve', lambda e: e.tensor_tensor(out=kp[:], in0=kp[:], in1=tk[:], op=ALU.mult), reads=['rw_kp', 'rw_tk'], writes=['rw_kp'])
            P.op('dve', lambda e: e.tensor_tensor(out=tb[:], in0=kk[:], in1=ta[:], op=ALU.mult), reads=['rw_kk', 'rw_ta'], writes=['rw_tb'])
            P.op('dve', lambda e, h=h: e.scalar_tensor_tensor(out=t1[:], in0=tr[:], scalar=rwp[:, 16 + h:17 + h], in1=kp[:], op0=ALU.mult, op1=ALU.mult), reads=['rw_tr', 'rw_kp', 'rw_p'], writes=['rw_t1'])
            for c in range(NCH):
                P.op('pe', lambda e, c=c, h=h: e.matmul(pb[6][0:64, c * 4 + h:c * 4 + h + 1], lhsT=t1[:, c * 64:(c + 1) * 64], rhs=C.ones_f[0:64, 0:1], start=True, stop=True), reads=['rw_t1', 'cst'], writes=['pb6'])
            P.op('act', lambda e: e.activation(out=ex[:], in_=cs[:], func=AF.Exp, scale=-C0), reads=['rw_cs'], writes=['rw_ex'])
            P.op('dve', lambda e, h=h: e.tensor_tensor(out=RT[:, h, :], in0=tr[:], in1=ex[:], op=ALU.mult), reads=['rw_tr', 'rw_ex'], writes=['rw_RT'])
            P.op('dve', lambda e: e.tensor_tensor(out=t1[:], in0=cs[:], in1=sig[:], op=ALU.subtract), reads=['rw_cs', 'rw_sig', 'pb6' if False else 'rw_t1'], writes=['rw_t1'])
            P.op('act', lambda e: e.activation(out=ex[:], in_=t1[:], func=AF.Exp, scale=-C0), reads=['rw_t1', 'rw_RT'], writes=['rw_ex'])
            P.op('dve', lambda e, h=h: e.scalar_tensor_tensor(out=AT[:, h, :], in0=kk[:], scalar=-1.0, in1=ex[:], op0=ALU.mult, op1=ALU.mult), reads=['rw_kk', 'rw_ex'], writes=['rw_AT'])
            P.op('act', lambda e: e.activation(out=ex[:], in_=cs[:], func=AF.Exp, scale=C0), reads=['rw_cs', 'rw_AT'], writes=['rw_ex'])
            P.op('dve', lambda e, h=h: e.tensor_tensor(out=BT[:, h, :], in0=tb[:], in1=ex[:], op=ALU.mult), reads=['rw_tb', 'rw_ex'], writes=['rw_BT'])
            P.op('dve', lambda e, h=h: e.tensor_tensor(out=KT[:, h, :], in0=kp[:], in1=ex[:], op=ALU.mult), reads=['rw_kp', 'rw_ex'], writes=['rw_KT'])
            cs3 = cs[:].rearrange("p (c t) -> p c t", t=64)
            P.op('dve', lambda e, cs3=cs3: e.tensor_tensor(out=t1[:].rearrange("p (c t) -> p c t", t=64), in0=bc3(cs3[:, :, 63], NCH, 64), in1=cs3, op=ALU.subtract), reads=['rw_cs'], writes=['rw_t1'])
            P.op('act', lambda e: e.activation(out=ex[:], in_=t1[:], func=AF.Exp, scale=-C0), reads=['rw_t1', 'rw_BT', 'rw_KT'], writes=['rw_ex'])
            P.op('act', lambda e, h=h, cs3=cs3: e.activation(out=WC[:, h, :], in_=cs3[:, :, 63], func=AF.Exp, scale=-C0), reads=['rw_cs'], writes=['rw_WC'])
            pT = pb[7][:].bitcast(BF16)
            for (src, dst, nm) in ((tb, Bh, 'rw_Bh'), (kp, Kh, 'rw_Kh'), (tv, Vt, 'rw_Vt')):
                if nm == 'rw_Vt':
                    P.op('act', lambda e, src=src: e.copy(out=t2b[:], in_=src[:]), reads=['rw_tv', 'pb7'], writes=['rw_t2b'])
                else:
                    P.op('dve', lambda e, src=src: e.tensor_tensor(out=t2b[:], in0=src[:], in1=ex[:], op=ALU.mult), reads=['rw_tb', 'rw_kp', 'rw_ex', 'pb7'], writes=['rw_t2b'])
                for c in range(NCH):
                    P.op('pe', lambda e, c=c: e.transpose(out=pT[0:64, c * 64:(c + 1) * 64], in_=t2b[:, c * 64:(c + 1) * 64], identity=C.ident_bf[0:64, 0:64]), reads=['rw_t2b', 'ident_bf'], writes=['pb7'])
                P.op('act', lambda e, dst=dst, h=h: e.copy(out=dst[:, h, :, :], in_=pT[0:64, :].rearrange("p (c k) -> p c k", k=64)), reads=['pb7'], writes=[nm])
            v3 = lambda bank: bank[0:64, :].rearrange("p (c k) -> p c k", k=64)
            for g in range(2):
                cs_ = range(g * 8, g * 8 + 8)
                for (bank, kb, lt, rt_, msk, dst, nm) in ((pb[3], 'pb3', KT, AT, sup, AakT, 'rw_Aak'), (pb[4], 'pb4', BT, RT, C.triu_f[0:64, 0:64], ArbT, 'rw_Arb'), (pb[5], 'pb5', KT, RT, C.triu_f[0:64, 0:64], ArkT, 'rw_Ark')):
                    for ci, c in enumerate(cs_):
                        P.op('pe', lambda e, bank=bank, lt=lt, rt_=rt_, ci=ci, c=c, h=h: e.matmul(bank[0:64, ci * 64:(ci + 1) * 64], lhsT=lt[:, h, c * 64:(c + 1) * 64], rhs=rt_[:, h, c * 64:(c + 1) * 64], start=True, stop=True),
                             reads=['rw_AT', 'rw_RT', 'rw_BT', 'rw_KT'], writes=[kb])
                    P.op('dve', lambda e, bank=bank, msk=msk, dst=dst, h=h, g=g: e.tensor_tensor(out=dst[:, h, g * 8:g * 8 + 8, :], in0=v3(bank), in1=mbc(msk), op=ALU.mult), reads=[kb, 'rw_sup', 'cst'], writes=[nm])
            BK = {0: (0, 1, 2), 1: (3, 4, 5)}
            for g in range(2):
                bn, bm, _ = BK[g]
                for ci in range(8):
                    c = g * 8 + ci
                    P.op('pe', lambda e, ci=ci, c=c, h=h, bn=bn: e.matmul(pb[bn][0:64, ci * 64:(ci + 1) * 64], lhsT=BT[:, h, c * 64:(c + 1) * 64], rhs=AT[:, h, c * 64:(c + 1) * 64], start=True, stop=True), reads=['rw_AT', 'rw_BT'], writes=[f'pb{bn}'])
                    P.op('pe', lambda e, ci=ci, c=c, h=h, bm=bm: e.matmul(pb[bm][0:64, ci * 64:(ci + 1) * 64], lhsT=AT[:, h, c * 64:(c + 1) * 64], rhs=BT[:, h, c * 64:(c + 1) * 64], start=True, stop=True), reads=['rw_AT', 'rw_BT'], writes=[f'pb{bm}'])
                P.op('dve', lambda e, g=g, bn=bn: e.tensor_tensor(out=Nb[g][0][:], in0=v3(pb[bn]), in1=mbc(sup), op=ALU.mult), reads=[f'pb{bn}', 'rw_sup'], writes=[f'rw_Nb{g}0'])
                P.op('dve', lambda e, g=g, bm=bm: e.tensor_tensor(out=Mb[g][0][:], in0=v3(pb[bm]), in1=mbc(slo), op=ALU.mult), reads=[f'pb{bm}', 'rw_slo'], writes=[f'rw_Mb{g}0'])
                P.op('dve', lambda e, g=g: e.tensor_tensor(out=Pb[g][0][:], in0=Nb[g][0][:], in1=mbc(C.ident_f[0:64, 0:64]), op=ALU.add), reads=[f'rw_Nb{g}0', 'cst'], writes=[f'rw_Pb{g}0'])
            for i in range(1, 6):
                a, b_ = (i - 1) % 2, i % 2
                for g in range(2):
                    bn, bm, bp = BK[g]
                    for ci in range(8):
                        if i < 5:
                            P.op('pe', lambda e, ci=ci, a=a, g=g, bn=bn: e.matmul(pb[bn][0:64, ci * 64:(ci + 1) * 64], lhsT=Mb[g][a][:, ci, :], rhs=Nb[g][a][:, ci, :], start=True, stop=True), reads=[f'rw_Nb{g}{a}', f'rw_Mb{g}{a}'], writes=[f'pb{bn}'])
                        P.op('pe', lambda e, ci=ci, a=a, g=g, bm=bm: e.matmul(pb[bm][0:64, ci * 64:(ci + 1) * 64], lhsT=Nb[g][a][:, ci, :], rhs=Mb[g][a][:, ci, :], start=True, stop=True), reads=[f'rw_Nb{g}{a}', f'rw_Mb{g}{a}'], writes=[f'pb{bm}'])
                    if i < 5:
                        P.op('act', lambda e, b_=b_, g=g, bn=bn: e.copy(out=Nb[g][b_][:], in_=v3(pb[bn])), reads=[f'pb{bn}'], writes=[f'rw_Nb{g}{b_}'])
                    P.op('act', lambda e, b_=b_, g=g, bm=bm: e.copy(out=Mb[g][b_][:], in_=v3(pb[bm])), reads=[f'pb{bm}'], writes=[f'rw_Mb{g}{b_}'])
                for g in range(2):
                    bn, bm, bp = BK[g]
                    for ci in range(8):
                        P.op('pe', lambda e, ci=ci, a=a, b_=b_, g=g, bp=bp: e.matmul(pb[bp][0:64, ci * 64:(ci + 1) * 64], lhsT=Mb[g][b_][:, ci, :], rhs=Pb[g][a][:, ci, :], start=True, stop=True), reads=[f'rw_Mb{g}{b_}', f'rw_Pb{g}{a}'], writes=[f'pb{bp}'])
                    if i < 5:
                        P.op('dve', lambda e, a=a, b_=b_, g=g, bp=bp: e.tensor_tensor(out=Pb[g][b_][:], in0=v3(pb[bp]), in1=Pb[g][a][:], op=ALU.add), reads=[f'pb{bp}', f'rw_Pb{g}{a}'], writes=[f'rw_Pb{g}{b_}'])
                    else:
                        P.op('dve', lambda e, a=a, h=h, g=g, bp=bp: e.tensor_tensor(out=TT[:, h, g * 8:g * 8 + 8, :], in0=v3(pb[bp]), in1=Pb[g][a][:], op=ALU.add), reads=[f'pb{bp}', f'rw_Pb{g}{a}'], writes=['rw_TT'])
            P.op('dve', lambda e, h=h: e.tensor_copy(out=rkb[:, :, h], in_=pb[6][0:64, 0:64].rearrange("p (c h) -> p c h", h=4)[:, :, h]), reads=['pb6'], writes=['rw_rkb'])

        P.op('dve', lambda e: e.tensor_tensor(out=STw[:], in0=ST[:], in1=bc3(WC[:, :, 0], 4, 64), op=ALU.mult), reads=['rw_ST', 'rw_WC'], writes=['rw_STw'])
        for c in range(RW_SCAN_CHUNKS):
            sl = slice(c * 64, (c + 1) * 64)
            pU, pY, pS = pb[0][0:64, 0:256].rearrange("p (h v) -> p h v", h=4), pb[1][0:64, 0:256].rearrange("p (h v) -> p h v", h=4), pb[2][0:64, 0:256].rearrange("p (h v) -> p h v", h=4)
            pU2 = pb[3][0:64, 0:256].rearrange("p (h v) -> p h v", h=4)
            for h in range(4):
                P.op('pe', lambda e, h=h, sl=sl, pU=pU: e.matmul(pU[:, h, :], lhsT=AT[:, h, sl], rhs=STb[:, h, :], start=True, stop=False), reads=['rw_AT', 'rw_STb'], writes=['pb0'])
                P.op('pe', lambda e, h=h, c=c, pU=pU: e.matmul(pU[:, h, :], lhsT=AakT[:, h, c, :], rhs=Vt[:, h, c, :], start=False, stop=True), reads=['rw_Aak', 'rw_Vt'], writes=['pb0'])
            P.op('act', lambda e, pU=pU: e.copy(out=Upb[:], in_=pU), reads=['pb0'], writes=['rw_Upb'])
            for h in range(4):
                P.op('pe', lambda e, h=h, c=c, pU2=pU2: e.matmul(pU2[:, h, :], lhsT=TT[:, h, c, :], rhs=Upb[:, h, :], start=True, stop=True), reads=['rw_TT', 'rw_Upb'], writes=['pb3'])
            P.op('act', lambda e, pU2=pU2: e.copy(out=Ub[:], in_=pU2), reads=['pb3'], writes=['rw_Ub'])
            for h in range(4):
                P.op('pe', lambda e, h=h, sl=sl, pY=pY: e.matmul(pY[:, h, :], lhsT=RT[:, h, sl], rhs=STb[:, h, :], start=True, stop=False), reads=['rw_RT', 'rw_STb'], writes=['pb1'])
                P.op('pe', lambda e, h=h, c=c, pY=pY: e.matmul(pY[:, h, :], lhsT=ArbT[:, h, c, :], rhs=Ub[:, h, :], start=False, stop=False), reads=['rw_Arb', 'rw_Ub'], writes=['pb1'])
                P.op('pe', lambda e, h=h, c=c, pY=pY: e.matmul(pY[:, h, :], lhsT=ArkT[:, h, c, :], rhs=Vt[:, h, c, :], start=False, stop=True), reads=['rw_Ark', 'rw_Vt'], writes=['pb1'])
            for h in range(4):
                P.op('pe', lambda e, h=h, c=c, pS=pS: e.matmul(pS[:, h, :], lhsT=Bh[:, h, c, :], rhs=Ub[:, h, :], start=True, stop=False), reads=['rw_Bh', 'rw_Ub'], writes=['pb2'])
                P.op('pe', lambda e, h=h, c=c, pS=pS: e.matmul(pS[:, h, :], lhsT=Kh[:, h, c, :], rhs=Vt[:, h, c, :], start=False, stop=True), reads=['rw_Kh', 'rw_Vt'], writes=['pb2'])
            P.op('dve', lambda e, pS=pS: e.tensor_tensor(out=STb[:], in0=STw[:], in1=pS, op=ALU.add), reads=['rw_STw', 'pb2'], writes=['rw_STb'])
            P.op('dve', lambda e, pS=pS: e.tensor_tensor(out=ST[:], in0=STw[:], in1=pS, op=ALU.add), reads=['rw_STw', 'pb2'], writes=['rw_ST'])
            if not (half == 1 and c == NCH - 1):
                cn, hn = (c + 1) % NCH, (c + 1) // NCH
                if hn == 0:
                    P.op('dve', lambda e, cn=cn: e.tensor_tensor(out=STw[:], in0=ST[:], in1=bc3(WC[:, :, cn], 4, 64), op=ALU.mult), reads=['rw_ST', 'rw_WC'], writes=['rw_STw'])
            yi = c % 2
            Yc, kY = Y[yi], f'rw_Y{yi}'
            P.op('act', lambda e, Yc=Yc, pY=pY: e.copy(out=Yc[:], in_=pY), reads=['pb1'], writes=[kY])
            P.op('pe', lambda e, sl=sl: e.matmul(pb[4][0:64, 0:256], lhsT=sg[:, sl], rhs=gub[:], start=True, stop=True), reads=['rw_sg', 'rw_gub'], writes=['pb4'])
            P.op('dve', lambda e, Yc=Yc: e.tensor_reduce(out=st4[:, 0:4], in_=Yc[:], axis=AX.X, op=ALU.add), reads=[kY], writes=['rw_st4'])
            P.op('dve', lambda e: e.tensor_scalar(out=st4[:, 0:4], in0=st4[:, 0:4], scalar1=1.0 / 64, scalar2=None, op0=ALU.mult), reads=['rw_st4'], writes=['rw_st4'])
            P.op('dve', lambda e, Yc=Yc: e.tensor_tensor(out=yc[:], in0=Yc[:], in1=bc3(st4[:, 0:4], 4, 64), op=ALU.subtract), reads=[kY, 'rw_st4'], writes=['rw_yc'])
            P.op('dve', lambda e: e.tensor_tensor(out=ysq[:], in0=yc[:], in1=yc[:], op=ALU.mult), reads=['rw_yc'], writes=['rw_ysq'])
            P.op('dve', lambda e: e.tensor_reduce(out=st4[:, 4:8], in_=ysq[:], axis=AX.X, op=ALU.add), reads=['rw_ysq'], writes=['rw_st4'])
            P.op('dve', lambda e: e.tensor_scalar(out=st4[:, 4:8], in0=st4[:, 4:8], scalar1=1.0 / 64, scalar2=GN_EPS, op0=ALU.mult, op1=ALU.add), reads=['rw_st4'], writes=['rw_st4'])
            P.op('act', lambda e: e.activation(out=st4[:, 4:8], in_=st4[:, 4:8], func=AF.Sqrt), reads=['rw_st4'], writes=['rw_st4'])
            P.op('dve', lambda e: e.reciprocal(out=st4[:, 4:8], in_=st4[:, 4:8]), reads=['rw_st4'], writes=['rw_st4'])
            P.op('dve', lambda e: e.tensor_tensor(out=yc[:], in0=yc[:], in1=bc3(st4[:, 4:8], 4, 64), op=ALU.mult), reads=['rw_yc', 'rw_st4'], writes=['rw_yc'])
            ycf = yc[:].rearrange("p h v -> p (h v)")
            P.op('dve', lambda e, ycf=ycf: e.tensor_tensor(out=ycf, in0=ycf, in1=gng[:], op=ALU.mult), reads=['rw_yc', 'rw_gng'], writes=['rw_yc'])
            P.op('dve', lambda e, ycf=ycf: e.tensor_tensor(out=ycf, in0=ycf, in1=gnb[:], op=ALU.add), reads=['rw_yc', 'rw_gnb'], writes=['rw_yc'])
            P.op('dve', lambda e, c=c: e.tensor_tensor(out=ysq[:], in0=Vt[:, :, c, :], in1=bc3(rkb[:, c, :], 4, 64), op=ALU.mult), reads=['rw_Vt', 'rw_rkb'], writes=['rw_ysq'])
            P.op('dve', lambda e: e.tensor_tensor(out=yc[:], in0=yc[:], in1=ysq[:], op=ALU.add), reads=['rw_yc', 'rw_ysq'], writes=['rw_yc'])
            oi = c % 2
            P.op('dve', lambda e, oi=oi, ycf=ycf: e.tensor_tensor(out=ob[oi][:], in0=ycf, in1=pb[4][0:64, 0:256], op=ALU.mult), reads=['rw_yc', 'pb4'], writes=[f'rw_ob{oi}'])
            P.dma('sp', L.otok[t0 + c * 64:t0 + (c + 1) * 64, 512:768], ob[oi][:], reads=[f'rw_ob{oi}'], writes=[f'otok{l}_rw'])


def ln_consts(C, L, which, tag):
    P, l = C.P, L.l
    g = P.sb([128, D], F32, f"{tag}_lng{l}")
    b = P.sb([128, D], F32, f"{tag}_lnb{l}")
    P.dma('sp', g[:], C.ln_g[l, which].partition_broadcast(128), writes=[f'{tag}_lng'])
    P.dma('sp', b[:], C.ln_b[l, which].partition_broadcast(128), writes=[f'{tag}_lnb'])
    eps = P.sb([128, 2], F32, f"{tag}_eps{l}")
    P.op('dve', lambda e: e.memset(eps[:], LN_EPS), writes=[f'{tag}_eps'])
    tmp = dict(g=g, b=b, eps=eps, tag=tag,
               c=[P.sb([128, D], F32, f"{tag}_lc{l}_{i}") for i in range(2)],
               sq=P.sb([128, D], F32, f"{tag}_lsq{l}"),
               st=[P.sb([128, 8], F32, f"{tag}_lst{l}_{i}") for i in range(2)])
    return tmp


def layer_norm_tile(C, LN, i, h, kh, out_dram, kout):
    P = C.P
    tag = LN['tag']
    c, sq, st = LN['c'][i], LN['sq'], LN['st'][i]
    kc, ksq, kst = f'{tag}_lc{i}', f'{tag}_lsq', f'{tag}_lst{i}'
    P.op('dve', lambda e: e.tensor_tensor(out=st[:, 0:1], in0=st[:, 4:5], in1=st[:, 5:6], op=ALU.add), reads=[kst], writes=[kst])
    P.op('dve', lambda e: e.tensor_scalar(out=st[:, 0:1], in0=st[:, 0:1], scalar1=-1.0 / D, scalar2=None, op0=ALU.mult), reads=[kst], writes=[kst])
    P.op('act', lambda e: e.activation(out=c[:], in_=h, func=AF.Identity, bias=st[:, 0:1]), reads=[kh, kst], writes=[kc])
    P.op('act', lambda e: e.activation(out=sq[:], in_=c[:], func=AF.Square, accum_out=st[:, 1:2]), reads=[kc], writes=[ksq, kst])
    P.op('act', lambda e: e.activation(out=st[:, 2:3], in_=st[:, 1:2], func=AF.Sqrt, scale=1.0 / D, bias=LN['eps'][:, 0:1]), reads=[kst, f'{tag}_eps'], writes=[kst])
    P.op('dve', lambda e: e.reciprocal(out=st[:, 2:3], in_=st[:, 2:3]), reads=[kst], writes=[kst])
    P.op('dve', lambda e: e.scalar_tensor_tensor(out=c[:], in0=c[:], scalar=st[:, 2:3], in1=LN['g'][:], op0=ALU.mult, op1=ALU.mult), reads=[kc, kst, f'{tag}_lng'], writes=[kc])
    P.op('pool', lambda e: e.tensor_tensor(out=c[:], in0=c[:], in1=LN['b'][:], op=ALU.add), reads=[kc, f'{tag}_lnb'], writes=[kc])
    P.dma('act', out_dram, c[:], reads=[kc], writes=[kout])


def stage_merge(C, L):
    P, l = C.P, L.l
    P.stage_begin()
    pb = C.pb
    mT = P.sb([128, 8, S], BF16, f"mg_mT{l}")
    m0 = P.mark()
    oT = P.sb([128, 10, S], BF16, f"mg_oT{l}")
    xT = P.sb([128, 8, S], BF16, f"mg_xT{l}")
    P.dma('sp', xT[:], L.xT, reads=[f'xT{l}'], writes=['mg_xT'])
    ot = [P.sb([128, 1280], BF16, f"mg_ot{l}_{i}") for i in range(2)]
    wbf = [P.sb([128, 1024], F32, f"mg_wbf{l}_{i}") for i in range(2)]
    wbb = P.sb([128, 10, 1024], BF16, f"mg_wbb{l}")
    wgf = [P.sb([128, 8, 4, 128], F32, f"mg_wgf{l}_{i}") for i in range(2)]
    wgb = [P.sb([128, 8, 4, 128], BF16, f"mg_wgb{l}_{i}") for i in range(2)]
    P.dma('sp', wgf[0][:], C.wG[l, 0], writes=['mg_wgf0'])
    P.op('pool', lambda e: e.tensor_copy(out=wgb[0][:], in_=wgf[0][:]), reads=['mg_wgf0'], writes=['mg_wgb0'])
    for t in range(NT):
        i = t % 2
        if t < 10:
            cc = t
            P.dma('sp', wbf[cc % 2][:], C.w_branch[l, cc * 128:(cc + 1) * 128, :], writes=[f'mg_wbf{cc % 2}'])
            P.op('pool', lambda e, cc=cc: e.tensor_copy(out=wbb[:, cc, :], in_=wbf[cc % 2][:]), reads=[f'mg_wbf{cc % 2}'], writes=['mg_wbb'])
        P.dma('sp', ot[i][:], L.otok[t * 128:(t + 1) * 128, :], reads=[f'otok{l}_sw', f'otok{l}_rw', f'otok{l}_fx', f'otok{l}_ds'], writes=[f'mg_ot{i}'])
        for (c0, c1, bk) in ((0, 8, 0), (8, 10, 1)):
            pT = pb[bk][:].bitcast(BF16)
            for cc in range(c0, c1):
                P.op('pe', lambda e, i=i, cc=cc, c0=c0, pT=pT: e.transpose(out=pT[:, (cc - c0) * 128:(cc - c0 + 1) * 128], in_=ot[i][:, cc * 128:(cc + 1) * 128], identity=C.ident_bf[:]), reads=[f'mg_ot{i}', 'ident_bf'], writes=[f'pb{bk}'])
            P.op('act', lambda e, t=t, c0=c0, c1=c1, pT=pT: e.copy(out=oT[:, c0:c1, t * 128:(t + 1) * 128], in_=pT[:, 0:(c1 - c0) * 128].rearrange("p (k c) -> p k c", c=128)), reads=[f'pb{bk}'], writes=['mg_oT'])
    gb = P.sb([128, 4, 8], F32, f"mg_gb{l}")
    P.dma('sp', gb[:], C.gbias[l], writes=['mg_gb'])
    gt = [P.sb([128, 512], F32, f"mg_gt{l}_{i}") for i in range(2)]
    macc = P.sb([128, 512], F32, f"mg_macc{l}")
    mtmp = P.sb([128, 512], F32, f"mg_mtmp{l}")
    BR = ((0, 4), (4, 6), (6, 8), (8, 10))
    n = 0
    for f in range(8):
        fi = f % 2
        if f > 0:
            P.dma('sp', wgf[fi][:], C.wG[l, f], writes=[f'mg_wgf{fi}'])
            P.op('pool', lambda e, fi=fi: e.tensor_copy(out=wgb[fi][:], in_=wgf[fi][:]), reads=[f'mg_wgf{fi}'], writes=[f'mg_wgb{fi}'])
        for tb in range(4):
            ts_ = slice(tb * 512, (tb + 1) * 512)
            for br in range(4):
                i = n % 2
                n += 1
                bg, bp = 2 + i, 4 + i
                for kc in range(8):
                    P.op('pe', lambda e, fi=fi, kc=kc, br=br, ts_=ts_, bg=bg: e.matmul(pb[bg][:], lhsT=wgb[fi][:, kc, br, :], rhs=xT[:, kc, ts_], start=(kc == 0), stop=(kc == 7)), reads=[f'mg_wgb{fi}', 'mg_xT'], writes=[f'pb{bg}'])
                P.op('act', lambda e, i=i, bg=bg, br=br, f=f: e.activation(out=gt[i][:], in_=pb[bg][:], func=AF.Sigmoid, bias=gb[:, br, f:f + 1]), reads=[f'pb{bg}', 'mg_gb'], writes=[f'mg_gt{i}'])
                c0, c1 = BR[br]
                for cc in range(c0, c1):
                    P.op('pe', lambda e, cc=cc, f=f, ts_=ts_, bp=bp, c0=c0, c1=c1: e.matmul(pb[bp][:], lhsT=wbb[:, cc, f * 128:(f + 1) * 128], rhs=oT[:, cc, ts_], start=(cc == c0), stop=(cc == c1 - 1)), reads=['mg_wbb', 'mg_oT'], writes=[f'pb{bp}'])
                if br == 0:
                    P.op('dve', lambda e, i=i, bp=bp: e.tensor_tensor(out=macc[:], in0=gt[i][:], in1=pb[bp][:], op=ALU.mult), reads=[f'mg_gt{i}', f'pb{bp}'], writes=['mg_macc'])
                else:
                    P.op('dve', lambda e, i=i, bp=bp: e.tensor_tensor(out=mtmp[:], in0=gt[i][:], in1=pb[bp][:], op=ALU.mult), reads=[f'mg_gt{i}', f'pb{bp}'], writes=['mg_mtmp'])
                    if br < 3:
                        P.op('dve', lambda e: e.tensor_tensor(out=macc[:], in0=macc[:], in1=mtmp[:], op=ALU.add), reads=['mg_macc', 'mg_mtmp'], writes=['mg_macc'])
                    else:
                        P.op('dve', lambda e, f=f, ts_=ts_: e.tensor_tensor(out=mT[:, f, ts_], in0=macc[:], in1=mtmp[:], op=ALU.add), reads=['mg_macc', 'mg_mtmp'], writes=['mg_mT'])
    P.release(m0)
    wbf2 = P.sb([128, 1024], F32, f"mg_wbf2{l}")
    wob = P.sb([128, 8, 1024], BF16, f"mg_wob{l}")
    for cc in range(8):
        P.dma('sp', wbf2[:], C.w_out[l, cc * 128:(cc + 1) * 128, :], writes=['mg_wbf2'])
        P.op('pool', lambda e, cc=cc: e.tensor_copy(out=wob[:, cc, :], in_=wbf2[:]), reads=['mg_wbf2'], writes=['mg_wob'])
    LN = ln_consts(C, L, 0, "mg")
    xr = [P.sb([128, D], F32, f"mg_xr{l}_{i}") for i in range(2)]
    for t in range(NT):
        i = t % 2
        P.dma('sp', xr[i][:], L.xin[t * 128:(t + 1) * 128, :], reads=['xin'], writes=[f'mg_xr{i}'])
        for hf in range(2):
            bk = 6 + hf
            for f in range(8):
                P.op('pe', lambda e, f=f, t=t, hf=hf, bk=bk: e.matmul(pb[bk][:], lhsT=mT[:, f, t * 128:(t + 1) * 128], rhs=wob[:, f, hf * 512:(hf + 1) * 512], start=(f == 0), stop=(f == 7)), reads=['mg_mT', 'mg_wob'], writes=[f'pb{bk}'])
            P.op('dve', lambda e, i=i, hf=hf, bk=bk: e.scalar_tensor_tensor(out=xr[i][:, hf * 512:(hf + 1) * 512], in0=xr[i][:, hf * 512:(hf + 1) * 512], scalar=ALPHA, in1=pb[bk][:], op0=ALU.mult, op1=ALU.add, accum_out=LN['st'][i][:, 4 + hf:5 + hf]), reads=[f'mg_xr{i}', f'pb{bk}'], writes=[f'mg_xr{i}', f'mg_lst{i}'])
        layer_norm_tile(C, LN, i, xr[i][:], f'mg_xr{i}', L.x1[t * 128:(t + 1) * 128, :], f'x1_{l}')


def make_xT(C, L, tag, src, ksrc, want_f32_router=None, xT=None):
    P, l = C.P, L.l
    pb = C.pb
    if xT is None:
        xT = P.sb([128, 8, S], BF16, f"{tag}_xT{l}")
    xs = [P.sb([128, D], F32, f"{tag}_xs{l}_{i}") for i in range(2)]
    xb = [P.sb([128, D], BF16, f"{tag}_xb{l}_{i}") for i in range(2)]
    if want_f32_router is not None:
        rwf, lg = want_f32_router
        xtf = [P.sb([128, 8, 128], F32, f"{tag}_xtf{l}_{i}") for i in range(2)]
    for t in range(NT):
        i = t % 2
        P.dma('sp', xs[i][:], src[t * 128:(t + 1) * 128, :], reads=[ksrc], writes=[f'{tag}_xs{i}'])
        P.op('dve', lambda e, i=i: e.tensor_copy(out=xb[i][:], in_=xs[i][:]), reads=[f'{tag}_xs{i}'], writes=[f'{tag}_xb{i}'])
        pT = pb[t % 2][:].bitcast(BF16)
        for kc in range(8):
            P.op('pe', lambda e, i=i, kc=kc, pT=pT: e.transpose(out=pT[:, kc * 128:(kc + 1) * 128], in_=xb[i][:, kc * 128:(kc + 1) * 128], identity=C.ident_bf[:]), reads=[f'{tag}_xb{i}', 'ident_bf'], writes=[f'pb{t % 2}'])
        P.op('act', lambda e, t=t, pT=pT: e.copy(out=xT[:, :, t * 128:(t + 1) * 128], in_=pT.rearrange("p (k c) -> p k c", k=8)), reads=[f'pb{t % 2}'], writes=[f'{tag}_xT'])
        if want_f32_router is not None:
            for hf in range(2):
                bk = 2 + hf
                for kk_ in range(4):
                    kc = hf * 4 + kk_
                    P.op('pe', lambda e, i=i, kc=kc, kk_=kk_, bk=bk: e.transpose(out=pb[bk][:, kk_ * 128:(kk_ + 1) * 128], in_=xs[i][:, kc * 128:(kc + 1) * 128], identity=C.ident_f), reads=[f'{tag}_xs{i}', 'cst'], writes=[f'pb{bk}'])
                P.op('act', lambda e, i=i, hf=hf, bk=bk: e.copy(out=xtf[i][:, hf * 4:hf * 4 + 4, :], in_=pb[bk][:].rearrange("p (k c) -> p k c", c=128)), reads=[f'pb{bk}'], writes=[f'{tag}_xtf{i}'])
            for kc in range(8):
                P.op('pe', lambda e, i=i, kc=kc: e.matmul(pb[4][:, 0:8], lhsT=xtf[i][:, kc, :], rhs=rwf[:, kc, :], start=(kc == 0), stop=(kc == 7)), reads=[f'{tag}_xtf{i}', 'moe_rwf'], writes=['pb4'])
            P.op('dve', lambda e, t=t: e.tensor_copy(out=lg[:, t, :], in_=pb[4][:, 0:8]), reads=['pb4'], writes=['moe_lg'])
    return xT


def ffn_phase1(C, L, tag, xT, w13, nf, hT, w13f, w13b, w2=None, w2b=None, w2f=None):
    P, l = C.P, L.l
    pb = C.pb
    st = C.ffn_sil
    n = 0
    for f in range(nf):
        fi = f % 2
        P.dma('sp', w13f[fi][:], w13[f], writes=[f'{tag}_w13f{fi}'])
        P.op('pool', lambda e, fi=fi: e.tensor_copy(out=w13b[fi][:], in_=w13f[fi][:]), reads=[f'{tag}_w13f{fi}'], writes=[f'{tag}_w13b{fi}'])
        if w2 is not None:
            P.dma('sp', w2f[fi][:], w2[f], writes=[f'{tag}_w2f{fi}'])
            P.op('pool', lambda e, fi=fi, f=f: e.tensor_copy(out=w2b[:, f, :], in_=w2f[fi][:]), reads=[f'{tag}_w2f{fi}'], writes=[f'{tag}_w2b{f}'])
        for tb in range(4):
            ts_ = slice(tb * 512, (tb + 1) * 512)
            i = n % 2
            n += 1
            ba, bb = i, 2 + i
            for kc in range(8):
                P.op('pe', lambda e, fi=fi, kc=kc, ts_=ts_, ba=ba: e.matmul(pb[ba][:], lhsT=w13b[fi][:, kc, 0, :], rhs=xT[:, kc, ts_], start=(kc == 0), stop=(kc == 7)), reads=[f'{tag}_w13b{fi}', f'{tag}_xT'], writes=[f'pb{ba}'])
            for kc in range(8):
                P.op('pe', lambda e, fi=fi, kc=kc, ts_=ts_, bb=bb: e.matmul(pb[bb][:], lhsT=w13b[fi][:, kc, 1, :], rhs=xT[:, kc, ts_], start=(kc == 0), stop=(kc == 7)), reads=[f'{tag}_w13b{fi}', f'{tag}_xT'], writes=[f'pb{bb}'])
            P.op('act', lambda e, i=i, ba=ba: e.activation(out=st[i][:], in_=pb[ba][:], func=AF.Silu), reads=[f'pb{ba}'], writes=[f'ffn_sil{i}'])
            P.op('dve', lambda e, i=i, bb=bb, f=f, ts_=ts_: e.tensor_tensor(out=hT[:, f, ts_], in0=st[i][:], in1=pb[bb][:], op=ALU.mult), reads=[f'ffn_sil{i}', f'pb{bb}'], writes=[f'{tag}_hT'])


def ffn_phase2(C, L, tag, w2, nf, hT, w2b, w2f, y_cb):
    P, l = C.P, L.l
    pb = C.pb
    for t in range(NT):
        for hf in range(2):
            bk = 4 + (2 * t + hf) % 4
            for f in range(nf):
                P.op('pe', lambda e, f=f, t=t, hf=hf, bk=bk: e.matmul(pb[bk][:], lhsT=hT[:, f, t * 128:(t + 1) * 128], rhs=w2b[:, f, hf * 512:(hf + 1) * 512], start=(f == 0), stop=(f == nf - 1)), reads=[f'{tag}_hT', f'{tag}_w2b{f}'], writes=[f'pb{bk}'])
            y_cb(t, hf, pb[bk], f'pb{bk}')


def stage_ffn_dense(C, L):
    P, l = C.P, L.l
    P.stage_begin()
    nf = D_FF // 128
    hT = P.sb([128, nf, S], BF16, f"ff_hT{l}")
    w2b = P.sb([128, nf, D], BF16, f"ff_w2b{l}")
    w2f = [P.sb([128, D], F32, f"ff_w2f{l}_{i}") for i in range(2)]
    m0 = P.mark()
    xT = P.sb([128, 8, S], BF16, f"ff_xT{l}")
    m1 = P.mark()
    make_xT(C, L, "ff", L.x1, f'x1_{l}', xT=xT)
    P.release(m1)
    w13f = [P.sb([128, 8, 2, 128], F32, f"ff_w13f{l}_{i}") for i in range(2)]
    w13b = [P.sb([128, 8, 2, 128], BF16, f"ff_w13b{l}_{i}") for i in range(2)]
    C.ffn_sil = [P.sb([128, 512], F32, f"ff_sil{l}_{i}") for i in range(2)]
    ffn_phase1(C, L, "ff", xT, C.w13d, nf, hT, w13f, w13b, C.w2d, w2b, w2f)
    P.release(m0)
    LN = ln_consts(C, L, 1, "ff")
    xr = [P.sb([128, D], F32, f"ff_xr{l}_{i}") for i in range(2)]
    dst = L.xout

    def y_cb(t, hf, bank, kb):
        i = t % 2
        if hf == 0:
            P.dma('sp', xr[i][:], L.x1[t * 128:(t + 1) * 128, :], reads=[f'x1_{l}'], writes=[f'ff_xr{i}'])
        P.op('dve', lambda e: e.scalar_tensor_tensor(out=xr[i][:, hf * 512:(hf + 1) * 512], in0=xr[i][:, hf * 512:(hf + 1) * 512], scalar=ALPHA, in1=bank[:], op0=ALU.mult, op1=ALU.add, accum_out=LN['st'][i][:, 4 + hf:5 + hf]), reads=[f'ff_xr{i}', kb], writes=[f'ff_xr{i}', f'ff_lst{i}'])
        if hf == 1:
            layer_norm_tile(C, LN, i, xr[i][:], f'ff_xr{i}', dst[t * 128:(t + 1) * 128, :], f'xout{l}')

    ffn_phase2(C, L, "ff", C.w2d, nf, hT, w2b, w2f, y_cb)


def stage_moe(C, L):
    P, l = C.P, L.l
    P.stage_begin()
    pb = C.pb
    rwf = P.sb([128, 8, NEXP], F32, f"moe_rwf{l}")
    P.dma('sp', rwf[:], C.router_w.rearrange("(k p) e -> p k e", p=128), writes=['moe_rwf'])
    rb = P.sb([128, NEXP], F32, f"moe_rb{l}")
    P.dma('sp', rb[:], C.router_b.partition_broadcast(128), writes=['moe_rb'])
    lg = P.sb([128, NT, NEXP], F32, f"moe_lg{l}")
    m1 = P.sb([128, NT], F32, f"moe_m1{l}")
    m2 = P.sb([128, NT], F32, f"moe_m2{l}")
    eq1 = P.sb([128, NT, NEXP], F32, f"moe_eq1{l}")
    eq2 = P.sb([128, NT, NEXP], F32, f"moe_eq2{l}")
    lg2 = P.sb([128, NT, NEXP], F32, f"moe_lg2{l}")
    comb = P.sb([128, NT, NEXP], F32, f"moe_comb{l}")
    yacc = P.sb([128, NT, D], F32, f"moe_yacc{l}")
    xT = P.sb([128, 8, S], BF16, f"moe_xT{l}")
    m0 = P.mark()
    make_xT(C, L, "moe", L.x1, f'x1_{l}', want_f32_router=(rwf, lg), xT=xT)
    bcx = lambda a: a.unsqueeze(2).to_broadcast([128, NT, NEXP])
    P.op('dve', lambda e: e.tensor_tensor(out=lg[:], in0=lg[:], in1=rb[:].unsqueeze(1).to_broadcast([128, NT, NEXP]), op=ALU.add), reads=['moe_lg', 'moe_rb'], writes=['moe_lg'])
    P.op('dve', lambda e: e.tensor_reduce(out=m1[:], in_=lg[:], axis=AX.X, op=ALU.max), reads=['moe_lg'], writes=['moe_m1'])
    P.op('dve', lambda e: e.tensor_tensor(out=eq1[:], in0=lg[:], in1=bcx(m1[:]), op=ALU.is_equal), reads=['moe_lg', 'moe_m1'], writes=['moe_eq1'])
    P.op('dve', lambda e: e.scalar_tensor_tensor(out=lg2[:], in0=eq1[:], scalar=-1.0e30, in1=lg[:], op0=ALU.mult, op1=ALU.add), reads=['moe_eq1', 'moe_lg'], writes=['moe_lg2'])
    P.op('dve', lambda e: e.tensor_reduce(out=m2[:], in_=lg2[:], axis=AX.X, op=ALU.max), reads=['moe_lg2'], writes=['moe_m2'])
    P.op('dve', lambda e: e.tensor_tensor(out=eq2[:], in0=lg2[:], in1=bcx(m2[:]), op=ALU.is_equal), reads=['moe_lg2', 'moe_m2'], writes=['moe_eq2'])
    P.op('dve', lambda e: e.tensor_tensor(out=m2[:], in0=m2[:], in1=m1[:], op=ALU.subtract), reads=['moe_m1', 'moe_m2'], writes=['moe_m2'])
    P.op('act', lambda e: e.activation(out=m2[:], in_=m2[:], func=AF.Exp), reads=['moe_m2'], writes=['moe_m2'])
    P.op('dve', lambda e: e.tensor_scalar(out=m1[:], in0=m2[:], scalar1=1.0, scalar2=None, op0=ALU.add), reads=['moe_m2'], writes=['moe_m1'])
    P.op('dve', lambda e: e.reciprocal(out=m1[:], in_=m1[:]), reads=['moe_m1'], writes=['moe_m1'])
    P.op('dve', lambda e: e.tensor_tensor(out=m2[:], in0=m2[:], in1=m1[:], op=ALU.mult), reads=['moe_m1', 'moe_m2'], writes=['moe_m2'])
    P.op('dve', lambda e: e.tensor_tensor(out=comb[:], in0=eq1[:], in1=bcx(m1[:]), op=ALU.mult), reads=['moe_eq1', 'moe_m1'], writes=['moe_comb'])
    P.op('dve', lambda e: e.tensor_tensor(out=eq2[:], in0=eq2[:], in1=bcx(m2[:]), op=ALU.mult), reads=['moe_eq2', 'moe_m2'], writes=['moe_eq2'])
    P.op('dve', lambda e: e.tensor_tensor(out=comb[:], in0=comb[:], in1=eq2[:], op=ALU.add), reads=['moe_comb', 'moe_eq2'], writes=['moe_comb'])
    if 'moecomb' in C.dbg:
        P.dma('sp', C.scratch("moecomb", [128, NT, NEXP], F32), comb[:], reads=['moe_comb'], writes=['moecomb'])

    P.release(m0)
    nf = D_FFE // 128
    hT = P.sb([128, nf, S], BF16, f"moe_hT{l}")
    w2b = P.sb([128, nf, D], BF16, f"moe_w2b{l}")
    w13f = [P.sb([128, 8, 2, 128], F32, f"moe_w13f{l}_{i}") for i in range(2)]
    w13b = [P.sb([128, 8, 2, 128], BF16, f"moe_w13b{l}_{i}") for i in range(2)]
    w2f = [P.sb([128, D], F32, f"moe_w2f{l}_{i}") for i in range(2)]
    C.ffn_sil = [P.sb([128, 512], F32, f"moe_sil{l}_{i}") for i in range(2)]
    for ex in range(NEXP):
        def y_cb(t, hf, bank, kb, ex=ex):
            sl = slice(hf * 512, (hf + 1) * 512)
            if ex == 0:
                P.op('dve', lambda e: e.tensor_scalar(out=yacc[:, t, sl], in0=bank[:], scalar1=comb[:, t, ex:ex + 1], scalar2=None, op0=ALU.mult), reads=[kb, 'moe_comb'], writes=[f'moe_yacc{t}'])
            else:
                P.op('dve', lambda e: e.scalar_tensor_tensor(out=yacc[:, t, sl], in0=bank[:], scalar=comb[:, t, ex:ex + 1], in1=yacc[:, t, sl], op0=ALU.mult, op1=ALU.add), reads=[kb, 'moe_comb', f'moe_yacc{t}'], writes=[f'moe_yacc{t}'])
        ffn_phase1(C, L, "moe", xT, C.w13e[ex], nf, hT, w13f, w13b, C.w2e[ex], w2b, w2f)
        ffn_phase2(C, L, "moe", C.w2e[ex], nf, hT, w2b, w2f, y_cb)
    P.release(m0)
    LN = ln_consts(C, L, 1, "mo")
    xr = [P.sb([128, D], F32, f"mo_xr{l}_{i}") for i in range(2)]
    for t in range(NT):
        i = t % 2
        P.dma('sp', xr[i][:], L.x1[t * 128:(t + 1) * 128, :], reads=[f'x1_{l}'], writes=[f'mo_xr{i}'])
        for hf in range(2):
            P.op('dve', lambda e, i=i, t=t, hf=hf: e.scalar_tensor_tensor(out=xr[i][:, hf * 512:(hf + 1) * 512], in0=xr[i][:, hf * 512:(hf + 1) * 512], scalar=ALPHA, in1=yacc[:, t, hf * 512:(hf + 1) * 512], op0=ALU.mult, op1=ALU.add, accum_out=LN['st'][i][:, 4 + hf:5 + hf]), reads=[f'mo_xr{i}', f'moe_yacc{t}'], writes=[f'mo_xr{i}', f'mo_lst{i}'])
        layer_norm_tile(C, LN, i, xr[i][:], f'mo_xr{i}', L.xout[t * 128:(t + 1) * 128, :], f'xout{l}')


def kernel(**inputs):
    sh = prep_shared(inputs)
    nc = build_program("ABCDEFG", layers=(0, 1))
    x = np.asarray(inputs['x'])
    n = x.shape[0]
    in_maps = [dict(sh, x=np.ascontiguousarray(x[b])) for b in range(n)]
    res = run_bass_kernel_spmd(nc, in_maps, core_ids=list(range(n)))
    return np.stack([np.asarray(r['out']) for r in res.results]).astype(np.float32)


BIGM = 32768.0
DSA_ACT_J = tuple(range(11, 16))


def stage_dsa2(C, L):
    P, l = C.P, L.l
    P.stage_begin()
    pb = C.pb
    qT = load_heads(C, L, "ds_q", HS_DSQ, 4)
    kT = load_heads(C, L, "ds_k", HS_DSK, 1)
    va = load_vaug(C, L, "ds_v", 384, 1)
    wi = P.sb([128, NT, 4], F32, f"ds_wi{l}")
    P.dma('sp', wi[:], L.tokm.rearrange("(t p) c -> p t c", p=128)[:, :, 452:456], reads=[f'tokm{l}'], writes=['ds_wi'])
    OFF = [128 * j * (j + 1) // 2 for j in range(NT + 1)]
    sc = P.sb([128, OFF[NT]], F32, f"ds_sc{l}")
    mpm = P.sb([128, OFF[NT]], BF16, f"ds_mpm{l}")
    ods = P.sb([128, NT, 256], BF16, f"ds_o{l}")
    identB = P.sb([128, 128], BF16, f"ds_identB{l}")
    P.op('dve', lambda e: e.tensor_scalar(out=identB[:], in0=C.ident_f, scalar1=BIGM, scalar2=None, op0=ALU.mult), reads=['cst'], writes=['ds_identB'])
    stt = {nm: P.sb([128, NT], F32, f"ds_{nm}{l}") for nm in ('lo', 'rng', 'mid', 'wc', 'sel', 'mx', 'thr', 'need')}
    cnt = P.sb([128, NT], F32, f"ds_cnt{l}")
    rec = P.sb([128, 4], F32, f"ds_rec{l}")
    m0 = P.mark()
    qiT = load_heads(C, L, "ds_qi", HS_DSQI, 4)
    kiT = load_heads(C, L, "ds_ki", HS_DSKI, 1)
    rl = [P.sb([128, 512], F32, f"ds_rl{l}_{i}") for i in range(4)]
    nr = 0
    for j in range(NT):
        n = (j + 1) * 128
        o = OFF[j]
        ksc = f'ds_sc{j}'
        for c0 in range(0, n, 512):
            nn = min(512, n - c0)
            for h in range(4):
                bk = nr % 8
                ri = nr % 4
                nr += 1
                P.op('pe', lambda e, h=h, j=j, c0=c0, nn=nn, bk=bk: e.matmul(pb[bk][:, 0:nn], lhsT=qiT[:, h, j * 128:(j + 1) * 128], rhs=kiT[:, 0, c0:c0 + nn], start=True, stop=True),
                     reads=['ds_qi', 'ds_ki'], writes=[f'pb{bk}'])
                P.op('act', lambda e, bk=bk, ri=ri, nn=nn: e.activation(out=rl[ri][:, 0:nn], in_=pb[bk][:, 0:nn], func=AF.Relu), reads=[f'pb{bk}'], writes=[f'ds_rl{ri}'])
                if h == 0:
                    P.op('dve', lambda e, ri=ri, nn=nn, c0=c0, j=j, o=o: e.tensor_scalar(out=sc[:, o + c0:o + c0 + nn], in0=rl[ri][:, 0:nn], scalar1=wi[:, j, 0:1], scalar2=None, op0=ALU.mult),
                         reads=[f'ds_rl{ri}', 'ds_wi'], writes=[ksc])
                else:
                    P.op('dve', lambda e, ri=ri, nn=nn, c0=c0, j=j, h=h, o=o: e.scalar_tensor_tensor(out=sc[:, o + c0:o + c0 + nn], in0=rl[ri][:, 0:nn], scalar=wi[:, j, h:h + 1], in1=sc[:, o + c0:o + c0 + nn], op0=ALU.mult, op1=ALU.add),
                         reads=[f'ds_rl{ri}', 'ds_wi', ksc], writes=[ksc])
        if j >= 2:
            P.op('dve', lambda e, j=j, n=n, o=o: e.tensor_reduce(out=stt['lo'][:, j:j + 1], in_=sc[:, o:o + n - 64], axis=AX.X, op=ALU.min), reads=[ksc], writes=[f'ds_lo{j}'])
        P.op('dve', lambda e, n=n, o=o: e.memset(sc[0:64, o + n - 64:o + n], NEG), reads=[ksc], writes=[ksc])
        if j >= 2:
            P.op('dve', lambda e, j=j, n=n, o=o: e.tensor_reduce(out=stt['mx'][:, j:j + 1], in_=sc[:, o:o + n], axis=AX.X, op=ALU.max), reads=[ksc], writes=[f'ds_mx{j}'])
    P.release(m0)
    junk = {eng: P.sb([128, S], BF16, f"ds_junk{l}_{eng}") for eng in ('dve', 'act')}
    junkr = {eng: [P.sb([128, S], BF16, f"ds_junkr{l}_{eng}{i}") for i in range(3)] for eng in ('dve', 'act')}
    tA = {eng: P.sb([128, S], F32, f"ds_tA{l}_{eng}") for eng in ('dve',)}
    tE = {eng: P.sb([128, S], F32, f"ds_tE{l}_{eng}") for eng in ('dve',)}
    halfn = P.sb([128, NT], F32, f"ds_halfn{l}")
    ssum = P.sb([128, NT], F32, f"ds_ssum{l}")
    nmid = P.sb([128, NT], F32, f"ds_nmid{l}")
    for j in DSA_ACT_J:
        P.op('dve', lambda e, j=j: e.memset(halfn[:, j:j + 1], float((j + 1) * 64)), writes=['ds_halfn'])
    PT = [P.sb([128, 4, 128], BF16, f"ds_PT{l}_{i}") for i in range(2)]
    JS = list(range(2, NT))
    ENG = {j: ('act' if j in DSA_ACT_J else 'dve') for j in JS}
    allk = lambda nm: [f'ds_{nm}{j}' for j in JS]
    c2 = slice(2, NT)
    P.op('dve', lambda e: e.tensor_tensor(out=stt['rng'][:, c2], in0=stt['mx'][:, c2], in1=stt['lo'][:, c2], op=ALU.subtract), reads=allk('mx') + allk('lo'), writes=['ds_rng'])
    for b in range(NBIS):
        P.op('dve', lambda e, b=b: e.tensor_scalar(out=stt['wc'][:, c2], in0=stt['rng'][:, c2], scalar1=float(0.5 ** (b + 1)), scalar2=None, op0=ALU.mult), reads=['ds_rng'], writes=['ds_wc'])
        P.op('dve', lambda e: e.tensor_tensor(out=stt['mid'][:, c2], in0=stt['lo'][:, c2], in1=stt['wc'][:, c2], op=ALU.add), reads=allk('lo') + ['ds_wc'], writes=['ds_mid'])
        ca = slice(DSA_ACT_J[0], DSA_ACT_J[-1] + 1)
        P.op('dve', lambda e, ca=ca: e.scalar_tensor_tensor(out=nmid[:, ca], in0=stt['lo'][:, ca], scalar=-1.0, in1=stt['wc'][:, ca], op0=ALU.mult, op1=ALU.subtract), reads=allk('lo') + ['ds_wc'], writes=['ds_nmid'])
        for j in JS:
            n, o, eng = (j + 1) * 128, OFF[j], ENG[j]
            if eng == 'dve':
                P.op(eng, lambda e, j=j, n=n, o=o, eng=eng: e.tensor_scalar(out=junkr[eng][j % 3][:, 0:n], in0=sc[:, o:o + n], scalar1=stt['mid'][:, j:j + 1], scalar2=None, op0=ALU.is_ge, op1=ALU.add, accum_out=cnt[:, j:j + 1]),
                     reads=['ds_mid', f'ds_sc{j}'], writes=[f'ds_cnt{j}', f'ds_junkr_{eng}{j % 3}'])
            else:
                P.op(eng, lambda e, j=j, n=n, o=o, eng=eng: e.activation(out=junkr[eng][j % 3][:, 0:n], in_=sc[:, o:o + n], func=AF.Sign, bias=nmid[:, j:j + 1], accum_out=ssum[:, j:j + 1]),
                     reads=['ds_nmid', f'ds_sc{j}'], writes=[f'ds_ssum{j}', f'ds_junkr_{eng}{j % 3}'])
        P.op('dve', lambda e, ca=ca: e.scalar_tensor_tensor(out=cnt[:, ca], in0=ssum[:, ca], scalar=0.5, in1=halfn[:, ca], op0=ALU.mult, op1=ALU.add), reads=[f'ds_ssum{j}' for j in DSA_ACT_J] + ['ds_halfn'], writes=[f'ds_cnt{j}' for j in DSA_ACT_J])
        P.op('dve', lambda e: e.scalar_tensor_tensor(out=stt['sel'][:, c2], in0=cnt[:, c2], scalar=255.5, in1=stt['wc'][:, c2], op0=ALU.is_ge, op1=ALU.mult), reads=allk('cnt') + ['ds_wc'], writes=['ds_sel'])
        P.op('dve', lambda e: e.tensor_tensor(out=stt['lo'][:, c2], in0=stt['lo'][:, c2], in1=stt['sel'][:, c2], op=ALU.add), reads=allk('lo') + ['ds_sel'], writes=allk('lo'))
    for j in range(NT):
        n, o = (j + 1) * 128, OFF[j]
        ksc, kmk = f'ds_sc{j}', f'ds_mpm{j}'
        if j < 2:
            P.op('dve', lambda e, n=n, o=o: e.tensor_scalar(out=mpm[:, o:o + n], in0=sc[:, o:o + n], scalar1=-1.0e29, scalar2=-1.0, op0=ALU.is_lt, op1=ALU.mult), reads=[ksc], writes=[kmk])
            continue
        eng = 'dve'
        a, t_, jk = tA[eng], tE[eng], junk[eng]
        ka, ke, kj = f'ds_tA_{eng}', f'ds_tE_{eng}', f'ds_junk_{eng}'
        thr, need = stt['thr'][:, j:j + 1], stt['need'][:, j:j + 1]
        lo = stt['lo'][:, j:j + 1]
        P.op(eng, lambda e, a=a, n=n, o=o, lo=lo: e.tensor_scalar(out=a[:, 0:n], in0=sc[:, o:o + n], scalar1=lo, scalar2=1.0e37, op0=ALU.is_lt, op1=ALU.mult), reads=[ksc, f'ds_lo{j}'], writes=[ka])
        P.op(eng, lambda e, a=a, n=n, o=o: e.tensor_tensor(out=a[:, 0:n], in0=a[:, 0:n], in1=sc[:, o:o + n], op=ALU.add), reads=[ksc, ka], writes=[ka])
        if eng == 'dve':
            P.op(eng, lambda e, a=a, n=n, thr=thr: e.tensor_reduce(out=thr, in_=a[:, 0:n], axis=AX.X, op=ALU.min), reads=[ka], writes=[f'ds_thr{j}'])
        else:
            P.op(eng, lambda e, a=a, n=n, thr=thr, t_=t_: e.tensor_scalar(out=t_[:, 0:n], in0=a[:, 0:n], scalar1=0.0, scalar2=None, op0=ALU.add, op1=ALU.min, accum_out=thr), reads=[ka], writes=[f'ds_thr{j}', ke])
        P.op(eng, lambda e, jk=jk, n=n, o=o, thr=thr, need=need: e.tensor_scalar(out=jk[:, 0:n], in0=sc[:, o:o + n], scalar1=thr, scalar2=None, op0=ALU.is_le, op1=ALU.add, accum_out=need), reads=[ksc, f'ds_thr{j}'], writes=[kj, f'ds_need{j}'])
        P.op(eng, lambda e, need=need, n=n: e.tensor_scalar(out=need, in0=need, scalar1=float(256 - n), scalar2=None, op0=ALU.add), reads=[f'ds_need{j}'], writes=[f'ds_need{j}'])
        P.op(eng, lambda e, t_=t_, n=n, o=o, thr=thr: e.tensor_scalar(out=t_[:, 0:n], in0=sc[:, o:o + n], scalar1=thr, scalar2=None, op0=ALU.is_equal), reads=[ksc, f'ds_thr{j}'], writes=[ke])
        P.op(eng, lambda e, a=a, t_=t_, n=n: e.tensor_tensor_scan(out=a[:, 0:n], data0=C.ones_f[:, 0:1].to_broadcast([128, n]), data1=t_[:, 0:n], initial=0.0, op0=ALU.mult, op1=ALU.add), reads=[ke, 'cst'], writes=[ka])
        P.op(eng, lambda e, a=a, t_=t_, n=n, need=need: e.scalar_tensor_tensor(out=a[:, 0:n], in0=a[:, 0:n], scalar=need, in1=t_[:, 0:n], op0=ALU.is_le, op1=ALU.mult), reads=[ka, ke, f'ds_need{j}'], writes=[ka])
        P.op(eng, lambda e, a=a, jk=jk, n=n, o=o: e.tensor_tensor(out=mpm[:, o:o + n], in0=a[:, 0:n], in1=jk[:, 0:n], op=ALU.subtract), reads=[ka, kj], writes=[kmk])
    items = [(j, kt) for j in range(NT) for kt in range(j + 1)]
    po = pb[7][:, 0:260].rearrange("p (h c) -> p h c", h=4)

    def qk(idx):
        j, kt = items[idx]
        i = idx % 4
        o = OFF[j]
        bs4 = pb[i][:].rearrange("p (h q) -> p h q", h=4)
        ks = f'pb{i}'
        P.op('pe', lambda e: e.matmul(bs4, lhsT=kT[:, 0, kt * 128:(kt + 1) * 128], rhs=qT[:, :, j * 128:(j + 1) * 128], start=True, stop=False),
             reads=['ds_q', 'ds_k'], writes=[ks])
        P.op('pe', lambda e: e.matmul(bs4, lhsT=mpm[:, o + kt * 128:o + (kt + 1) * 128], rhs=identB[:].unsqueeze(1).to_broadcast([128, 4, 128]), start=False, stop=True),
             reads=[f'ds_mpm{j}', 'ds_identB'], writes=[ks])
        pi = idx % 2
        P.op('act', lambda e: e.activation(out=PT[pi][:], in_=bs4, func=AF.Exp, scale=0.125), reads=[ks], writes=[f'ds_PT{pi}'])

    def pv(idx):
        j, kt = items[idx]
        pi = idx % 2
        if kt == 0:
            P.op('dve', lambda e: e.memset(pb[7][:, 0:260], 0.0), writes=['pb7'])
        for h in range(4):
            P.op('pe', lambda e, h=h: e.matmul(po[:, h, :], lhsT=PT[pi][:, h, :], rhs=va[:, kt, 0, :], start=False, stop=(kt == j), skip_group_check=True),
                 reads=[f'ds_PT{pi}', 'ds_v'], writes=['pb7'])
        if kt == j:
            P.op('dve', lambda e: e.reciprocal(out=rec[:], in_=po[:, :, 64]), reads=['pb7'], writes=['ds_rec'])
            for h in range(4):
                P.op('dve', lambda e, h=h: e.tensor_scalar(out=ods[:, j, h * 64:(h + 1) * 64], in0=po[:, h, 0:64], scalar1=rec[:, h:h + 1], scalar2=None, op0=ALU.mult), reads=['pb7', 'ds_rec'], writes=['ds_o'])

    qk(0)
    for idx in range(len(items)):
        if idx + 1 < len(items):
            qk(idx + 1)
        pv(idx)
    P.dma('sp', L.otok.rearrange("(t p) c -> p t c", p=128)[:, :, 1024:1280], ods[:], reads=['ds_o'], writes=[f'otok{l}_ds'])
```
